# Optimizing a Trainium2 kernel written in Bass

```python
import jax
import jax.numpy as jnp
from jax import lax
import numpy as np

D_MODEL = 1024
BATCH = 8
SEQ = 4096
DEPTH = 1

D_MIX = D_MODEL
D_ATT = D_MIX // 2
ATT_HEAD_DIM = 64
ATT_HEADS = D_ATT // ATT_HEAD_DIM
D_MLSTM = D_MIX - D_ATT
MLSTM_HEADS = 4
MLSTM_HEAD_DIM = D_MLSTM // MLSTM_HEADS
IN_WIDTH = 3 * D_ATT + 4 * D_MLSTM + 2 * MLSTM_HEADS

MOBA_BLOCK = 256
MOBA_TOPK = 3
MOBA_Q_CHUNK = 32
ROPE_THETA = 10000.0

MLSTM_CHUNK = 128
CONV_WIDTH = 4

N_GROUPS = 4
EXPERTS_PER_GROUP = 8
N_EXPERTS = N_GROUPS * EXPERTS_PER_GROUP
TOP_K_EXPERTS = 2
D_EXPERT = 512
MOE_BLOCK_ROWS = 256

NORM_EPS = 1e-6
NEG_INF = -1e30

kernel_name = "hymba_moba_mlstm_hmoe_layer"


def rms_norm(x, g):
    xf = x.astype(jnp.float32)
    y = xf * lax.rsqrt(jnp.mean(xf * xf, axis=-1, keepdims=True) + NORM_EPS)
    return (y * g.astype(jnp.float32)).astype(x.dtype)


def head_rms_norm(h, g, n_heads):
    b, s, w = h.shape
    hf = h.astype(jnp.float32).reshape(b, s, n_heads, w // n_heads)
    hf = hf * lax.rsqrt(jnp.mean(hf * hf, axis=-1, keepdims=True) + NORM_EPS)
    return (hf.reshape(b, s, w) * g.astype(jnp.float32)).astype(h.dtype)


def modulate(x, g, shift, scale):
    return rms_norm(x, g) * (1.0 + scale[:, None, :]) + shift[:, None, :]


def split_heads(t, n_heads):
    b, s, w = t.shape
    return t.reshape(b, s, n_heads, w // n_heads).transpose(0, 2, 1, 3)


def merge_heads(t):
    b, h, s, d = t.shape
    return t.transpose(0, 2, 1, 3).reshape(b, s, h * d)


def rope(t, positions):
    half = t.shape[-1] // 2
    inv_freq = ROPE_THETA ** (-jnp.arange(half, dtype=jnp.float32) / half)
    ang = positions.astype(jnp.float32)[:, None, :, None] * inv_freq
    cos, sin = jnp.cos(ang), jnp.sin(ang)
    tf = t.astype(jnp.float32)
    t1, t2 = tf[..., :half], tf[..., half:]
    return jnp.concatenate([t1 * cos - t2 * sin, t2 * cos + t1 * sin], axis=-1).astype(t.dtype)


def moba_attention(q, k, v):
    b, h, s, dh = q.shape
    nb = -(-s // MOBA_BLOCK)
    s_pad = nb * MOBA_BLOCK
    pad = ((0, 0), (0, 0), (0, s_pad - s), (0, 0))
    kb = jnp.pad(k, pad).reshape(b, h, nb, MOBA_BLOCK, dh)
    vb = jnp.pad(v, pad).reshape(b, h, nb, MOBA_BLOCK, dh)
    scale = dh ** -0.5

    k_mean = jnp.mean(kb.astype(jnp.float32), axis=3)
    gate = jnp.einsum('bhsd,bhnd->bhsn', q.astype(jnp.float32), k_mean)
    q_blk = jnp.arange(s) // MOBA_BLOCK
    past = jnp.arange(nb)[None, :] < q_blk[:, None]
    gate = jnp.where(past, gate, NEG_INF)
    topk = min(MOBA_TOPK, nb)
    _, sel = lax.top_k(gate, topk)
    sel_valid = sel < q_blk[:, None]

    nqc = s // MOBA_Q_CHUNK

    def to_chunks(t):
        t = t.reshape((b, h, nqc, MOBA_Q_CHUNK) + t.shape[3:])
        return jnp.moveaxis(t, 2, 0)

    bi = jnp.arange(b)[:, None, None]
    hi = jnp.arange(h)[None, :, None]

    def chunk(args):
        q_c, sel_c, valid_c, ci = args
        start = ci * MOBA_Q_CHUNK
        j = start // MOBA_BLOCK
        k_own = lax.dynamic_index_in_dim(kb, j, axis=2, keepdims=False)
        v_own = lax.dynamic_index_in_dim(vb, j, axis=2, keepdims=False)
        q_pos = start + jnp.arange(MOBA_Q_CHUNK)
        k_pos = j * MOBA_BLOCK + jnp.arange(MOBA_BLOCK)
        causal = k_pos[None, :] <= q_pos[:, None]
        qf = q_c.astype(jnp.float32) * scale
        s_own = jnp.where(causal, jnp.einsum('bhqd,bhkd->bhqk', qf, k_own.astype(jnp.float32)), NEG_INF)
        scores = [s_own]
        for r in range(topk):
            k_r = kb[bi, hi, sel_c[..., r]]
            s_r = jnp.einsum('bhqd,bhqkd->bhqk', qf, k_r.astype(jnp.float32))
            scores.append(jnp.where(valid_c[..., r, None], s_r, NEG_INF))
        s_all = jnp.stack(scores, axis=3)
        p = jax.nn.softmax(s_all.reshape(b, h, MOBA_Q_CHUNK, -1), axis=-1).reshape(s_all.shape)
        out = jnp.einsum('bhqk,bhkd->bhqd', p[..., 0, :], v_own.astype(jnp.float32))
        for r in range(topk):
            v_r = vb[bi, hi, sel_c[..., r]]
            out = out + jnp.einsum('bhqk,bhqkd->bhqd', p[..., r + 1, :], v_r.astype(jnp.float32))
        return out.astype(v.dtype)

    out = lax.map(chunk, (to_chunks(q), to_chunks(sel), to_chunks(sel_valid),
                          jnp.arange(nqc, dtype=jnp.int32)))
    return jnp.moveaxis(out, 0, 2).reshape(b, h, s, dh)


def mlstm_chunkwise(q, k, v, i_pre, f_pre):
    b, nh, s, dh = q.shape
    L = MLSTM_CHUNK
    nc = s // L
    q = q.reshape(b, nh, nc, L, dh)
    k = k.reshape(b, nh, nc, L, dh)
    v = v.reshape(b, nh, nc, L, dh)
    logf = jax.nn.log_sigmoid(f_pre).reshape(b, nh, nc, L)
    ig = i_pre.reshape(b, nh, nc, L)
    cum = jnp.cumsum(logf, axis=-1)
    a = cum[..., -1]

    g = a[..., None] - cum + ig
    m_loc = jnp.max(g, axis=-1)
    w = jnp.exp(g - m_loc[..., None])
    c_loc = jnp.einsum('bhcs,bhcse,bhcsd->bhced', w, v, k)
    n_loc = jnp.einsum('bhcs,bhcsd->bhcd', w, k)

    def step(carry, inp):
        c_st, n_st, m_st = carry
        a_c, m_loc_c, c_loc_c, n_loc_c = inp
        m_new = jnp.maximum(a_c + m_st, m_loc_c)
        s_prev = jnp.exp(a_c + m_st - m_new)
        s_loc = jnp.exp(m_loc_c - m_new)
        c_new = s_prev[..., None, None] * c_st + s_loc[..., None, None] * c_loc_c
        n_new = s_prev[..., None] * n_st + s_loc[..., None] * n_loc_c
        return (c_new, n_new, m_new), (c_st, n_st, m_st)

    init = (jnp.zeros((b, nh, dh, dh), jnp.float32),
            jnp.zeros((b, nh, dh), jnp.float32),
            jnp.zeros((b, nh), jnp.float32))
    xs = (jnp.moveaxis(a, 2, 0), jnp.moveaxis(m_loc, 2, 0),
          jnp.moveaxis(c_loc, 2, 0), jnp.moveaxis(n_loc, 2, 0))
    _, (c_prev, n_prev, m_prev) = lax.scan(step, init, xs)
    c_prev = jnp.moveaxis(c_prev, 0, 2)
    n_prev = jnp.moveaxis(n_prev, 0, 2)
    m_prev = jnp.moveaxis(m_prev, 0, 2)

    causal = jnp.tril(jnp.ones((L, L), dtype=bool))
    dmat = jnp.where(causal, cum[..., :, None] - cum[..., None, :] + ig[..., None, :], NEG_INF)
    inter = cum + m_prev[..., None]
    m_t = jnp.maximum(inter, jnp.max(dmat, axis=-1))
    s_qk = jnp.einsum('bhctd,bhcsd->bhcts', q, k) * jnp.exp(dmat - m_t[..., None])
    w_inter = jnp.exp(inter - m_t)
    num = (jnp.einsum('bhcts,bhcse->bhcte', s_qk, v)
           + w_inter[..., None] * jnp.einsum('bhced,bhctd->bhcte', c_prev, q))
    den = jnp.sum(s_qk, axis=-1) + w_inter * jnp.einsum('bhcd,bhctd->bhct', n_prev, q)
    h = num / jnp.maximum(jnp.abs(den), jnp.exp(-m_t))[..., None]
    return h.reshape(b, nh, s, dh)


def causal_conv(u, w, bias):
    ch = u.shape[-1]
    out = lax.conv_general_dilated(u, w[:, None, :], window_strides=(1,),
                                   padding=[(CONV_WIDTH - 1, 0)],
                                   dimension_numbers=('NWC', 'WIO', 'NWC'),
                                   feature_group_count=ch)
    return out + bias


def hier_moe(h, w_rg, b_rg, w_re, b_re, w1, w3, w2):
    t, d = h.shape
    gp = jax.nn.softmax((h @ w_rg + b_rg).astype(jnp.float32), axis=-1)
    g_w, g_idx = lax.top_k(gp, 1)
    el = (h @ w_re + b_re).astype(jnp.float32).reshape(t, N_GROUPS, EXPERTS_PER_GROUP)
    el_sel = jnp.take_along_axis(el, g_idx[:, :, None], axis=1)[:, 0]
    ep = jax.nn.softmax(el_sel, axis=-1)
    e_w, e_loc = lax.top_k(ep, TOP_K_EXPERTS)
    weights = g_w * e_w / jnp.sum(e_w, axis=-1, keepdims=True)
    expert_id = g_idx * EXPERTS_PER_GROUP + e_loc

    n_assign = t * TOP_K_EXPERTS
    flat_e = expert_id.reshape(-1)
    flat_w = weights.reshape(-1)
    flat_tok = jnp.repeat(jnp.arange(t, dtype=jnp.int32), TOP_K_EXPERTS)
    order = jnp.argsort(flat_e)
    sorted_e = flat_e[order]
    counts = jnp.bincount(flat_e, length=N_EXPERTS)
    padded = ((counts + MOE_BLOCK_ROWS - 1) // MOE_BLOCK_ROWS) * MOE_BLOCK_ROWS
    pad_end = jnp.cumsum(padded)
    pad_start = pad_end - padded
    cnt_start = jnp.cumsum(counts) - counts
    dest = pad_start[sorted_e] + (jnp.arange(n_assign) - cnt_start[sorted_e])
    n_rows = n_assign + N_EXPERTS * MOE_BLOCK_ROWS
    n_blocks = n_rows // MOE_BLOCK_ROWS
    row_tok = jnp.zeros((n_rows,), jnp.int32).at[dest].set(flat_tok[order])
    row_w = jnp.zeros((n_rows,), jnp.float32).at[dest].set(flat_w[order])
    block_e = jnp.minimum(jnp.searchsorted(pad_end, jnp.arange(n_blocks) * MOE_BLOCK_ROWS, side='right'),
                          N_EXPERTS - 1)
    xs = h[row_tok].reshape(n_blocks, MOE_BLOCK_ROWS, d)

    def expert_block(args):
        xb, e = args
        return (jax.nn.silu(xb @ w1[e]) * (xb @ w3[e])) @ w2[e]

    ys = lax.map(expert_block, (xs, block_e)).reshape(n_rows, d)
    ys = ys * row_w[:, None].astype(h.dtype)
    return jnp.zeros((t, d), h.dtype).at[row_tok].add(ys)


def setup_inputs(seed: int = 0) -> dict:
    key = jax.random.key(seed)
    ks = jax.random.split(key, 24)

    def nrm(k, shape, scale):
        return jax.random.normal(k, shape, jnp.float32) * scale

    x = nrm(ks[0], (BATCH, SEQ, D_MODEL), 1.0)
    c = nrm(ks[1], (BATCH, D_MODEL), 1.0)
    positions = (jnp.arange(SEQ, dtype=jnp.int32)[None, :]
                 + jax.random.randint(ks[2], (BATCH, 1), 0, 1024, dtype=jnp.int32))
    w_ada = nrm(ks[3], (DEPTH, D_MODEL, 6 * D_MODEL), 0.5 * D_MODEL ** -0.5)
    b_ada = nrm(ks[4], (DEPTH, 6 * D_MODEL), 0.02)
    norm1_g = 1.0 + nrm(ks[5], (DEPTH, D_MODEL), 0.02)
    w_in = nrm(ks[6], (DEPTH, D_MODEL, IN_WIDTH), D_MODEL ** -0.5)
    b_gate = jnp.concatenate([
        nrm(ks[7], (DEPTH, MLSTM_HEADS), 0.1),
        jnp.linspace(3.0, 6.0, MLSTM_HEADS, dtype=jnp.float32)[None, :]
        + nrm(ks[8], (DEPTH, MLSTM_HEADS), 0.1)], axis=-1)
    conv_w = nrm(ks[9], (DEPTH, CONV_WIDTH, 2 * D_MLSTM), CONV_WIDTH ** -0.5)
    conv_b = nrm(ks[10], (DEPTH, 2 * D_MLSTM), 0.02)
    attn_out_g = 1.0 + nrm(ks[11], (DEPTH, D_ATT), 0.02)
    mlstm_out_g = 1.0 + nrm(ks[12], (DEPTH, D_MLSTM), 0.02)
    w_out = nrm(ks[13], (DEPTH, D_MIX, D_MODEL), D_MIX ** -0.5)
    norm2_g = 1.0 + nrm(ks[14], (DEPTH, D_MODEL), 0.02)
    w_rg = nrm(ks[15], (DEPTH, D_MODEL, N_GROUPS), D_MODEL ** -0.5)
    b_rg = nrm(ks[16], (DEPTH, N_GROUPS), 0.01)
    w_re = nrm(ks[17], (DEPTH, D_MODEL, N_EXPERTS), D_MODEL ** -0.5)
    b_re = nrm(ks[18], (DEPTH, N_EXPERTS), 0.01)
    w1 = nrm(ks[19], (DEPTH, N_EXPERTS, D_MODEL, D_EXPERT), D_MODEL ** -0.5)
    w3 = nrm(ks[20], (DEPTH, N_EXPERTS, D_MODEL, D_EXPERT), D_MODEL ** -0.5)
    w2 = nrm(ks[21], (DEPTH, N_EXPERTS, D_EXPERT, D_MODEL), D_EXPERT ** -0.5)
    norm_f_g = 1.0 + nrm(ks[22], (D_MODEL,), 0.02)
    return {"x": x, "c": c, "positions": positions, "w_ada": w_ada, "b_ada": b_ada,
            "norm1_g": norm1_g, "w_in": w_in, "b_gate": b_gate, "conv_w": conv_w,
            "conv_b": conv_b, "attn_out_g": attn_out_g, "mlstm_out_g": mlstm_out_g,
            "w_out": w_out, "norm2_g": norm2_g, "w_rg": w_rg, "b_rg": b_rg,
            "w_re": w_re, "b_re": b_re, "w1": w1, "w3": w3, "w2": w2, "norm_f_g": norm_f_g}


def reference(x, c, positions, w_ada, b_ada, norm1_g, w_in, b_gate, conv_w, conv_b,
              attn_out_g, mlstm_out_g, w_out, norm2_g, w_rg, b_rg, w_re, b_re,
              w1, w3, w2, norm_f_g):
    b, s, d = x.shape
    offs = [D_ATT, 2 * D_ATT, 3 * D_ATT, 3 * D_ATT + 2 * D_MLSTM,
            3 * D_ATT + 3 * D_MLSTM, 3 * D_ATT + 4 * D_MLSTM]
    for l in range(DEPTH):
        mod = jax.nn.silu(c) @ w_ada[l] + b_ada[l]
        sh1, sc1, g1, sh2, sc2, g2 = jnp.split(mod, 6, axis=-1)

        h = modulate(x, norm1_g[l], sh1, sc1)
        proj = h @ w_in[l]
        q_a, k_a, v_a, qk_m, v_m, o_m, gates = jnp.split(proj, offs, axis=-1)

        qa = rope(split_heads(q_a, ATT_HEADS), positions)
        ka = rope(split_heads(k_a, ATT_HEADS), positions)
        attn = merge_heads(moba_attention(qa, ka, split_heads(v_a, ATT_HEADS)))
        attn = head_rms_norm(attn, attn_out_g[l], ATT_HEADS)

        qk_m = jax.nn.silu(causal_conv(qk_m, conv_w[l], conv_b[l]))
        q_m, k_m = jnp.split(qk_m, 2, axis=-1)
        gates = gates.astype(jnp.float32) + b_gate[l].astype(jnp.float32)
        i_pre = jnp.transpose(gates[..., :MLSTM_HEADS], (0, 2, 1))
        f_pre = jnp.transpose(gates[..., MLSTM_HEADS:], (0, 2, 1))
        hm = mlstm_chunkwise(split_heads(q_m, MLSTM_HEADS).astype(jnp.float32),
                             split_heads(k_m, MLSTM_HEADS).astype(jnp.float32) * MLSTM_HEAD_DIM ** -0.5,
                             split_heads(v_m, MLSTM_HEADS).astype(jnp.float32),
                             i_pre, f_pre)
        hm = head_rms_norm(merge_heads(hm), mlstm_out_g[l], MLSTM_HEADS)
        hm = (hm * jax.nn.sigmoid(o_m.astype(jnp.float32))).astype(x.dtype)

        y = jnp.concatenate([attn.astype(x.dtype), hm], axis=-1) @ w_out[l]
        x = x + g1[:, None, :] * y

        h2 = modulate(x, norm2_g[l], sh2, sc2)
        moe = hier_moe(h2.reshape(b * s, d), w_rg[l], b_rg[l], w_re[l], b_re[l],
                       w1[l], w3[l], w2[l]).reshape(b, s, d)
        x = x + g2[:, None, :] * moe
    return rms_norm(x, norm_f_g)
```

```python
import contextlib
import os
import numpy as np
import concourse.bass as bass
import concourse.mybir as mybir
from concourse.bass_utils import run_bass_kernel_spmd

F32 = mybir.dt.float32
BF16 = mybir.dt.bfloat16
I32 = mybir.dt.int32
U32 = mybir.dt.uint32
AF = mybir.ActivationFunctionType
ALU = mybir.AluOpType
AX = mybir.AxisListType

ENGS = ['pe', 'act', 'dve', 'pool', 'sp']


class Buf:
    __slots__ = ('name', 't', 'w', 'r')

    def __init__(self, name, t=None):
        self.name = name
        self.t = t
        self.w = None
        self.r = {}


class Sched:
    def __init__(self, nc, n_dma_sems=32, same_engine_sync=True):
        self.nc = nc
        self.stack = contextlib.ExitStack()
        self.lists = {e: [] for e in ENGS}
        self.cnt = {e: 0 for e in ENGS}
        self.esem = {e: self.stack.enter_context(nc.semaphore(f"s_{e}")) for e in ['pe', 'act', 'dve', 'pool']}
        self.dsem = [self.stack.enter_context(nc.semaphore(f"d_{i}")) for i in range(n_dma_sems)]
        self.dcnt = [0] * n_dma_sems
        self.dnext = 0
        self.swsem = [self.stack.enter_context(nc.semaphore(f"w_{i}")) for i in range(40)]
        self.swcnt = [0] * 40
        self.swnext = 0
        self.mark_t = self.stack.enter_context(nc.sbuf_tensor("mark_t", [1, 8], F32))
        self.waited = {e: {} for e in ENGS}
        self.same_engine_sync = same_engine_sync
        self.nops = 0

    def sb(self, name, shape, dtype):
        return Buf(name, self.stack.enter_context(self.nc.sbuf_tensor(name, shape, dtype)))

    def ps(self, name, shape, dtype):
        return Buf(name, self.stack.enter_context(self.nc.psum_tensor(name, shape, dtype)))

    def view(self, name, t):
        return Buf(name, t)

    def store(self, out, in_, reads):
        return self.swdma(lambda e, out=out, in_=in_: e.dma_start(out=out, in_=in_), reads, ())

    def sem_of(self, k):
        if k[0] == 'e':
            return self.esem[k[1]]
        if k[0] == 'w':
            return self.swsem[k[1]]
        return self.dsem[k[1]]

    def swdma(self, fn, reads=(), writes=()):
        deps = self._deps(reads, writes)
        i = self.swnext
        self.swnext = (i + 1) % len(self.swsem)
        if self.swcnt[i] > 0:
            kk = ('w', i)
            deps[kk] = max(deps.get(kk, 0), 16 * self.swcnt[i])
        ws = self._waits('pool', deps)
        self.swcnt[i] += 1
        tok = (('w', i), 16 * self.swcnt[i])
        self.lists['pool'].append((ws, fn, (self.swsem[i], 16)))
        self._commit(tok, reads, writes)
        self.nops += 1
        return tok

    def _deps(self, reads, writes):
        deps = {}

        def add(k, v):
            if deps.get(k, 0) < v:
                deps[k] = v
        for b in reads:
            if b.w is not None:
                add(*b.w)
        for b in writes:
            if b.w is not None:
                add(*b.w)
            for k, v in b.r.items():
                add(k, v)
        return deps

    def _waits(self, eng, deps):
        ws = []
        for k, v in deps.items():
            if k == ('e', eng) and (eng == 'pe' or not self.same_engine_sync):
                continue
            if self.waited[eng].get(k, 0) >= v:
                continue
            self.waited[eng][k] = v
            ws.append((k, v))
        return ws

    def _commit(self, tok, reads, writes):
        k, v = tok
        for b in reads:
            if b.r.get(k, 0) < v:
                b.r[k] = v
        for b in writes:
            b.w = tok
            b.r = {}

    def op(self, eng, fn, reads=(), writes=()):
        deps = self._deps(reads, writes)
        ws = self._waits(eng, deps)
        self.cnt[eng] += 1
        tok = (('e', eng), self.cnt[eng])
        self.lists[eng].append((ws, fn, (self.esem[eng], 1)))
        self._commit(tok, reads, writes)
        self.nops += 1
        return tok

    def dma(self, eng, out, in_, reads=(), writes=(), fn=None, **kw):
        deps = self._deps(reads, writes)
        i = self.dnext
        self.dnext = (self.dnext + 1) % len(self.dsem)
        if self.dcnt[i] > 0:
            k = ('d', i)
            deps[k] = max(deps.get(k, 0), 16 * self.dcnt[i])
        ws = self._waits(eng, deps)
        self.dcnt[i] += 1
        tok = (('d', i), 16 * self.dcnt[i])
        if fn is None:
            def fn(e, out=out, in_=in_, kw=kw):
                return e.dma_start(out=out, in_=in_, **kw)
        self.lists[eng].append((ws, fn, (self.dsem[i], 16)))
        self._commit(tok, reads, writes)
        self.nops += 1
        return tok

    def finish(self):
        nc = self.nc
        fin = []
        for i in range(len(self.dsem)):
            if self.dcnt[i] > 0 and self.waited['sp'].get(('d', i), 0) < 16 * self.dcnt[i]:
                fin.append((('d', i), 16 * self.dcnt[i]))
        for i in range(len(self.swsem)):
            if self.swcnt[i] > 0:
                fin.append((('w', i), 16 * self.swcnt[i]))
        for e in ['pe', 'act', 'dve', 'pool']:
            if self.cnt[e] > 0:
                fin.append((('e', e), self.cnt[e]))
        self.lists['sp'].append((fin, None, None))

        def replay(name, eng):
            for ent in self.lists[name]:
                ws, fn, inc = ent[0], ent[1], ent[2]
                for k, v in ws:
                    eng.wait_ge(self.sem_of(k), v)
                if len(ent) > 3:
                    for sm_ in ent[3]:
                        eng.sem_clear(sm_)
                if fn is not None:
                    ins = fn(eng)
                    ins.then_inc(inc[0], inc[1])

        with nc.Block() as block:
            @block.tensor
            def _(e):
                replay('pe', e)

            @block.scalar
            def _(e):
                replay('act', e)

            @block.vector
            def _(e):
                replay('dve', e)

            @block.gpsimd
            def _(e):
                replay('pool', e)

            @block.sync
            def _(e):
                replay('sp', e)
        self.stack.close()

    def make_identity(self, ident_bf, ident_f32):
        for b in (ident_bf, ident_f32):
            if b is None:
                continue
            n = b.t.shape[0]
            m = b.t.shape[1]
            self.op('pool', lambda e, b=b: e.memset(b.t[:], 1.0), writes=[b])
            self.op('pool', lambda e, b=b, m=m: e.affine_select(
                out=b.t[:], in_=b.t[:], pattern=[[-1, m]], compare_op=ALU.is_equal,
                fill=0.0, base=0, channel_multiplier=1), reads=[b], writes=[b])

    def barrier(self):
        cur = {}
        for e in ['pe', 'act', 'dve', 'pool']:
            if self.cnt[e] > 0:
                cur[('e', e)] = self.cnt[e]
        for i in range(len(self.dsem)):
            if self.dcnt[i] > 0:
                cur[('d', i)] = 16 * self.dcnt[i]
        for i in range(len(self.swsem)):
            if self.swcnt[i] > 0:
                cur[('w', i)] = 16 * self.swcnt[i]
        for e in ENGS:
            ws = []
            for k, v in cur.items():
                if self.waited[e].get(k, 0) < v:
                    self.waited[e][k] = v
                    ws.append((k, v))
            if ws:
                self.lists[e].append((ws, None, None))

    def tt(self, eng, out, in0, in1, op, R, W):
        return self.op(eng, lambda e: e.tensor_tensor(out, in0, in1, op=op), R, W)

    def ts(self, eng, out, in0, s1, s2, op0, op1, R, W, accum_out=None):
        if accum_out is not None:
            return self.op(eng, lambda e: e.tensor_scalar(out, in0, s1, s2, op0, op1, accum_out=accum_out), R, W)
        if op1 is None:
            return self.op(eng, lambda e: e.tensor_scalar(out, in0, s1, None, op0), R, W)
        return self.op(eng, lambda e: e.tensor_scalar(out, in0, s1, s2, op0, op1), R, W)

    def stt(self, eng, out, in0, sc, in1, op0, op1, R, W):
        return self.op(eng, lambda e: e.scalar_tensor_tensor(out, in0, sc, in1, op0, op1), R, W)

    def act(self, out, in_, func, R, W, bias=None, scale=None, accum_out=None):
        kw = {}
        if bias is not None:
            kw['bias'] = bias
        if scale is not None:
            kw['scale'] = scale
        if accum_out is not None:
            kw['accum_out'] = accum_out
        return self.op('act', lambda e: e.activation(out, in_, func, **kw), R, W)

    def cp(self, eng, out, in_, R, W):
        if eng == 'act':
            return self.op('act', lambda e: e.copy(out, in_), R, W)
        return self.op(eng, lambda e: e.tensor_copy(out, in_), R, W)

    def mm(self, out, lhsT, rhs, start, stop, R, W):
        return self.op('pe', lambda e: e.matmul(out, lhsT, rhs, start=start, stop=stop), R, W)

    def tr(self, out, in_, ident, R, W):
        return self.op('pe', lambda e: e.transpose(out, in_, ident), R, W)

    def ms(self, eng, ap, val, W):
        return self.op(eng, lambda e: e.memset(ap, val), (), W)


class Arena:
    def __init__(self, S, words):
        self.S = S
        self.base = S.stack.enter_context(S.nc.sbuf_tensor("arena", [128, words], F32))
        self.words = words
        self.top = 0

    def mark(self):
        return self.top

    def release(self, m):
        self.top = m

    def alloc(self, name, shape, dtype, parts=128):
        n = 1
        for s in shape[1:]:
            n *= s
        esz = 4 if dtype in (F32, I32, U32) else 2
        w = (n * esz + 3) // 4
        w = (w + 7) // 8 * 8
        assert self.top + w <= self.words, f"arena overflow at {name}: {self.top + w} > {self.words}"
        v = self.base[0:shape[0], self.top:self.top + w]
        if esz == 2:
            v = v.bitcast(BF16)
        elif dtype != F32:
            v = v.bitcast(dtype)
        v = v[:, 0:n]
        if len(shape) == 3:
            v = v.rearrange("p (a b) -> p a b", a=shape[1])
        elif len(shape) == 4:
            v = v.rearrange("p (a b c) -> p a b c", a=shape[1], b=shape[2])
        elif len(shape) == 5:
            v = v.rearrange("p (a b c d) -> p a b c d", a=shape[1], b=shape[2], c=shape[3])
        self.top += w
        return Buf(name, v)


D = 1024
EPS = 1e-6
TWO_PI = 6.283185307179586
C1 = 6.28125
C2 = TWO_PI - C1
MAGIC = 12582912.0
NEG = -1.0e30
N_EXP = 32


class K:
    pass


def build_nc(S_TOK=4096, CAP=1024, debug=False, phases="0123456"):
    nc = bass.Bass("TRN2", target_bir_lowering=False)
    NT = S_TOK // 128
    NBK = S_TOK // 256
    NQB = S_TOK // 512
    NP = 4 * NT
    NSLOT = N_EXP * CAP
    NG = min(CAP, 512)

    def din(name, shape, dt=F32):
        return nc.dram_tensor(name, shape, dt, kind="ExternalInput").ap()

    def dscr(name, shape, dt):
        return nc.dram_tensor(name, shape, dt, kind=("ExternalOutput" if debug else "Internal")).ap()

    x = din("x", [S_TOK, D])
    c_in = din("c", [8, 128])
    pos_in = din("positions", [1, S_TOK], I32)
    w_ada = din("w_ada", [D, 6 * D])
    b_ada = din("b_ada", [1, 6 * D])
    norm1_g = din("norm1_g", [1, D])
    w_in = din("w_in", [D, 3592])
    b_gate = din("b_gate", [8, 1])
    conv_w = din("conv_w", [4, D])
    conv_b = din("conv_b", [1, D])
    attn_g = din("attn_out_g", [1, 512])
    mlstm_g = din("mlstm_out_g", [1, 512])
    w_out = din("w_out", [D, D])
    norm2_g = din("norm2_g", [1, D])
    w_rg = din("w_rg", [D, 4])
    b_rg = din("b_rg", [1, 4])
    w_re = din("w_re", [D, 32])
    b_re = din("b_re", [1, 32])
    w1 = din("w1", [N_EXP, D, 512])
    w3 = din("w3", [N_EXP, D, 512])
    w2 = din("w2", [N_EXP, 512, D])
    normf_g = din("norm_f_g", [1, D])
    cst = din("cst", [128, 8])
    out = nc.dram_tensor("out", [S_TOK, D], F32, kind="ExternalOutput").ap()

    qT_d = dscr("qT_d", [512, S_TOK], BF16)
    kT_d = dscr("kT_d", [512, S_TOK], BF16)
    qkm_d = dscr("qkm_d", [1024, S_TOK], BF16)
    ig_d = dscr("ig_d", [4, S_TOK], F32)
    fg_d = dscr("fg_d", [4, S_TOK], F32)
    vm_d = dscr("vm_d", [S_TOK, 512], BF16)
    om_d = dscr("om_d", [S_TOK, 512], BF16)
    cat_d = dscr("cat_d", [S_TOK, D], BF16)
    x1_d = dscr("x1_d", [S_TOK, D], F32)
    XS_d = dscr("XS_d", [NSLOT, D], BF16)
    YS_d = dscr("YS_d", [NSLOT, D], BF16)

    S = Sched(nc, same_engine_sync=(os.environ.get("SES", "1") == "1"))
    xs_tok = Buf("xs_tok")
    A = Arena(S, 51200)
    pb = [S.ps(f"pb{i}", [128, 512], F32) for i in range(8)]

    def pbf(i):
        return pb[i].t[:].bitcast(BF16)

    ident_bf = A.alloc("ident_bf", [128, 128], BF16)
    ident_f = A.alloc("ident_f", [128, 128], F32)
    ones_f = A.alloc("ones_f", [128, 128], F32)
    ones_bf = A.alloc("ones_bf", [128, 128], BF16)
    tri_bf = A.alloc("tri_bf", [128, 128], BF16)
    stri_bf = A.alloc("stri_bf", [128, 128], BF16)
    cst_sb = A.alloc("cst_sb", [128, 8], F32)
    A1 = A.alloc("A1", [128, D], F32)
    B1 = A.alloc("B1", [128, D], F32)
    G1b = A.alloc("G1b", [128, D], F32)
    A2 = A.alloc("A2", [128, D], F32)
    B2 = A.alloc("B2", [128, D], F32)
    G2b = A.alloc("G2b", [128, D], F32)
    slot_i = A.alloc("slot_i", [128, NT, 2], I32)
    wts = A.alloc("wts", [128, NT, 2], F32)

    S.make_identity(ident_bf, ident_f)
    S.ms('pool', ones_f.t[:], 1.0, [ones_f])
    S.ms('pool', ones_bf.t[:], 1.0, [ones_bf])
    for b_, cmp_ in ((tri_bf, ALU.is_ge), (stri_bf, ALU.is_gt)):
        S.ms('pool', b_.t[:], 1.0, [b_])
        S.op('pool', lambda e, b_=b_, cmp_=cmp_: e.affine_select(
            out=b_.t[:], in_=b_.t[:], pattern=[[1, 128]], compare_op=cmp_,
            fill=0.0, base=0, channel_multiplier=-1), [b_], [b_])
    S.dma('sp', cst_sb.t[:], cst, (), [cst_sb])

    zt = A.alloc("zt", [128, 4, D], BF16)
    S.ms('pool', zt.t[:], 0.0, [zt])
    xs_v = XS_d.rearrange("(n p) d -> p n d", p=128)
    nrow = NSLOT // 128
    for i0 in range(0, nrow, 4):
        nn = min(4, nrow - i0)
        S.dma('act', xs_v[:, i0:i0 + nn, :], zt.t[:, 0:nn, :], [zt], [xs_tok])

    k = K()
    k.__dict__.update(locals())
    k._bcreg = None
    k.dbg_names = []

    def dbg(name, buf, ap, shape, dt):
        if not debug:
            return
        d_ = nc.dram_tensor("dbg_" + name, shape, dt, kind="ExternalOutput").ap()
        S.dma('sp', d_, ap, [buf], ())
        k.dbg_names.append("dbg_" + name)
    k.dbg = dbg

    def bcreg(e):
        if k._bcreg is None:
            k._bcreg = e.to_reg(NSLOT - 1)
        return k._bcreg
    k.bcreg = bcreg
    if '0' in phases:
        phase0_mod(k)
    if '1' in phases:
        phase_p1(k)
    if '2' in phases:
        phase_attn(k)
    if '3' in phases:
        phase_p2(k)
    if '4' in phases:
        phase_mlstm(k)
    if '5' in phases:
        phase_out(k)
    if '6' in phases:
        ph6 = os.environ.get("PH6", "ef")
        if 'e' in ph6:
            phase_experts(k)
        if 'f' in ph6:
            phase_final(k)
    S.finish()
    return nc


def rstd_from_ss(k, ss, tmp, rstd, n, R=()):
    S = k.S
    S.ts('dve', tmp.t[:], ss.t[:], 1.0 / n, EPS, ALU.mult, ALU.add, [ss], [tmp])
    S.act(tmp.t[:], tmp.t[:], AF.Sqrt, [tmp], [tmp])
    S.op('dve', lambda e: e.reciprocal(rstd.t[:], tmp.t[:]), [tmp], [rstd])


def load_w_bf16(k, dst_ap, dstbuf, src_ap, ncols, stg, perm=False, kc=8):
    S = k.S
    st = stg[k.stg_i % len(stg)]
    k.stg_i += 1
    sv = st.t[:, 0:kc, 0:ncols]
    S.dma('sp', sv, src_ap.rearrange("(c p) n -> p c n", p=128), (), [st])
    if not perm:
        S.cp('pool', dst_ap, sv, [st], [dstbuf])
    else:
        nh = ncols // 64
        d5 = dst_ap.rearrange("p c (h two j) -> p c h two j", two=2, j=32)
        s5 = sv.rearrange("p c (h two j) -> p c h two j", two=2, j=32)
        for cc in range(kc):
            S.cp('pool', d5[:, cc, :, 0, :], s5[:, cc, :, 1, :], [st], [dstbuf])
            S.cp('pool', d5[:, cc, :, 1, :], s5[:, cc, :, 0, :], [st], [dstbuf])


def bcast_load(k, dst, src_row):
    k.S.dma('sp', dst.t[:], src_row.partition_broadcast(128), (), [dst])


def phase0_mod(k):
    S, A = k.S, k.A
    m0 = A.mark()
    c8 = A.alloc("c8", [8, 128], F32)
    sc = A.alloc("sc", [128, 8], F32)
    rep = A.alloc("rep", [128, 8, 128], F32)
    brow = A.alloc("brow", [1, 6 * D], F32)
    wst = [A.alloc(f"wst{i}", [128, 8, 512], F32) for i in range(2)]
    modb = A.alloc("modb", [128, 6, D], F32)
    gn1 = A.alloc("gn1", [128, D], F32)
    gn2 = A.alloc("gn2", [128, D], F32)
    S.dma('sp', c8.t[:], k.c_in, (), [c8])
    S.dma('sp', brow.t[:], k.b_ada, (), [brow])
    bcast_load(k, gn1, k.norm1_g)
    bcast_load(k, gn2, k.norm2_g)
    S.act(c8.t[:], c8.t[:], AF.Silu, [c8], [c8])
    S.tr(k.pb[0].t[:, 0:8], c8.t[:], k.ident_f.t[0:8, 0:8], [c8, k.ident_f], [k.pb[0]])
    S.cp('dve', sc.t[:], k.pb[0].t[:, 0:8], [k.pb[0]], [sc])
    for kk in range(8):
        S.ts('dve', rep.t[:, kk, :], k.ones_f.t[:], sc.t[:, kk:kk + 1], None, ALU.mult, None, [k.ones_f, sc], [rep])
    for j in range(12):
        st = wst[j % 2]
        S.dma('sp', st.t[:], k.w_ada[:, j * 512:(j + 1) * 512].rearrange("(c p) n -> p c n", p=128), (), [st])
        P = k.pb[1 + (j % 2)]
        for kk in range(8):
            S.mm(P.t[:], rep.t[:, kk, :], st.t[:, kk, :], kk == 0, False, [rep, st], [P])
        S.mm(P.t[:], k.ones_f.t[0:1, :], brow.t[0:1, j * 512:(j + 1) * 512], False, True, [k.ones_f, brow], [P])
        S.cp('act', modb.t[:, j // 2, (j % 2) * 512:(j % 2 + 1) * 512], P.t[:], [P], [modb])
    S.stt('dve', k.A1.t[:], modb.t[:, 1, :], 1.0, gn1.t[:], ALU.add, ALU.mult, [modb, gn1], [k.A1])
    S.cp('pool', k.B1.t[:], modb.t[:, 0, :], [modb], [k.B1])
    S.cp('pool', k.G1b.t[:], modb.t[:, 2, :], [modb], [k.G1b])
    S.stt('dve', k.A2.t[:], modb.t[:, 4, :], 1.0, gn2.t[:], ALU.add, ALU.mult, [modb, gn2], [k.A2])
    S.cp('pool', k.B2.t[:], modb.t[:, 3, :], [modb], [k.B2])
    S.cp('pool', k.G2b.t[:], modb.t[:, 5, :], [modb], [k.G2b])
    k.dbg("A1", k.A1, k.A1.t[:], [128, D], F32)
    k.dbg("B1", k.B1, k.B1.t[:], [128, D], F32)
    k.dbg("sc", sc, sc.t[:], [128, 8], F32)
    k.dbg("modb", modb, modb.t[:].rearrange("p a b -> p (a b)"), [128, 6 * D], F32)
    S.barrier()
    A.release(m0)


def alloc_hT_tmps(k):
    A = k.A
    k.xt = [A.alloc(f"xt{i}", [128, D], F32) for i in range(2)]
    k.junk = A.alloc("junk", [128, D], BF16)
    k.ss = A.alloc("ss", [128, 1], F32)
    k.sst = A.alloc("sst", [128, 1], F32)
    k.rstd = A.alloc("rstd", [128, 1], F32)
    k.htmp = A.alloc("htmp", [128, D], F32)
    k.hb = [A.alloc(f"hb{i}", [128, D], BF16) for i in range(2)]
    k.hTb = [A.alloc(f"hTb{i}", [128, 8, 512], BF16) for i in range(2)]
    k.xi = 0


def emit_hT_block(k, tb, PTB):
    S = k.S
    hT = k.hTb[tb % 2]
    for j in range(4):
        t = tb * 4 + j
        xt = k.xt[k.xi % 2]
        hb = k.hb[k.xi % 2]
        k.xi += 1
        S.dma('sp', xt.t[:], k.x[t * 128:(t + 1) * 128, :], (), [xt])
        S.act(k.junk.t[:], xt.t[:], AF.Square, [xt], [k.junk, k.ss], accum_out=k.ss.t[:])
        rstd_from_ss(k, k.ss, k.sst, k.rstd, D)
        S.stt('dve', k.htmp.t[:], xt.t[:], k.rstd.t[:, 0:1], k.A1.t[:], ALU.mult, ALU.mult, [xt, k.rstd, k.A1], [k.htmp])
        S.tt('pool', hb.t[:], k.htmp.t[:], k.B1.t[:], ALU.add, [k.htmp, k.B1], [hb])
        P = k.pb[PTB]
        pv = k.pbf(PTB).rearrange("p (a b) -> p a b", a=8)
        for kk in range(8):
            S.tr(pv[:, kk, :], hb.t[:, kk * 128:(kk + 1) * 128], k.ident_bf.t[:], [hb, k.ident_bf], [P])
        S.cp('act', hT.t[:, :, j * 128:(j + 1) * 128], pv, [P], [hT])
    return hT


def phase_p1(k):
    S, A = k.S, k.A
    NT, NQB, S_TOK = k.NT, k.NQB, k.S_TOK
    k.m_v1 = A.mark()
    k.V1 = A.alloc("V1", [128, NT, 8, 65], BF16)
    S.ms('pool', k.V1.t[:], 1.0, [k.V1])
    m0 = A.mark()
    k.stg_i = 0
    stg = [A.alloc(f"stg{i}", [128, 8, 512], F32) for i in range(1)]
    Wq = A.alloc("Wq", [128, 8, 512], BF16)
    Wqp = A.alloc("Wqp", [128, 8, 512], BF16)
    Wk = A.alloc("Wk", [128, 8, 512], BF16)
    Wkp = A.alloc("Wkp", [128, 8, 512], BF16)
    Wv = A.alloc("Wv", [128, 8, 512], BF16)
    load_w_bf16(k, Wq.t[:], Wq, k.w_in[:, 0:512], 512, stg)
    load_w_bf16(k, Wqp.t[:], Wqp, k.w_in[:, 0:512], 512, stg, perm=True)
    load_w_bf16(k, Wk.t[:], Wk, k.w_in[:, 512:1024], 512, stg)
    load_w_bf16(k, Wkp.t[:], Wkp, k.w_in[:, 512:1024], 512, stg, perm=True)
    load_w_bf16(k, Wv.t[:], Wv, k.w_in[:, 1024:1536], 512, stg)
    alloc_hT_tmps(k)
    posi = A.alloc("posi", [128, 512], I32)
    ang = A.alloc("ang", [128, 512], F32)
    a2 = A.alloc("a2", [128, 512], F32)
    kq = A.alloc("kq", [128, 512], F32)
    cosb = A.alloc("cosb", [128, 512], F32)
    sinb = A.alloc("sinb", [128, 512], F32)
    t1 = [A.alloc(f"t1_{i}", [128, 512], F32) for i in range(2)]
    t2 = [A.alloc(f"t2_{i}", [128, 512], F32) for i in range(2)]
    qo = [A.alloc(f"qo{i}", [128, 512], BF16) for i in range(3)]
    invf = k.cst_sb.t[:, 0:1]
    sgn = k.cst_sb.t[:, 1:2]
    oi = 0
    for tb in range(NQB):
        blk = slice(tb * 512, (tb + 1) * 512)
        hT = emit_hT_block(k, tb, 0)
        S.dma('sp', posi.t[:], k.pos_in[0:1, blk].partition_broadcast(128), (), [posi])
        S.cp('dve', ang.t[:], posi.t[:], [posi], [ang])
        S.ts('dve', ang.t[:], ang.t[:], invf, None, ALU.mult, None, [ang, k.cst_sb], [ang])
        for shift, tab, scl in ((0.0, sinb, sgn), (np.pi / 2, cosb, None)):
            S.ts('dve', a2.t[:], ang.t[:], float(shift), None, ALU.add, None, [ang], [a2])
            S.ts('dve', kq.t[:], a2.t[:], 1.0 / TWO_PI, MAGIC, ALU.mult, ALU.add, [a2], [kq])
            S.ts('dve', kq.t[:], kq.t[:], -MAGIC, None, ALU.add, None, [kq], [kq])
            S.stt('dve', a2.t[:], kq.t[:], -C1, a2.t[:], ALU.mult, ALU.add, [kq, a2], [a2])
            S.stt('dve', a2.t[:], kq.t[:], -C2, a2.t[:], ALU.mult, ALU.add, [kq, a2], [a2])
            S.ts('dve', a2.t[:], a2.t[:], float(np.pi), float(-np.pi), ALU.min, ALU.max, [a2], [a2])
            if scl is None:
                S.act(tab.t[:], a2.t[:], AF.Sin, [a2], [tab])
            else:
                S.act(tab.t[:], a2.t[:], AF.Sin, [a2, k.cst_sb], [tab], scale=scl)
        for (W, Wp, dst) in ((Wq, Wqp, k.qT_d), (Wk, Wkp, k.kT_d)):
            for c in range(4):
                P0 = k.pb[1 + 2 * (oi % 2)]
                P1 = k.pb[2 + 2 * (oi % 2)]
                for kk in range(8):
                    S.mm(P0.t[:], W.t[:, kk, c * 128:(c + 1) * 128], hT.t[:, kk, :], kk == 0, kk == 7, [W, hT], [P0])
                for kk in range(8):
                    S.mm(P1.t[:], Wp.t[:, kk, c * 128:(c + 1) * 128], hT.t[:, kk, :], kk == 0, kk == 7, [Wp, hT], [P1])
                ta, tb_ = t1[oi % 2], t2[oi % 2]
                qb_ = qo[oi % 3]
                S.tt('dve', ta.t[:], P0.t[:], cosb.t[:], ALU.mult, [P0, cosb], [ta])
                S.tt('dve', tb_.t[:], P1.t[:], sinb.t[:], ALU.mult, [P1, sinb], [tb_])
                S.tt('pool', qb_.t[:], ta.t[:], tb_.t[:], ALU.add, [ta, tb_], [qb_])
                S.store(dst[c * 128:(c + 1) * 128, blk], qb_.t[:], [qb_])
                oi += 1
        if tb == 0:
            k.dbg("hT", hT, hT.t[:].rearrange("p a b -> p (a b)"), [128, 8 * 512], BF16)
            k.dbg("sinb", sinb, sinb.t[:], [128, 512], F32)
            k.dbg("cosb", cosb, cosb.t[:], [128, 512], F32)
            k.dbg("ang", ang, ang.t[:], [128, 512], F32)
        for j in range(4):
            t = tb * 4 + j
            P = k.pb[5 + (j % 2)]
            for kk in range(8):
                S.mm(P.t[:], hT.t[:, kk, j * 128:(j + 1) * 128], Wv.t[:, kk, :], kk == 0, kk == 7, [hT, Wv], [P])
            S.cp('act', k.V1.t[:, t, :, 0:64], P.t[:].rearrange("p (h d) -> p h d", h=8), [P], [k.V1])
    S.barrier()
    A.release(m0)


def make_consts():
    cst = np.zeros((128, 8), np.float32)
    p = np.arange(128)
    j = (p % 32).astype(np.float32)
    cst[:, 0] = (np.float32(10000.0) ** (-j / np.float32(32.0))).astype(np.float32)
    cst[:, 1] = np.where((p % 64) < 32, -1.0, 1.0)
    return cst


def make_in_map(inputs, b):
    m = {
        "x": np.ascontiguousarray(inputs["x"][b]),
        "c": np.ascontiguousarray(inputs["c"][b].reshape(8, 128)),
        "positions": np.ascontiguousarray(inputs["positions"][b].reshape(1, -1)).astype(np.int32),
        "w_ada": np.ascontiguousarray(inputs["w_ada"][0]),
        "b_ada": np.ascontiguousarray(inputs["b_ada"][0].reshape(1, -1)),
        "norm1_g": np.ascontiguousarray(inputs["norm1_g"][0].reshape(1, -1)),
        "w_in": np.ascontiguousarray(inputs["w_in"][0]),
        "b_gate": np.ascontiguousarray(inputs["b_gate"][0].reshape(8, 1)),
        "conv_w": np.ascontiguousarray(inputs["conv_w"][0]),
        "conv_b": np.ascontiguousarray(inputs["conv_b"][0].reshape(1, -1)),
        "attn_out_g": np.ascontiguousarray(inputs["attn_out_g"][0].reshape(1, -1)),
        "mlstm_out_g": np.ascontiguousarray(inputs["mlstm_out_g"][0].reshape(1, -1)),
        "w_out": np.ascontiguousarray(inputs["w_out"][0]),
        "norm2_g": np.ascontiguousarray(inputs["norm2_g"][0].reshape(1, -1)),
        "w_rg": np.ascontiguousarray(inputs["w_rg"][0]),
        "b_rg": np.ascontiguousarray(inputs["b_rg"][0].reshape(1, -1)),
        "w_re": np.ascontiguousarray(inputs["w_re"][0]),
        "b_re": np.ascontiguousarray(inputs["b_re"][0].reshape(1, -1)),
        "w1": np.ascontiguousarray(inputs["w1"][0]),
        "w3": np.ascontiguousarray(inputs["w3"][0]),
        "w2": np.ascontiguousarray(inputs["w2"][0]),
        "norm_f_g": np.ascontiguousarray(inputs["norm_f_g"].reshape(1, -1)),
        "cst": make_consts(),
    }
    return m


def phase_attn(k):
    S, A = k.S, k.A
    NT, NQB, NBK, S_TOK = k.NT, k.NQB, k.NBK, k.S_TOK
    qT = A.alloc("qT", [128, 4, S_TOK], BF16)
    kT = A.alloc("kT", [128, 4, S_TOK], BF16)
    for c in range(4):
        S.dma('sp', qT.t[:, c, :], k.qT_d[c * 128:(c + 1) * 128, :], (), [qT])
        S.dma('sp', kT.t[:, c, :], k.kT_d[c * 128:(c + 1) * 128, :], (), [kT])
    ksum = A.alloc("ksum", [128, 4, 16], F32)
    kmT = A.alloc("kmT", [128, 4, 16], BF16)
    S.ms('dve', ksum.t[:], 0.0, [ksum])
    for c in range(4):
        S.op('dve', lambda e, c=c: e.tensor_reduce(out=ksum.t[:, c, 0:NBK], in_=kT.t[:, c, :].rearrange("p (n s) -> p n s", s=256),
                                                   axis=AX.X, op=ALU.add), [kT], [ksum])
    S.ts('dve', kmT.t[:], ksum.t[:], 1.0 / 256.0, None, ALU.mult, None, [ksum], [kmT])
    LV = int(os.environ.get("ATT_LEVEL", "9"))
    if LV <= 1:
        S.barrier(); A.release(k.m_v1); return
    zer = A.alloc("zer", [128, 16, 16], F32)
    pastb = A.alloc("pastb", [128, 16, 16], F32)
    pastm = A.alloc("pastm", [128, 16, 16], F32)
    ownm = A.alloc("ownm", [128, 16, 16], F32)
    onesm = A.alloc("onesm", [128, 16, 16], F32)
    S.ms('pool', zer.t[:], 0.0, [zer])
    S.ms('pool', onesm.t[:], 1.0, [onesm])
    pat = [[1, 16], [-1, 16]]
    S.op('pool', lambda e: e.affine_select(out=pastb.t[:], in_=zer.t[:], pattern=pat, compare_op=ALU.is_gt,
                                           fill=NEG, base=0, channel_multiplier=0), [zer], [pastb])
    S.op('pool', lambda e: e.affine_select(out=pastm.t[:], in_=onesm.t[:], pattern=pat, compare_op=ALU.is_gt,
                                           fill=0.0, base=0, channel_multiplier=0), [onesm], [pastm])
    S.op('pool', lambda e: e.affine_select(out=ownm.t[:], in_=onesm.t[:], pattern=pat, compare_op=ALU.is_equal,
                                           fill=0.0, base=0, channel_multiplier=0), [onesm], [ownm])
    if LV <= 2:
        S.barrier(); A.release(k.m_v1); return
    selfull = A.alloc("selfull", [128, NT, 8, 16], F32)
    gm = A.alloc("gm", [128, 8, 16], F32)
    top8 = A.alloc("top8", [128, 8, 8], F32)
    selt = A.alloc("selt", [128, 8, 16], F32)
    PG = [k.pb[7], k.pb[6]]
    pg = [PG[hp].t[:, 0:64].rearrange("p (c n) -> p c n", c=4) for hp in range(2)]
    gmv = gm.t[:].rearrange("p (c two) n -> p c two n", two=2)
    m8 = A.alloc("m8", [128, 8], F32)
    g2 = A.alloc("g2", [128, 8, 16], F32)
    for t in range(NT):
        jb = t // 2
        for h in range(8):
            c, hp = h // 2, h % 2
            rows = slice(hp * 64, hp * 64 + 64)
            S.mm(pg[hp][:, c, :], qT.t[rows, c, t * 128:(t + 1) * 128], kmT.t[rows, c, :], True, True, [qT, kmT], [PG[hp]])
        for hp in range(2):
            S.tt('dve', gmv[:, :, hp, :], pg[hp], pastb.t[:, jb:jb + 1, :].broadcast_to([128, 4, 16]), ALU.add,
                 [PG[hp], pastb], [gm])
        src = gm
        for rnd in range(2):
            S.op('dve', lambda e, src=src: e.tensor_reduce(out=m8.t[:], in_=src.t[:], axis=AX.X, op=ALU.max), [src], [m8])
            S.tt('dve', selt.t[:], src.t[:], m8.t[:].unsqueeze(2).broadcast_to([128, 8, 16]), ALU.is_ge, [src, m8], [selt])
            S.stt('dve', g2.t[:], selt.t[:], NEG, src.t[:], ALU.mult, ALU.add, [selt, src], [g2])
            src = g2
        S.op('dve', lambda e: e.tensor_reduce(out=m8.t[:], in_=g2.t[:], axis=AX.X, op=ALU.max), [g2], [m8])
        S.tt('dve', selt.t[:], gm.t[:], m8.t[:].unsqueeze(2).broadcast_to([128, 8, 16]), ALU.is_ge, [gm, m8], [selt])
        S.tt('dve', selt.t[:], selt.t[:], pastm.t[:, jb:jb + 1, :].broadcast_to([128, 8, 16]), ALU.mult, [selt, pastm], [selt])
        S.tt('dve', selfull.t[:, t, :, :], selt.t[:], ownm.t[:, jb:jb + 1, :].broadcast_to([128, 8, 16]), ALU.add,
             [selt, ownm], [selfull])
    if LV <= 3:
        S.barrier(); A.release(k.m_v1); return
    PT = [A.alloc(f"PT{i}", [128, 512], BF16) for i in range(4)]
    acc = [A.alloc(f"acc{i}", [128, 4, 65], F32) for i in range(2)]
    rden = A.alloc("rden", [128, 4], F32)
    o_n = A.alloc("o_n", [128, 4, 64], F32)
    osq = A.alloc("osq", [128, 4, 64], F32)
    ss4 = A.alloc("ss4", [128, 4], F32)
    ss4t = A.alloc("ss4t", [128, 4], F32)
    rs4 = A.alloc("rs4", [128, 4], F32)
    gattn = A.alloc("gattn", [128, 512], F32)
    bcast_load(k, gattn, k.attn_g)
    attn_sb = A.alloc("attn_sb", [128, NT, 512], BF16)
    it = 0
    pti = 0
    atmp = [A.alloc(f"atmp{i}", [128, 4, 65], F32) for i in range(2)]
    pon = 0
    for h in range(8):
        c, hp = h // 2, h % 2
        rows = slice(hp * 64, hp * 64 + 64)
        for qb in range(NQB):
            ac = acc[it % 2]
            it += 1
            S.ms('pool', ac.t[:], 0.0, [ac])
            for n in range(2 * qb + 2):
                PO = k.pb[6 + (pon % 2)]
                pon += 1
                po = PO.t[:, 0:260].rearrange("p (j d) -> p j d", j=4)
                pts = {}
                for kc in (2 * n, 2 * n + 1):
                    jmin = max(0, kc - 4 * qb)
                    if jmin > 3:
                        continue
                    PS = k.pb[3 * hp + (pti % 3)]
                    pt = PT[pti % 3]
                    pti += 1
                    cols = slice(jmin * 128, 512)
                    S.mm(PS.t[:, cols], kT.t[rows, c, kc * 128:(kc + 1) * 128],
                         qT.t[rows, c, qb * 512 + jmin * 128:(qb + 1) * 512], True, True, [kT, qT], [PS])
                    S.act(pt.t[:, cols], PS.t[:, cols], AF.Exp, [PS], [pt], scale=0.125)
                    jd = kc - 4 * qb
                    if 0 <= jd <= 3:
                        S.tt('pool', pt.t[:, jd * 128:(jd + 1) * 128], pt.t[:, jd * 128:(jd + 1) * 128], k.tri_bf.t[:],
                             ALU.mult, [pt, k.tri_bf], [pt])
                    pts[kc] = pt
                full = (n <= 2 * qb)
                for j in range(4):
                    qt = 4 * qb + j
                    if n > qt // 2:
                        continue
                    chunks = [kc for kc in (2 * n, 2 * n + 1) if kc <= qt]
                    for idx, kc in enumerate(chunks):
                        S.mm(po[:, j, :], pts[kc].t[:, j * 128:(j + 1) * 128], k.V1.t[:, kc, h, :],
                             idx == 0, idx == len(chunks) - 1, [pts[kc], k.V1], [PO])
                    if not full:
                        S.stt('dve', ac.t[:, j, :], po[:, j, :], selfull.t[:, qt, h, n:n + 1], ac.t[:, j, :],
                              ALU.mult, ALU.add, [PO, selfull, ac], [ac])
                if full:
                    tm = atmp[pon % 2]
                    S.tt('dve', tm.t[:], po, selfull.t[:, 4 * qb:4 * qb + 4, h, n:n + 1].broadcast_to([128, 4, 65]),
                         ALU.mult, [PO, selfull], [tm])
                    S.tt('pool', ac.t[:], ac.t[:], tm.t[:], ALU.add, [ac, tm], [ac])
            S.op('dve', lambda e, ac=ac: e.reciprocal(rden.t[:], ac.t[:, :, 64]), [ac], [rden])
            S.tt('dve', o_n.t[:], ac.t[:, :, 0:64], rden.t[:].unsqueeze(2).broadcast_to([128, 4, 64]), ALU.mult, [ac, rden], [o_n])
            S.tt('pool', osq.t[:], o_n.t[:], o_n.t[:], ALU.mult, [o_n], [osq])
            S.op('dve', lambda e: e.tensor_reduce(out=ss4.t[:], in_=osq.t[:], axis=AX.X, op=ALU.add), [osq], [ss4])
            rstd_from_ss(k, ss4, ss4t, rs4, 64)
            S.tt('dve', o_n.t[:], o_n.t[:], rs4.t[:].unsqueeze(2).broadcast_to([128, 4, 64]), ALU.mult, [o_n, rs4], [o_n])
            S.tt('dve', attn_sb.t[:, qb * 4:(qb + 1) * 4, h * 64:(h + 1) * 64], o_n.t[:],
                 gattn.t[:, h * 64:(h + 1) * 64].unsqueeze(1).broadcast_to([128, 4, 64]), ALU.mult, [o_n, gattn], [attn_sb])
    S.dma('sp', k.cat_d[:, 0:512].rearrange("(t p) d -> p t d", p=128), attn_sb.t[:], [attn_sb], ())
    S.barrier()
    A.release(k.m_v1)


def phase_p2(k):
    S, A = k.S, k.A
    NT, NQB = k.NT, k.NQB
    m0 = A.mark()
    k.stg_i = 0
    stg = [A.alloc(f"stg{i}", [128, 8, 512], F32) for i in range(2)]
    Wqkm = A.alloc("Wqkm", [128, 8, 1024], BF16)
    Wvm = A.alloc("Wvm", [128, 8, 512], BF16)
    Wom = A.alloc("Wom", [128, 8, 512], BF16)
    Wg = A.alloc("Wg", [128, 8, 8], BF16)
    load_w_bf16(k, Wqkm.t[:, :, 0:512], Wqkm, k.w_in[:, 1536:2048], 512, stg)
    load_w_bf16(k, Wqkm.t[:, :, 512:1024], Wqkm, k.w_in[:, 2048:2560], 512, stg)
    load_w_bf16(k, Wvm.t[:], Wvm, k.w_in[:, 2560:3072], 512, stg)
    load_w_bf16(k, Wom.t[:], Wom, k.w_in[:, 3072:3584], 512, stg)
    load_w_bf16(k, Wg.t[:], Wg, k.w_in[:, 3584:3592], 8, stg)
    alloc_hT_tmps(k)
    qo = [A.alloc(f"qo{i}", [128, 512], BF16) for i in range(3)]
    gsb = [A.alloc(f"gsb{i}", [4, 512], F32) for i in range(2)]
    oi = 0
    for tb in range(NQB):
        blk = slice(tb * 512, (tb + 1) * 512)
        hT = emit_hT_block(k, tb, 0)
        for c in range(8):
            P = k.pb[1 + (oi % 2)]
            for kk in range(8):
                S.mm(P.t[:], Wqkm.t[:, kk, c * 128:(c + 1) * 128], hT.t[:, kk, :], kk == 0, kk == 7, [Wqkm, hT], [P])
            q_ = qo[oi % 3]
            S.cp('act' if oi % 2 else 'dve', q_.t[:], P.t[:], [P], [q_])
            S.store(k.qkm_d[c * 128:(c + 1) * 128, blk], q_.t[:], [q_])
            oi += 1
        for gi, dst in ((0, k.ig_d), (1, k.fg_d)):
            P = k.pb[3 + gi]
            for kk in range(8):
                S.mm(P.t[0:4, :], Wg.t[:, kk, gi * 4:(gi + 1) * 4], hT.t[:, kk, :], kk == 0, kk == 7, [Wg, hT], [P])
            S.cp('dve', gsb[gi].t[:], P.t[0:4, :], [P], [gsb[gi]])
            S.store(dst[:, blk], gsb[gi].t[:], [gsb[gi]])
        for j in range(4):
            t = tb * 4 + j
            for wi, (W, dst) in enumerate(((Wvm, k.vm_d), (Wom, k.om_d))):
                P = k.pb[5 + wi]
                for kk in range(8):
                    S.mm(P.t[:], hT.t[:, kk, j * 128:(j + 1) * 128], W.t[:, kk, :], kk == 0, kk == 7, [hT, W], [P])
                q_ = qo[oi % 3]
                S.cp('act' if wi else 'dve', q_.t[:], P.t[:], [P], [q_])
                S.store(dst[t * 128:(t + 1) * 128, :], q_.t[:], [q_])
                oi += 1
    S.barrier()
    A.release(m0)


def phase_mlstm(k):
    S, A = k.S, k.A
    NT, S_TOK, NP = k.NT, k.S_TOK, k.NP
    m0 = A.mark()
    KSC = float(128.0 ** -0.5)
    cw5 = A.alloc("cw5", [5, D], F32)
    cwT = A.alloc("cwT", [128, 8, 5], F32)
    S.dma('sp', cw5.t[0:4, :], k.conv_w, (), [cw5])
    S.dma('sp', cw5.t[4:5, :], k.conv_b, (), [cw5])
    for fc in range(8):
        P = k.pb[fc % 2]
        S.tr(P.t[:, 0:5], cw5.t[:, fc * 128:(fc + 1) * 128], k.ident_f.t[0:5, 0:5], [cw5, k.ident_f], [P])
        S.cp('dve', cwT.t[:, fc, :], P.t[:, 0:5], [P], [cwT])
    qmT = A.alloc("qmT", [128, 4, S_TOK], BF16)
    kmT = A.alloc("kmT2", [128, 4, S_TOK], BF16)
    mc = A.mark()
    raw = [A.alloc(f"raw{i}", [128, 3 + S_TOK], BF16) for i in range(2)]
    cacc = A.alloc("cacc", [128, S_TOK], F32)
    for fc in range(8):
        rw = raw[fc % 2]
        S.ms('pool', rw.t[:, 0:3], 0.0, [rw])
        S.dma('sp', rw.t[:, 3:3 + S_TOK], k.qkm_d[fc * 128:(fc + 1) * 128, :], (), [rw])
        S.ts('dve', cacc.t[:], rw.t[:, 0:S_TOK], cwT.t[:, fc, 0:1], cwT.t[:, fc, 4:5], ALU.mult, ALU.add, [rw, cwT], [cacc])
        for j in range(1, 4):
            S.stt('dve', cacc.t[:], rw.t[:, j:j + S_TOK], cwT.t[:, fc, j:j + 1], cacc.t[:], ALU.mult, ALU.add,
                  [rw, cwT, cacc], [cacc])
        dst = qmT if fc < 4 else kmT
        S.act(dst.t[:, fc % 4, :], cacc.t[:], AF.Silu, [cacc], [dst])
    S.barrier()
    A.release(mc)
    def g_(name, shape=None):
        return A.alloc(name, shape or [NP, 128], F32)
    ig, fg, cs, u, cmu, r, tq, wint, enm, wz, eu, er = [g_(n) for n in
        ("ig", "fg", "cs", "u", "cmu", "r", "tq", "wint", "enm", "wz", "eu", "er")]
    bcol = g_("bcol", [NP, 2])
    nbf, acol, mloc, mcol, mpcol, amm = [g_(n, [NP, 1]) for n in ("nbf", "acol", "mloc", "mcol", "mpcol", "amm")]
    arow, mlrow, mrow, mprow, sprow = [g_(n, [1, NP]) for n in ("arow", "mlrow", "mrow", "mprow", "sprow")]
    sprev_b = A.alloc("sprev_b", [128, NP], F32)
    tmq = A.alloc("tmq", [128, 5, NP], F32)
    onesn = k.ones_f.t[0:NP, :]
    idn = k.ident_f.t[0:NP, 0:NP]
    S.dma('sp', ig.t[:], k.ig_d.rearrange("h (c l) -> (h c) l", l=128), (), [ig])
    S.dma('sp', fg.t[:], k.fg_d.rearrange("h (c l) -> (h c) l", l=128), (), [fg])
    for h in range(4):
        S.dma('sp', bcol.t[h * NT:(h + 1) * NT, 0:1], k.b_gate[h:h + 1, :].partition_broadcast(NT), (), [bcol])
        S.dma('sp', bcol.t[h * NT:(h + 1) * NT, 1:2], k.b_gate[4 + h:5 + h, :].partition_broadcast(NT), (), [bcol])
    S.ts('dve', nbf.t[:], bcol.t[:, 1:2], -1.0, None, ALU.mult, None, [bcol], [nbf])
    S.act(fg.t[:], fg.t[:], AF.Exp, [fg, nbf], [fg], bias=nbf.t[:, 0:1], scale=-1.0)
    S.act(fg.t[:], fg.t[:], AF.Ln, [fg], [fg], bias=1.0)
    S.op('dve', lambda e: e.tensor_tensor_scan(cs.t[:], onesn, fg.t[:], 0.0, ALU.mult, ALU.add), [fg, k.ones_f], [cs])
    S.ts('dve', ig.t[:], ig.t[:], bcol.t[:, 0:1], None, ALU.add, None, [ig, bcol], [ig])
    S.tt('dve', u.t[:], ig.t[:], cs.t[:], ALU.add, [ig, cs], [u])
    S.op('dve', lambda e: e.tensor_tensor_scan(cmu.t[:], onesn, u.t[:], NEG, ALU.mult, ALU.max), [u, k.ones_f], [cmu])
    S.ts('dve', acol.t[:], cs.t[:, 127:128], -1.0, None, ALU.mult, None, [cs], [acol])
    S.tt('dve', mloc.t[:], acol.t[:], cmu.t[:, 127:128], ALU.add, [acol, cmu], [mloc])
    P = k.pb[0]
    S.tr(P.t[0:1, 0:NP], acol.t[:], idn, [acol, k.ident_f], [P])
    S.cp('dve', arow.t[:], P.t[0:1, 0:NP], [P], [arow])
    P = k.pb[1]
    S.tr(P.t[0:1, 0:NP], mloc.t[:], idn, [mloc, k.ident_f], [P])
    S.cp('dve', mlrow.t[:], P.t[0:1, 0:NP], [P], [mlrow])
    S.ms('dve', mprow.t[:], 0.0, [mprow])
    for h in range(4):
        sl = slice(h * NT, (h + 1) * NT)
        S.op('dve', lambda e, sl=sl: e.tensor_tensor_scan(mrow.t[:, sl], arow.t[:, sl], mlrow.t[:, sl], 0.0, ALU.add, ALU.max),
             [arow, mlrow], [mrow])
        if NT > 1:
            S.cp('dve', mprow.t[:, h * NT + 1:(h + 1) * NT], mrow.t[:, h * NT:(h + 1) * NT - 1], [mrow], [mprow])
    S.tt('dve', sprow.t[:], arow.t[:], mprow.t[:], ALU.add, [arow, mprow], [sprow])
    S.tt('dve', sprow.t[:], sprow.t[:], mrow.t[:], ALU.subtract, [sprow, mrow], [sprow])
    S.act(sprow.t[:], sprow.t[:], AF.Exp, [sprow], [sprow])
    P = k.pb[2]
    S.mm(P.t[:, 0:NP], k.ones_f.t[0:1, :], sprow.t[:], True, True, [k.ones_f, sprow], [P])
    S.cp('dve', sprev_b.t[:], P.t[:, 0:NP], [P], [sprev_b])
    P = k.pb[3]
    S.mm(P.t[0:NP, 0:1], mrow.t[:], k.ones_f.t[0:1, 0:1], True, True, [mrow, k.ones_f], [P])
    S.cp('dve', mcol.t[:], P.t[0:NP, 0:1], [P], [mcol])
    P = k.pb[4]
    S.mm(P.t[0:NP, 0:1], mprow.t[:], k.ones_f.t[0:1, 0:1], True, True, [mprow, k.ones_f], [P])
    S.cp('dve', mpcol.t[:], P.t[0:NP, 0:1], [P], [mpcol])
    S.ts('dve', r.t[:], cmu.t[:], mpcol.t[:, 0:1], -1.0, ALU.max, ALU.mult, [cmu, mpcol], [r])
    S.act(wint.t[:], r.t[:], AF.Exp, [r, mpcol], [wint], bias=mpcol.t[:, 0:1])
    S.tt('dve', tq.t[:], r.t[:], cs.t[:], ALU.add, [r, cs], [tq])
    S.act(enm.t[:], tq.t[:], AF.Exp, [tq], [enm])
    S.tt('dve', amm.t[:], acol.t[:], mcol.t[:], ALU.subtract, [acol, mcol], [amm])
    S.ts('dve', amm.t[:], amm.t[:], float(np.log(KSC)), None, ALU.add, None, [amm], [amm])
    S.act(wz.t[:], u.t[:], AF.Exp, [u, amm], [wz], bias=amm.t[:, 0:1])
    S.act(eu.t[:], u.t[:], AF.Exp, [u], [eu])
    S.act(er.t[:], r.t[:], AF.Exp, [r], [er])
    for qi, Q in enumerate((eu, er, wint, enm, wz)):
        P = k.pb[5 + (qi % 2)]
        S.tr(P.t[:, 0:NP], Q.t[:], idn, [Q, k.ident_f], [P])
        S.cp('dve', tmq.t[:, qi, :], P.t[:, 0:NP], [P], [tmq])
    tri_s = A.alloc("tri_s", [128, 128], F32)
    S.ts('dve', tri_s.t[:], k.tri_bf.t[:], KSC, None, ALU.mult, None, [k.tri_bf], [tri_s])
    vaug = A.alloc("vaug", [128, NT, 4, 129], BF16)
    S.ms('pool', vaug.t[:], 1.0, [vaug])
    for h in range(4):
        S.dma('sp', vaug.t[:, :, h, 0:128], k.vm_d[:, h * 128:(h + 1) * 128].rearrange("(c p) d -> p c d", p=128), (), [vaug])
    CT = [A.alloc(f"CT{h}", [128, 129], F32) for h in range(4)]
    CTb = [A.alloc(f"CTb{h}", [128, 129], BF16) for h in range(4)]
    for h in range(4):
        S.ms('pool', CT[h].t[:], 0.0, [CT[h]])
        S.ms('pool', CTb[h].t[:], 0.0, [CTb[h]])
    gml = A.alloc("gml", [128, 512], F32)
    bcast_load(k, gml, k.mlstm_g)
    Am = [A.alloc(f"Am{i}", [128, 128], BF16) for i in range(2)]
    vu = [A.alloc(f"vu{i}", [128, 129], BF16) for i in range(2)]
    wv = [A.alloc(f"wv{i}", [128, 129], BF16) for i in range(2)]
    ktm = [A.alloc(f"ktm{i}", [128, 128], BF16) for i in range(2)]
    inter = [A.alloc(f"inter{i}", [128, 129], F32) for i in range(2)]
    tot = [A.alloc(f"tot{i}", [128, 129], F32) for i in range(2)]
    den = A.alloc("den", [128, 1], F32)
    rdn = A.alloc("rdn", [128, 1], F32)
    hmraw = [A.alloc(f"hmraw{i}", [128, 4, 128], F32) for i in range(2)]
    hsq = A.alloc("hsq", [128, 4, 128], F32)
    ss4 = A.alloc("mss4", [128, 4], F32)
    ss4t = A.alloc("mss4t", [128, 4], F32)
    rs4 = A.alloc("mrs4", [128, 4], F32)
    omt = [A.alloc(f"omt{i}", [128, 512], BF16) for i in range(2)]
    sgm = A.alloc("sgm", [128, 512], F32)
    hout = [A.alloc(f"hout{i}", [128, 512], BF16) for i in range(2)]
    it = 0
    for c in range(NT):
        ck = slice(c * 128, (c + 1) * 128)
        hr = hmraw[c % 2]
        S.dma('sp', omt[c % 2].t[:], k.om_d[ck, :], (), [omt[c % 2]])
        for h in range(4):
            hc = h * NT + c
            i2 = it % 2
            it += 1
            PA = k.pb[0 + i2]
            S.mm(PA.t[:, 0:128], kmT.t[:, h, ck], qmT.t[:, h, ck], True, True, [kmT, qmT], [PA])
            S.tt('dve', Am[i2].t[:], PA.t[:, 0:128], tri_s.t[:], ALU.mult, [PA, tri_s], [Am[i2]])
            S.ts('pool', vu[i2].t[:], vaug.t[:, c, h, :], tmq.t[:, 0, hc:hc + 1], None, ALU.mult, None, [vaug, tmq], [vu[i2]])
            PI = k.pb[2 + i2]
            S.mm(PI.t[:, 0:129], Am[i2].t[:], vu[i2].t[:], True, True, [Am[i2], vu[i2]], [PI])
            PN = k.pb[4 + i2]
            S.mm(PN.t[:, 0:129], qmT.t[:, h, ck], CTb[h].t[:], True, True, [qmT, CTb[h]], [PN])
            S.act(inter[i2].t[:], PN.t[:, 0:129], AF.Copy, [PN, tmq], [inter[i2]], scale=tmq.t[:, 2, hc:hc + 1])
            S.stt('dve', tot[i2].t[:], PI.t[:, 0:129], tmq.t[:, 1, hc:hc + 1], inter[i2].t[:], ALU.mult, ALU.add,
                  [PI, tmq, inter[i2]], [tot[i2]])
            S.ts('dve', rdn.t[:], tot[i2].t[:, 128:129], -1.0, None, ALU.mult, None, [tot[i2]], [rdn])
            S.stt('dve', den.t[:], tot[i2].t[:, 128:129], rdn.t[:, 0:1], tmq.t[:, 3, hc:hc + 1], ALU.max, ALU.max,
                  [tot[i2], rdn, tmq], [den])
            S.op('dve', lambda e: e.reciprocal(rdn.t[:], den.t[:]), [den], [rdn])
            S.act(hr.t[:, h, :], tot[i2].t[:, 0:128], AF.Copy, [tot[i2], rdn], [hr], scale=rdn.t[:, 0:1])
            if c < NT - 1:
                S.ts('pool', wv[i2].t[:], vaug.t[:, c, h, :], tmq.t[:, 4, hc:hc + 1], None, ALU.mult, None, [vaug, tmq], [wv[i2]])
                PK = k.pb[7]
                pk = k.pbf(7)[:, 0:128]
                S.tr(pk, kmT.t[:, h, ck], k.ident_bf.t[:], [kmT, k.ident_bf], [PK])
                S.cp('act', ktm[i2].t[:], pk, [PK], [ktm[i2]])
                PC = k.pb[6]
                S.mm(PC.t[:, 0:129], ktm[i2].t[:], wv[i2].t[:], True, True, [ktm[i2], wv[i2]], [PC])
                S.stt('dve', CT[h].t[:], CT[h].t[:], sprev_b.t[:, hc:hc + 1], PC.t[:, 0:129], ALU.mult, ALU.add,
                      [CT[h], sprev_b, PC], [CT[h]])
                S.cp('act', CTb[h].t[:], CT[h].t[:], [CT[h]], [CTb[h]])
        om = omt[c % 2]
        ho = hout[c % 2]
        S.tt('pool', hsq.t[:], hr.t[:], hr.t[:], ALU.mult, [hr], [hsq])
        S.op('dve', lambda e: e.tensor_reduce(out=ss4.t[:], in_=hsq.t[:], axis=AX.X, op=ALU.add), [hsq], [ss4])
        rstd_from_ss(k, ss4, ss4t, rs4, 128)
        S.tt('dve', hr.t[:], hr.t[:], rs4.t[:].unsqueeze(2).broadcast_to([128, 4, 128]), ALU.mult, [hr, rs4], [hr])
        hr2 = hr.t[:].rearrange("p h d -> p (h d)")
        S.tt('pool', hr2, hr2, gml.t[:], ALU.mult, [hr, gml], [hr])
        S.act(sgm.t[:], om.t[:], AF.Sigmoid, [om], [sgm])
        S.tt('dve', ho.t[:], hr2, sgm.t[:], ALU.mult, [hr, sgm], [ho])
        S.dma('sp', k.cat_d[ck, 512:1024], ho.t[:], [ho], ())
    S.barrier()
    A.release(m0)


def phase_out(k):
    S, A = k.S, k.A
    NT, CAP, NSLOT = k.NT, k.CAP, k.NSLOT
    m0 = A.mark()
    k.stg_i = 0
    stg = [A.alloc(f"stg{i}", [128, 8, 512], F32) for i in range(2)]
    Wout = A.alloc("Wout", [128, 8, 1024], BF16)
    load_w_bf16(k, Wout.t[:, :, 0:512], Wout, k.w_out[:, 0:512], 512, stg)
    load_w_bf16(k, Wout.t[:, :, 512:1024], Wout, k.w_out[:, 512:1024], 512, stg)
    Wr = A.alloc("Wr", [128, 8, 36], F32)
    S.dma('sp', Wr.t[:, :, 0:4], k.w_rg.rearrange("(c p) n -> p c n", p=128), (), [Wr])
    S.dma('sp', Wr.t[:, :, 4:36], k.w_re.rearrange("(c p) n -> p c n", p=128), (), [Wr])
    brow = A.alloc("brow_r", [1, 36], F32)
    S.dma('sp', brow.t[:, 0:4], k.b_rg, (), [brow])
    S.dma('sp', brow.t[:, 4:36], k.b_re, (), [brow])
    ecap = A.alloc("ecap", [128, 32], F32)
    S.op('pool', lambda e: e.iota(ecap.t[:], pattern=[[CAP, 32]], base=0, channel_multiplier=0,
                                  allow_small_or_imprecise_dtypes=True), (), [ecap])
    cntb = A.alloc("cntb", [128, 32], F32)
    S.ms('pool', cntb.t[:], 0.0, [cntb])
    catt = [A.alloc(f"catt{i}", [128, D], BF16) for i in range(2)]
    catT = [A.alloc(f"catT{i}", [128, 8, 128], BF16) for i in range(2)]
    xt = [A.alloc(f"oxt{i}", [128, D], F32) for i in range(2)]
    ytmp = A.alloc("ytmp", [128, D], F32)
    x1 = [A.alloc(f"x1_{i}", [128, D], F32) for i in range(2)]
    junk = A.alloc("ojunk", [128, D], BF16)
    ss = A.alloc("oss", [128, 1], F32)
    sst = A.alloc("osst", [128, 1], F32)
    rstd = A.alloc("orstd", [128, 1], F32)
    h2f = A.alloc("h2f", [128, D], F32)
    h2b = [A.alloc(f"h2b{i}", [128, D], BF16) for i in range(2)]
    h2T = A.alloc("h2T", [128, 8, 128], F32)
    lg = A.alloc("lg", [128, 36], F32)
    sm = {n: A.alloc("r_" + n, sh, F32) for n, sh in (
        ("gmax", [128, 1]), ("ngmax", [128, 1]), ("eg", [128, 4]), ("sumg", [128, 1]), ("gw", [128, 1]),
        ("ohg", [128, 4]), ("elm", [128, 4, 8]), ("els", [128, 8]), ("top8", [128, 8]), ("dd", [128, 1]),
        ("rd", [128, 1]), ("wk", [128, 2]), ("oh", [128, 2, 8]), ("E", [128, 2, 32]), ("mask", [128, 32]),
        ("pos", [128, 32]), ("val", [128, 32]), ("tmp32", [128, 32]), ("sk", [128, 2]), ("pk", [128, 2]),
        ("ok", [128, 2]))}
    maskb = A.alloc("maskb", [128, 32], BF16)
    BIGV = float(NSLOT + 4096)
    def load_t(t):
        rows = slice(t * 128, (t + 1) * 128)
        S.dma('sp', catt[t % 2].t[:], k.cat_d[rows, :], (), [catt[t % 2]])
        S.dma('sp', xt[t % 2].t[:], k.x[rows, :], (), [xt[t % 2]])
    load_t(0)
    for t in range(NT):
        rows = slice(t * 128, (t + 1) * 128)
        i2 = t % 2
        if t + 1 < NT:
            load_t(t + 1)
        PT_ = k.pb[0]
        pv = k.pbf(0).rearrange("p (a b) -> p a b", a=8)
        for kk in range(8):
            S.tr(pv[:, kk, :], catt[i2].t[:, kk * 128:(kk + 1) * 128], k.ident_bf.t[:], [catt[i2], k.ident_bf], [PT_])
        S.cp('act', catT[i2].t[:], pv, [PT_], [catT[i2]])
        for half in range(2):
            P = k.pb[1 + half]
            hs = slice(half * 512, (half + 1) * 512)
            for kk in range(8):
                S.mm(P.t[:], catT[i2].t[:, kk, :], Wout.t[:, kk, hs], kk == 0, kk == 7, [catT[i2], Wout], [P])
            S.tt('dve', ytmp.t[:, hs], P.t[:], k.G1b.t[:, hs], ALU.mult, [P, k.G1b], [ytmp])
        S.tt('pool', x1[i2].t[:], ytmp.t[:], xt[i2].t[:], ALU.add, [ytmp, xt[i2]], [x1[i2]])
        S.dma('sp', k.x1_d[rows, :], x1[i2].t[:], [x1[i2]], ())
        S.act(junk.t[:], x1[i2].t[:], AF.Square, [x1[i2]], [junk, ss], accum_out=ss.t[:])
        rstd_from_ss(k, ss, sst, rstd, D)
        S.stt('dve', h2f.t[:], x1[i2].t[:], rstd.t[:, 0:1], k.A2.t[:], ALU.mult, ALU.mult, [x1[i2], rstd, k.A2], [h2f])
        S.tt('pool', h2f.t[:], h2f.t[:], k.B2.t[:], ALU.add, [h2f, k.B2], [h2f])
        S.cp('act', h2b[i2].t[:], h2f.t[:], [h2f], [h2b[i2]])
        for g4 in range(2):
            P = k.pb[3 + g4]
            pv4 = P.t[:].rearrange("p (a b) -> p a b", a=4)
            for q in range(4):
                kk = g4 * 4 + q
                S.tr(pv4[:, q, :], h2f.t[:, kk * 128:(kk + 1) * 128], k.ident_f.t[:], [h2f, k.ident_f], [P])
            S.cp('act' if g4 else 'dve', h2T.t[:, g4 * 4:(g4 + 1) * 4, :], pv4, [P], [h2T])
        P = k.pb[5]
        for kk in range(8):
            S.mm(P.t[:, 0:36], h2T.t[:, kk, :], Wr.t[:, kk, :], kk == 0, False, [h2T, Wr], [P])
        S.mm(P.t[:, 0:36], k.ones_f.t[0:1, :], brow.t[:], False, True, [k.ones_f, brow], [P])
        S.cp('dve', lg.t[:], P.t[:, 0:36], [P], [lg])
        g = sm
        S.op('dve', lambda e: e.tensor_reduce(out=g["gmax"].t[:], in_=lg.t[:, 0:4], axis=AX.X, op=ALU.max), [lg], [g["gmax"]])
        S.ts('dve', g["ngmax"].t[:], g["gmax"].t[:], -1.0, None, ALU.mult, None, [g["gmax"]], [g["ngmax"]])
        S.act(g["eg"].t[:], lg.t[:, 0:4], AF.Exp, [lg, g["ngmax"]], [g["eg"], g["sumg"]], bias=g["ngmax"].t[:, 0:1],
              accum_out=g["sumg"].t[:])
        S.op('dve', lambda e: e.reciprocal(g["gw"].t[:], g["sumg"].t[:]), [g["sumg"]], [g["gw"]])
        S.ts('dve', g["ohg"].t[:], lg.t[:, 0:4], g["gmax"].t[:, 0:1], None, ALU.is_equal, None, [lg, g["gmax"]], [g["ohg"]])
        S.tt('dve', g["elm"].t[:], lg.t[:, 4:36].rearrange("p (g e) -> p g e", g=4),
             g["ohg"].t[:].unsqueeze(2).broadcast_to([128, 4, 8]), ALU.mult, [lg, g["ohg"]], [g["elm"]])
        S.op('dve', lambda e: e.tensor_reduce(out=g["els"].t[:], in_=g["elm"].t[:].rearrange("p g e -> p e g"),
                                              axis=AX.X, op=ALU.add), [g["elm"]], [g["els"]])
        S.op('dve', lambda e: e.tensor_reduce(out=g["top8"].t[:, 0:1], in_=g["els"].t[:], axis=AX.X, op=ALU.max), [g["els"]], [g["top8"]])
        S.ts('dve', g["oh"].t[:, 0, :], g["els"].t[:], g["top8"].t[:, 0:1], None, ALU.is_equal, None, [g["els"], g["top8"]], [g["oh"]])
        S.stt('dve', g["oh"].t[:, 1, :], g["oh"].t[:, 0, :], NEG, g["els"].t[:], ALU.mult, ALU.add, [g["oh"], g["els"]], [g["oh"]])
        S.op('dve', lambda e: e.tensor_reduce(out=g["top8"].t[:, 1:2], in_=g["oh"].t[:, 1, :], axis=AX.X, op=ALU.max), [g["oh"]], [g["top8"]])
        S.tt('dve', g["dd"].t[:], g["top8"].t[:, 1:2], g["top8"].t[:, 0:1], ALU.subtract, [g["top8"]], [g["dd"]])
        S.act(g["dd"].t[:], g["dd"].t[:], AF.Exp, [g["dd"]], [g["dd"]])
        S.ts('dve', g["dd"].t[:], g["dd"].t[:], 1.0, None, ALU.add, None, [g["dd"]], [g["dd"]])
        S.op('dve', lambda e: e.reciprocal(g["rd"].t[:], g["dd"].t[:]), [g["dd"]], [g["rd"]])
        S.tt('dve', g["wk"].t[:, 0:1], g["gw"].t[:], g["rd"].t[:], ALU.mult, [g["gw"], g["rd"]], [g["wk"]])
        S.tt('dve', g["wk"].t[:, 1:2], g["gw"].t[:], g["wk"].t[:, 0:1], ALU.subtract, [g["gw"], g["wk"]], [g["wk"]])
        for kk in range(2):
            S.ts('dve', g["oh"].t[:, kk, :], g["els"].t[:], g["top8"].t[:, kk:kk + 1], None, ALU.is_equal, None,
                 [g["els"], g["top8"]], [g["oh"]])
            S.tt('dve', g["E"].t[:, kk, :].rearrange("p (g e) -> p g e", g=4),
                 g["ohg"].t[:].unsqueeze(2).broadcast_to([128, 4, 8]),
                 g["oh"].t[:, kk, :].unsqueeze(1).broadcast_to([128, 4, 8]), ALU.mult, [g["ohg"], g["oh"]], [g["E"]])
        S.tt('dve', g["mask"].t[:], g["E"].t[:, 0, :], g["E"].t[:, 1, :], ALU.add, [g["E"]], [g["mask"]])
        S.cp('dve', maskb.t[:], g["mask"].t[:], [g["mask"]], [maskb])
        P = k.pb[6]
        S.mm(P.t[:, 0:32], k.stri_bf.t[:], maskb.t[:], True, True, [k.stri_bf, maskb], [P])
        S.mm(P.t[:, 32:64], k.ones_bf.t[:], maskb.t[:], True, True, [k.ones_bf, maskb], [P])
        S.tt('dve', g["pos"].t[:], P.t[:, 0:32], cntb.t[:], ALU.add, [P, cntb], [g["pos"]])
        S.tt('dve', cntb.t[:], P.t[:, 32:64], cntb.t[:], ALU.add, [P, cntb], [cntb])
        S.tt('dve', g["val"].t[:], g["pos"].t[:], ecap.t[:], ALU.add, [g["pos"], ecap], [g["val"]])
        for kk in range(2):
            S.tt('dve', g["tmp32"].t[:], g["E"].t[:, kk, :], g["val"].t[:], ALU.mult, [g["E"], g["val"]], [g["tmp32"]])
            S.op('dve', lambda e, kk=kk: e.tensor_reduce(out=g["sk"].t[:, kk:kk + 1], in_=g["tmp32"].t[:], axis=AX.X, op=ALU.add),
                 [g["tmp32"]], [g["sk"]])
            S.tt('dve', g["tmp32"].t[:], g["E"].t[:, kk, :], g["pos"].t[:], ALU.mult, [g["E"], g["pos"]], [g["tmp32"]])
            S.op('dve', lambda e, kk=kk: e.tensor_reduce(out=g["pk"].t[:, kk:kk + 1], in_=g["tmp32"].t[:], axis=AX.X, op=ALU.add),
                 [g["tmp32"]], [g["pk"]])
        S.ts('dve', g["ok"].t[:], g["pk"].t[:], float(CAP) - 0.5, None, ALU.is_lt, None, [g["pk"]], [g["ok"]])
        S.ts('dve', g["sk"].t[:], g["sk"].t[:], -BIGV, None, ALU.add, None, [g["sk"]], [g["sk"]])
        S.tt('dve', g["sk"].t[:], g["sk"].t[:], g["ok"].t[:], ALU.mult, [g["sk"], g["ok"]], [g["sk"]])
        S.ts('dve', g["sk"].t[:], g["sk"].t[:], BIGV, None, ALU.add, None, [g["sk"]], [g["sk"]])
        S.cp('dve', k.slot_i.t[:, t, :], g["sk"].t[:], [g["sk"]], [k.slot_i])
        S.tt('dve', k.wts.t[:, t, :], g["wk"].t[:], g["ok"].t[:], ALU.mult, [g["wk"], g["ok"]], [k.wts])
        for kk in range(2):
            def fn(e, t=t, kk=kk, src=h2b[i2]):
                return e.indirect_dma_start(out=k.XS_d[:, :], out_offset=bass.IndirectOffsetOnAxis(ap=k.slot_i.t[:, t, kk:kk + 1], axis=0),
                                            in_=src.t[:], in_offset=None, bounds_check=k.bcreg(e), oob_is_err=False)
            S.swdma(fn, [h2b[i2], k.slot_i, k.xs_tok], [])
    S.barrier()
    A.release(m0)


def phase_experts(k):
    S, A = k.S, k.A
    CAP, NG = k.CAP, k.NG
    m0 = A.mark()
    stg = [A.alloc(f"estg{i}", [128, 8, 512], F32) for i in range(3)]
    W1b = [A.alloc(f"W1b{i}", [128, 8, 512], BF16) for i in range(2)]
    W3b = [A.alloc(f"W3b{i}", [128, 8, 512], BF16) for i in range(2)]
    W2b = [A.alloc(f"W2b{i}", [128, 4, 1024], BF16) for i in range(2)]
    xs = [A.alloc(f"xs{i}", [128, NG // 128, D], BF16) for i in range(2)]
    xT = [A.alloc(f"xT{i}", [128, 8, NG], BF16) for i in range(2)]
    sl = [A.alloc(f"sl{i}", [128, NG], F32) for i in range(2)]
    G = [A.alloc(f"G{i}", [128, 4, NG], BF16) for i in range(2)]
    yb = [A.alloc(f"yb{i}", [128, D], BF16) for i in range(2)]
    st = {"si": 0, "yi": 0}

    def load_w(e_):
        i2 = e_ % 2
        for (src, dst, kc) in ((k.w1[e_], W1b[i2], 8), (k.w3[e_], W3b[i2], 8)):
            sg = stg[st["si"] % len(stg)]
            st["si"] += 1
            S.dma('sp', sg.t[:], src.rearrange("(c p) n -> p c n", p=128), (), [sg])
            S.cp('pool', dst.t[:], sg.t[:], [sg], [dst])
        for hh in range(2):
            sg = stg[st["si"] % len(stg)]
            st["si"] += 1
            sv = sg.t[:, 0:4, :]
            S.dma('sp', sv, k.w2[e_][:, hh * 512:(hh + 1) * 512].rearrange("(c p) n -> p c n", p=128), (), [sg])
            S.cp('pool', W2b[i2].t[:, :, hh * 512:(hh + 1) * 512], sv, [sg], [W2b[i2]])

    groups = [(e_, gq) for e_ in range(N_EXP) for gq in range(CAP // NG)]

    def load_xs(idx):
        e_, gq = groups[idx]
        r0 = e_ * CAP + gq * NG
        S.dma('sp', xs[idx % 2].t[:], k.XS_d[r0:r0 + NG, :].rearrange("(j p) d -> p j d", p=128), (), [xs[idx % 2]])

    load_w(0)
    load_xs(0)
    for idx, (e_, gq) in enumerate(groups):
        i2 = e_ % 2
        g2 = idx % 2
        r0 = e_ * CAP + gq * NG
        if gq == 0 and e_ + 1 < N_EXP:
            load_w(e_ + 1)
        if idx + 1 < len(groups):
            load_xs(idx + 1)
        for j in range(NG // 128):
            PT_ = k.pb[j % 2]
            pv = k.pbf(j % 2).rearrange("p (a b) -> p a b", a=8)
            for kk in range(8):
                S.tr(pv[:, kk, :], xs[g2].t[:, j, kk * 128:(kk + 1) * 128], k.ident_bf.t[:], [xs[g2], k.ident_bf], [PT_])
            S.cp('act' if j % 2 else 'dve', xT[g2].t[:, :, j * 128:(j + 1) * 128], pv, [PT_], [xT[g2]])
        for f in range(4):
            P1_ = k.pb[2 + (f % 2)]
            P3_ = k.pb[4 + (f % 2)]
            fs = slice(f * 128, (f + 1) * 128)
            for kk in range(8):
                S.mm(P1_.t[:, 0:NG], W1b[i2].t[:, kk, fs], xT[g2].t[:, kk, :], kk == 0, kk == 7, [W1b[i2], xT[g2]], [P1_])
            for kk in range(8):
                S.mm(P3_.t[:, 0:NG], W3b[i2].t[:, kk, fs], xT[g2].t[:, kk, :], kk == 0, kk == 7, [W3b[i2], xT[g2]], [P3_])
            S.act(sl[f % 2].t[:], P1_.t[:, 0:NG], AF.Silu, [P1_], [sl[f % 2]])
            S.tt('dve', G[g2].t[:, f, :], sl[f % 2].t[:], P3_.t[:, 0:NG], ALU.mult, [sl[f % 2], P3_], [G[g2]])
        for j in range(NG // 128):
            y_ = yb[st["yi"] % len(yb)]
            st["yi"] += 1
            for half in range(2):
                P = k.pb[6 + half]
                for f in range(4):
                    S.mm(P.t[:], G[g2].t[:, f, j * 128:(j + 1) * 128], W2b[i2].t[:, f, half * 512:(half + 1) * 512],
                         f == 0, f == 3, [G[g2], W2b[i2]], [P])
                S.cp('act' if half else 'dve', y_.t[:, half * 512:(half + 1) * 512], P.t[:], [P], [y_])
            S.store(k.YS_d[r0 + j * 128:r0 + (j + 1) * 128, :], y_.t[:], [y_])
    S.barrier()
    A.release(m0)


def phase_final(k):
    S, A = k.S, k.A
    NT, NSLOT = k.NT, k.NSLOT
    m0 = A.mark()
    gnf = A.alloc("gnf", [128, D], F32)
    bcast_load(k, gnf, k.normf_g)
    Y = [[A.alloc(f"Y{i}{kk}", [128, D], BF16) for kk in range(2)] for i in range(2)]
    for i in range(2):
        for kk in range(2):
            S.ms('pool', Y[i][kk].t[:], 0.0, [Y[i][kk]])
    x1 = [A.alloc(f"fx1_{i}", [128, D], F32) for i in range(2)]
    moe = A.alloc("moe", [128, D], F32)
    x2 = A.alloc("x2", [128, D], F32)
    junk = A.alloc("fjunk", [128, D], BF16)
    ss = A.alloc("fss", [128, 1], F32)
    sst = A.alloc("fsst", [128, 1], F32)
    rstd = A.alloc("frstd", [128, 1], F32)
    ot = [A.alloc(f"ot{i}", [128, D], F32) for i in range(2)]
    def load_f(t):
        for kk in range(2):
            def fn(e, t=t, kk=kk, dst=Y[t % 2][kk]):
                return e.indirect_dma_start(out=dst.t[:], out_offset=None, in_=k.YS_d[:, :],
                                            in_offset=bass.IndirectOffsetOnAxis(ap=k.slot_i.t[:, t, kk:kk + 1], axis=0),
                                            bounds_check=k.bcreg(e), oob_is_err=False)
            S.swdma(fn, [k.slot_i], [Y[t % 2][kk]])
        S.dma('sp', x1[t % 2].t[:], k.x1_d[t * 128:(t + 1) * 128, :], (), [x1[t % 2]])
    load_f(0)
    for t in range(NT):
        rows = slice(t * 128, (t + 1) * 128)
        i2 = t % 2
        if t + 1 < NT:
            load_f(t + 1)
        S.ts('dve', moe.t[:], Y[i2][0].t[:], k.wts.t[:, t, 0:1], None, ALU.mult, None, [Y[i2][0], k.wts], [moe])
        S.stt('dve', moe.t[:], Y[i2][1].t[:], k.wts.t[:, t, 1:2], moe.t[:], ALU.mult, ALU.add, [Y[i2][1], k.wts, moe], [moe])
        S.tt('dve', moe.t[:], moe.t[:], k.G2b.t[:], ALU.mult, [moe, k.G2b], [moe])
        S.tt('dve', x2.t[:], moe.t[:], x1[i2].t[:], ALU.add, [moe, x1[i2]], [x2])
        S.act(junk.t[:], x2.t[:], AF.Square, [x2], [junk, ss], accum_out=ss.t[:])
        rstd_from_ss(k, ss, sst, rstd, D)
        S.stt('dve', ot[i2].t[:], x2.t[:], rstd.t[:, 0:1], gnf.t[:], ALU.mult, ALU.mult, [x2, rstd, gnf], [ot[i2]])
        S.dma('sp', k.out[rows, :], ot[i2].t[:], [ot[i2]], ())
    S.barrier()
    A.release(m0)


CAP_DEFAULT = 1024


def kernel(**inputs):
    inputs = {kk: np.asarray(v) for kk, v in inputs.items()}
    n = inputs["x"].shape[0]
    s_tok = inputs["x"].shape[1]
    nc = build_nc(s_tok, CAP_DEFAULT)
    in_maps = [make_in_map(inputs, b) for b in range(n)]
    res = run_bass_kernel_spmd(nc, in_maps, core_ids=list(range(n)))
    out = np.stack([np.asarray(r["out"]) for r in res.results], axis=0)
    return out.astype(np.float32)
```

```python
import contextlib
import os
import numpy as np
import concourse.bass as bass
import concourse.mybir as mybir
from concourse.bass_utils import run_bass_kernel_spmd

F32 = mybir.dt.float32
BF16 = mybir.dt.bfloat16
I32 = mybir.dt.int32
U32 = mybir.dt.uint32
AF = mybir.ActivationFunctionType
ALU = mybir.AluOpType
AX = mybir.AxisListType

ENGS = ['pe', 'act', 'dve', 'pool', 'sp']


class Buf:
    __slots__ = ('name', 't', 'w', 'r')

    def __init__(self, name, t=None):
        self.name = name
        self.t = t
        self.w = None
        self.r = {}


class Sched:
    def __init__(self, nc, n_dma_sems=32, same_engine_sync=True):
        self.nc = nc
        self.stack = contextlib.ExitStack()
        self.lists = {e: [] for e in ENGS}
        self.cnt = {e: 0 for e in ENGS}
        self.esem = {e: self.stack.enter_context(nc.semaphore(f"s_{e}")) for e in ['pe', 'act', 'dve', 'pool']}
        self.dsem = [self.stack.enter_context(nc.semaphore(f"d_{i}")) for i in range(n_dma_sems)]
        self.dcnt = [0] * n_dma_sems
        self.dnext = 0
        self.swsem = [self.stack.enter_context(nc.semaphore(f"w_{i}")) for i in range(40)]
        self.swcnt = [0] * 40
        self.swnext = 0
        self.mark_t = self.stack.enter_context(nc.sbuf_tensor("mark_t", [1, 8], F32))
        self.waited = {e: {} for e in ENGS}
        self.same_engine_sync = same_engine_sync
        self.nops = 0

    def sb(self, name, shape, dtype):
        return Buf(name, self.stack.enter_context(self.nc.sbuf_tensor(name, shape, dtype)))

    def ps(self, name, shape, dtype):
        return Buf(name, self.stack.enter_context(self.nc.psum_tensor(name, shape, dtype)))

    def view(self, name, t):
        return Buf(name, t)

    def sem_of(self, k):
        if k[0] == 'e':
            return self.esem[k[1]]
        if k[0] == 'w':
            return self.swsem[k[1]]
        return self.dsem[k[1]]

    def swdma(self, fn, reads=(), writes=()):
        deps = self._deps(reads, writes)
        i = self.swnext
        self.swnext = (i + 1) % len(self.swsem)
        if self.swcnt[i] > 0:
            kk = ('w', i)
            deps[kk] = max(deps.get(kk, 0), 16 * self.swcnt[i])
        ws = self._waits('pool', deps)
        self.swcnt[i] += 1
        tok = (('w', i), 16 * self.swcnt[i])
        self.lists['pool'].append((ws, fn, (self.swsem[i], 16)))
        self._commit(tok, reads, writes)
        self.nops += 1
        return tok

    def _deps(self, reads, writes):
        deps = {}

        def add(k, v):
            if deps.get(k, 0) < v:
                deps[k] = v
        for b in reads:
            if b.w is not None:
                add(*b.w)
        for b in writes:
            if b.w is not None:
                add(*b.w)
            for k, v in b.r.items():
                add(k, v)
        return deps

    def _waits(self, eng, deps):
        ws = []
        for k, v in deps.items():
            if k == ('e', eng) and (eng == 'pe' or not self.same_engine_sync):
                continue
            if self.waited[eng].get(k, 0) >= v:
                continue
            self.waited[eng][k] = v
            ws.append((k, v))
        return ws

    def _commit(self, tok, reads, writes):
        k, v = tok
        for b in reads:
            if b.r.get(k, 0) < v:
                b.r[k] = v
        for b in writes:
            b.w = tok
            b.r = {}

    def op(self, eng, fn, reads=(), writes=()):
        deps = self._deps(reads, writes)
        ws = self._waits(eng, deps)
        self.cnt[eng] += 1
        tok = (('e', eng), self.cnt[eng])
        self.lists[eng].append((ws, fn, (self.esem[eng], 1)))
        self._commit(tok, reads, writes)
        self.nops += 1
        return tok

    def dma(self, eng, out, in_, reads=(), writes=(), fn=None, **kw):
        deps = self._deps(reads, writes)
        i = self.dnext
        self.dnext = (self.dnext + 1) % len(self.dsem)
        if self.dcnt[i] > 0:
            k = ('d', i)
            deps[k] = max(deps.get(k, 0), 16 * self.dcnt[i])
        ws = self._waits(eng, deps)
        self.dcnt[i] += 1
        tok = (('d', i), 16 * self.dcnt[i])
        if fn is None:
            def fn(e, out=out, in_=in_, kw=kw):
                return e.dma_start(out=out, in_=in_, **kw)
        self.lists[eng].append((ws, fn, (self.dsem[i], 16)))
        self._commit(tok, reads, writes)
        self.nops += 1
        return tok

    def finish(self):
        nc = self.nc
        fin = []
        for i in range(len(self.dsem)):
            if self.dcnt[i] > 0 and self.waited['sp'].get(('d', i), 0) < 16 * self.dcnt[i]:
                fin.append((('d', i), 16 * self.dcnt[i]))
        for i in range(len(self.swsem)):
            if self.swcnt[i] > 0:
                fin.append((('w', i), 16 * self.swcnt[i]))
        for e in ['pe', 'act', 'dve', 'pool']:
            if self.cnt[e] > 0:
                fin.append((('e', e), self.cnt[e]))
        self.lists['sp'].append((fin, None, None))

        def replay(name, eng):
            for ent in self.lists[name]:
                ws, fn, inc = ent[0], ent[1], ent[2]
                for k, v in ws:
                    eng.wait_ge(self.sem_of(k), v)
                if len(ent) > 3:
                    for sm_ in ent[3]:
                        eng.sem_clear(sm_)
                if fn is not None:
                    ins = fn(eng)
                    ins.then_inc(inc[0], inc[1])

        with nc.Block() as block:
            @block.tensor
            def _(e):
                replay('pe', e)

            @block.scalar
            def _(e):
                replay('act', e)

            @block.vector
            def _(e):
                replay('dve', e)

            @block.gpsimd
            def _(e):
                replay('pool', e)

            @block.sync
            def _(e):
                replay('sp', e)
        self.stack.close()

    def make_identity(self, ident_bf, ident_f32):
        for b in (ident_bf, ident_f32):
            if b is None:
                continue
            n = b.t.shape[0]
            m = b.t.shape[1]
            self.op('pool', lambda e, b=b: e.memset(b.t[:], 1.0), writes=[b])
            self.op('pool', lambda e, b=b, m=m: e.affine_select(
                out=b.t[:], in_=b.t[:], pattern=[[-1, m]], compare_op=ALU.is_equal,
                fill=0.0, base=0, channel_multiplier=1), reads=[b], writes=[b])

    def barrier(self):
        cur = {}
        for e in ['pe', 'act', 'dve', 'pool']:
            if self.cnt[e] > 0:
                cur[('e', e)] = self.cnt[e]
        for i in range(len(self.dsem)):
            if self.dcnt[i] > 0:
                cur[('d', i)] = 16 * self.dcnt[i]
        for i in range(len(self.swsem)):
            if self.swcnt[i] > 0:
                cur[('w', i)] = 16 * self.swcnt[i]
        for e in ENGS:
            ws = []
            for k, v in cur.items():
                if self.waited[e].get(k, 0) < v:
                    self.waited[e][k] = v
                    ws.append((k, v))
            if ws:
                self.lists[e].append((ws, None, None))

    def tt(self, eng, out, in0, in1, op, R, W):
        return self.op(eng, lambda e: e.tensor_tensor(out, in0, in1, op=op), R, W)

    def ts(self, eng, out, in0, s1, s2, op0, op1, R, W, accum_out=None):
        if accum_out is not None:
            return self.op(eng, lambda e: e.tensor_scalar(out, in0, s1, s2, op0, op1, accum_out=accum_out), R, W)
        if op1 is None:
            return self.op(eng, lambda e: e.tensor_scalar(out, in0, s1, None, op0), R, W)
        return self.op(eng, lambda e: e.tensor_scalar(out, in0, s1, s2, op0, op1), R, W)

    def stt(self, eng, out, in0, sc, in1, op0, op1, R, W):
        return self.op(eng, lambda e: e.scalar_tensor_tensor(out, in0, sc, in1, op0, op1), R, W)

    def act(self, out, in_, func, R, W, bias=None, scale=None, accum_out=None):
        kw = {}
        if bias is not None:
            kw['bias'] = bias
        if scale is not None:
            kw['scale'] = scale
        if accum_out is not None:
            kw['accum_out'] = accum_out
        return self.op('act', lambda e: e.activation(out, in_, func, **kw), R, W)

    def cp(self, eng, out, in_, R, W):
        if eng == 'act':
            return self.op('act', lambda e: e.copy(out, in_), R, W)
        return self.op(eng, lambda e: e.tensor_copy(out, in_), R, W)

    def mm(self, out, lhsT, rhs, start, stop, R, W):
        return self.op('pe', lambda e: e.matmul(out, lhsT, rhs, start=start, stop=stop), R, W)

    def tr(self, out, in_, ident, R, W):
        return self.op('pe', lambda e: e.transpose(out, in_, ident), R, W)

    def ms(self, eng, ap, val, W):
        return self.op(eng, lambda e: e.memset(ap, val), (), W)


class Arena:
    def __init__(self, S, words):
        self.S = S
        self.base = S.stack.enter_context(S.nc.sbuf_tensor("arena", [128, words], F32))
        self.words = words
        self.top = 0

    def mark(self):
        return self.top

    def release(self, m):
        self.top = m

    def alloc(self, name, shape, dtype, parts=128):
        n = 1
        for s in shape[1:]:
            n *= s
        esz = 4 if dtype in (F32, I32, U32) else 2
        w = (n * esz + 3) // 4
        w = (w + 7) // 8 * 8
        assert self.top + w <= self.words, f"arena overflow at {name}: {self.top + w} > {self.words}"
        v = self.base[0:shape[0], self.top:self.top + w]
        if esz == 2:
            v = v.bitcast(BF16)
        elif dtype != F32:
            v = v.bitcast(dtype)
        v = v[:, 0:n]
        if len(shape) == 3:
            v = v.rearrange("p (a b) -> p a b", a=shape[1])
        elif len(shape) == 4:
            v = v.rearrange("p (a b c) -> p a b c", a=shape[1], b=shape[2])
        elif len(shape) == 5:
            v = v.rearrange("p (a b c d) -> p a b c d", a=shape[1], b=shape[2], c=shape[3])
        self.top += w
        return Buf(name, v)


D = 1024
EPS = 1e-6
TWO_PI = 6.283185307179586
C1 = 6.28125
C2 = TWO_PI - C1
MAGIC = 12582912.0
NEG = -1.0e30
N_EXP = 32


class K:
    pass


def build_nc(S_TOK=4096, CAP=1024, debug=False, phases="0123456"):
    nc = bass.Bass("TRN2", target_bir_lowering=False)
    NT = S_TOK // 128
    NBK = S_TOK // 256
    NQB = S_TOK // 512
    NP = 4 * NT
    NSLOT = N_EXP * CAP
    NG = min(CAP, 512)

    def din(name, shape, dt=F32):
        return nc.dram_tensor(name, shape, dt, kind="ExternalInput").ap()

    def dscr(name, shape, dt):
        return nc.dram_tensor(name, shape, dt, kind=("ExternalOutput" if debug else "Internal")).ap()

    x = din("x", [S_TOK, D])
    c_in = din("c", [8, 128])
    pos_in = din("positions", [1, S_TOK], I32)
    w_ada = din("w_ada", [D, 6 * D])
    b_ada = din("b_ada", [1, 6 * D])
    norm1_g = din("norm1_g", [1, D])
    w_in = din("w_in", [D, 3592])
    b_gate = din("b_gate", [8, 1])
    conv_w = din("conv_w", [4, D])
    conv_b = din("conv_b", [1, D])
    attn_g = din("attn_out_g", [1, 512])
    mlstm_g = din("mlstm_out_g", [1, 512])
    w_out = din("w_out", [D, D])
    norm2_g = din("norm2_g", [1, D])
    w_rg = din("w_rg", [D, 4])
    b_rg = din("b_rg", [1, 4])
    w_re = din("w_re", [D, 32])
    b_re = din("b_re", [1, 32])
    w1 = din("w1", [N_EXP, D, 512])
    w3 = din("w3", [N_EXP, D, 512])
    w2 = din("w2", [N_EXP, 512, D])
    normf_g = din("norm_f_g", [1, D])
    cst = din("cst", [128, 8])
    out = nc.dram_tensor("out", [S_TOK, D], F32, kind="ExternalOutput").ap()

    qT_d = dscr("qT_d", [512, S_TOK], BF16)
    kT_d = dscr("kT_d", [512, S_TOK], BF16)
    qkm_d = dscr("qkm_d", [1024, S_TOK], BF16)
    ig_d = dscr("ig_d", [4, S_TOK], F32)
    fg_d = dscr("fg_d", [4, S_TOK], F32)
    vm_d = dscr("vm_d", [S_TOK, 512], BF16)
    om_d = dscr("om_d", [S_TOK, 512], BF16)
    cat_d = dscr("cat_d", [S_TOK, D], BF16)
    x1_d = dscr("x1_d", [S_TOK, D], F32)
    XS_d = dscr("XS_d", [NSLOT, D], BF16)
    YS_d = dscr("YS_d", [NSLOT, D], BF16)

    S = Sched(nc, same_engine_sync=(os.environ.get("SES", "1") == "1"))
    xs_tok = Buf("xs_tok")
    A = Arena(S, 51200)
    pb = [S.ps(f"pb{i}", [128, 512], F32) for i in range(8)]

    def pbf(i):
        return pb[i].t[:].bitcast(BF16)

    ident_bf = A.alloc("ident_bf", [128, 128], BF16)
    ident_f = A.alloc("ident_f", [128, 128], F32)
    ones_f = A.alloc("ones_f", [128, 128], F32)
    ones_bf = A.alloc("ones_bf", [128, 128], BF16)
    tri_bf = A.alloc("tri_bf", [128, 128], BF16)
    stri_bf = A.alloc("stri_bf", [128, 128], BF16)
    cst_sb = A.alloc("cst_sb", [128, 8], F32)
    A1 = A.alloc("A1", [128, D], F32)
    B1 = A.alloc("B1", [128, D], F32)
    G1b = A.alloc("G1b", [128, D], F32)
    A2 = A.alloc("A2", [128, D], F32)
    B2 = A.alloc("B2", [128, D], F32)
    G2b = A.alloc("G2b", [128, D], F32)
    slot_i = A.alloc("slot_i", [128, NT, 2], I32)
    wts = A.alloc("wts", [128, NT, 2], F32)

    S.make_identity(ident_bf, ident_f)
    S.ms('pool', ones_f.t[:], 1.0, [ones_f])
    S.ms('pool', ones_bf.t[:], 1.0, [ones_bf])
    for b_, cmp_ in ((tri_bf, ALU.is_ge), (stri_bf, ALU.is_gt)):
        S.ms('pool', b_.t[:], 1.0, [b_])
        S.op('pool', lambda e, b_=b_, cmp_=cmp_: e.affine_select(
            out=b_.t[:], in_=b_.t[:], pattern=[[1, 128]], compare_op=cmp_,
            fill=0.0, base=0, channel_multiplier=-1), [b_], [b_])
    S.dma('sp', cst_sb.t[:], cst, (), [cst_sb])

    zt = A.alloc("zt", [128, 4, D], BF16)
    S.ms('pool', zt.t[:], 0.0, [zt])
    xs_v = XS_d.rearrange("(n p) d -> p n d", p=128)
    nrow = NSLOT // 128
    for i0 in range(0, nrow, 4):
        nn = min(4, nrow - i0)
        S.dma('act', xs_v[:, i0:i0 + nn, :], zt.t[:, 0:nn, :], [zt], [xs_tok])

    k = K()
    k.__dict__.update(locals())
    k._bcreg = None
    k.dbg_names = []

    def dbg(name, buf, ap, shape, dt):
        if not debug:
            return
        d_ = nc.dram_tensor("dbg_" + name, shape, dt, kind="ExternalOutput").ap()
        S.dma('sp', d_, ap, [buf], ())
        k.dbg_names.append("dbg_" + name)
    k.dbg = dbg

    def bcreg(e):
        if k._bcreg is None:
            k._bcreg = e.to_reg(NSLOT - 1)
        return k._bcreg
    k.bcreg = bcreg
    if '0' in phases:
        phase0_mod(k)
    if '1' in phases:
        phase_p1(k)
    if '2' in phases:
        phase_attn(k)
    if '3' in phases:
        phase_p2(k)
    if '4' in phases:
        phase_mlstm(k)
    if '5' in phases:
        phase_out(k)
    if '6' in phases:
        ph6 = os.environ.get("PH6", "ef")
        if 'e' in ph6:
            phase_experts(k)
        if 'f' in ph6:
            phase_final(k)
    S.finish()
    return nc


def rstd_from_ss(k, ss, tmp, rstd, n, R=()):
    S = k.S
    S.ts('dve', tmp.t[:], ss.t[:], 1.0 / n, EPS, ALU.mult, ALU.add, [ss], [tmp])
    S.act(tmp.t[:], tmp.t[:], AF.Sqrt, [tmp], [tmp])
    S.op('dve', lambda e: e.reciprocal(rstd.t[:], tmp.t[:]), [tmp], [rstd])


def load_w_bf16(k, dst_ap, dstbuf, src_ap, ncols, stg, perm=False, kc=8):
    S = k.S
    st = stg[k.stg_i % len(stg)]
    k.stg_i += 1
    sv = st.t[:, 0:kc, 0:ncols]
    S.dma('sp', sv, src_ap.rearrange("(c p) n -> p c n", p=128), (), [st])
    if not perm:
        S.cp('pool', dst_ap, sv, [st], [dstbuf])
    else:
        nh = ncols // 64
        d5 = dst_ap.rearrange("p c (h two j) -> p c h two j", two=2, j=32)
        s5 = sv.rearrange("p c (h two j) -> p c h two j", two=2, j=32)
        for cc in range(kc):
            S.cp('pool', d5[:, cc, :, 0, :], s5[:, cc, :, 1, :], [st], [dstbuf])
            S.cp('pool', d5[:, cc, :, 1, :], s5[:, cc, :, 0, :], [st], [dstbuf])


def bcast_load(k, dst, src_row):
    k.S.dma('sp', dst.t[:], src_row.partition_broadcast(128), (), [dst])


def phase0_mod(k):
    S, A = k.S, k.A
    m0 = A.mark()
    c8 = A.alloc("c8", [8, 128], F32)
    sc = A.alloc("sc", [128, 8], F32)
    rep = A.alloc("rep", [128, 8, 128], F32)
    brow = A.alloc("brow", [1, 6 * D], F32)
    wst = [A.alloc(f"wst{i}", [128, 8, 512], F32) for i in range(2)]
    modb = A.alloc("modb", [128, 6, D], F32)
    gn1 = A.alloc("gn1", [128, D], F32)
    gn2 = A.alloc("gn2", [128, D], F32)
    S.dma('sp', c8.t[:], k.c_in, (), [c8])
    S.dma('sp', brow.t[:], k.b_ada, (), [brow])
    bcast_load(k, gn1, k.norm1_g)
    bcast_load(k, gn2, k.norm2_g)
    S.act(c8.t[:], c8.t[:], AF.Silu, [c8], [c8])
    S.tr(k.pb[0].t[:, 0:8], c8.t[:], k.ident_f.t[0:8, 0:8], [c8, k.ident_f], [k.pb[0]])
    S.cp('dve', sc.t[:], k.pb[0].t[:, 0:8], [k.pb[0]], [sc])
    for kk in range(8):
        S.ts('dve', rep.t[:, kk, :], k.ones_f.t[:], sc.t[:, kk:kk + 1], None, ALU.mult, None, [k.ones_f, sc], [rep])
    for j in range(12):
        st = wst[j % 2]
        S.dma('sp', st.t[:], k.w_ada[:, j * 512:(j + 1) * 512].rearrange("(c p) n -> p c n", p=128), (), [st])
        P = k.pb[1 + (j % 2)]
        for kk in range(8):
            S.mm(P.t[:], rep.t[:, kk, :], st.t[:, kk, :], kk == 0, False, [rep, st], [P])
        S.mm(P.t[:], k.ones_f.t[0:1, :], brow.t[0:1, j * 512:(j + 1) * 512], False, True, [k.ones_f, brow], [P])
        S.cp('act', modb.t[:, j // 2, (j % 2) * 512:(j % 2 + 1) * 512], P.t[:], [P], [modb])
    S.stt('dve', k.A1.t[:], modb.t[:, 1, :], 1.0, gn1.t[:], ALU.add, ALU.mult, [modb, gn1], [k.A1])
    S.cp('pool', k.B1.t[:], modb.t[:, 0, :], [modb], [k.B1])
    S.cp('pool', k.G1b.t[:], modb.t[:, 2, :], [modb], [k.G1b])
    S.stt('dve', k.A2.t[:], modb.t[:, 4, :], 1.0, gn2.t[:], ALU.add, ALU.mult, [modb, gn2], [k.A2])
    S.cp('pool', k.B2.t[:], modb.t[:, 3, :], [modb], [k.B2])
    S.cp('pool', k.G2b.t[:], modb.t[:, 5, :], [modb], [k.G2b])
    k.dbg("A1", k.A1, k.A1.t[:], [128, D], F32)
    k.dbg("B1", k.B1, k.B1.t[:], [128, D], F32)
    k.dbg("sc", sc, sc.t[:], [128, 8], F32)
    k.dbg("modb", modb, modb.t[:].rearrange("p a b -> p (a b)"), [128, 6 * D], F32)
    S.barrier()
    A.release(m0)


def alloc_hT_tmps(k):
    A = k.A
    k.xt = [A.alloc(f"xt{i}", [128, D], F32) for i in range(2)]
    k.junk = A.alloc("junk", [128, D], BF16)
    k.ss = A.alloc("ss", [128, 1], F32)
    k.sst = A.alloc("sst", [128, 1], F32)
    k.rstd = A.alloc("rstd", [128, 1], F32)
    k.htmp = A.alloc("htmp", [128, D], F32)
    k.hb = [A.alloc(f"hb{i}", [128, D], BF16) for i in range(2)]
    k.hTb = [A.alloc(f"hTb{i}", [128, 8, 512], BF16) for i in range(2)]
    k.xi = 0


def emit_hT_tile(k, tb, j, PTB):
    S = k.S
    hT = k.hTb[tb % 2]
    t = tb * 4 + j
    xt = k.xt[k.xi % 2]
    hb = k.hb[k.xi % 2]
    k.xi += 1
    S.dma('sp', xt.t[:], k.x[t * 128:(t + 1) * 128, :], (), [xt])
    S.act(k.junk.t[:], xt.t[:], AF.Square, [xt], [k.junk, k.ss], accum_out=k.ss.t[:])
    rstd_from_ss(k, k.ss, k.sst, k.rstd, D)
    S.stt('dve', k.htmp.t[:], xt.t[:], k.rstd.t[:, 0:1], k.A1.t[:], ALU.mult, ALU.mult, [xt, k.rstd, k.A1], [k.htmp])
    S.tt('pool', hb.t[:], k.htmp.t[:], k.B1.t[:], ALU.add, [k.htmp, k.B1], [hb])
    P = k.pb[PTB]
    pv = k.pbf(PTB).rearrange("p (a b) -> p a b", a=8)
    for kk in range(8):
        S.tr(pv[:, kk, :], hb.t[:, kk * 128:(kk + 1) * 128], k.ident_bf.t[:], [hb, k.ident_bf], [P])
    S.cp('act', hT.t[:, :, j * 128:(j + 1) * 128], pv, [P], [hT])
    return hT


def emit_hT_block(k, tb, PTB):
    for j in range(4):
        hT = emit_hT_tile(k, tb, j, PTB)
    return hT


def phase_p1(k):
    S, A = k.S, k.A
    NT, NQB, S_TOK = k.NT, k.NQB, k.S_TOK
    k.m_v1 = A.mark()
    k.V1 = A.alloc("V1", [128, NT, 8, 65], BF16)
    S.ms('pool', k.V1.t[:], 1.0, [k.V1])
    m0 = A.mark()
    k.stg_i = 0
    stg = [A.alloc(f"stg{i}", [128, 8, 512], F32) for i in range(1)]
    Wq = A.alloc("Wq", [128, 8, 512], BF16)
    Wqp = A.alloc("Wqp", [128, 8, 512], BF16)
    Wk = A.alloc("Wk", [128, 8, 512], BF16)
    Wkp = A.alloc("Wkp", [128, 8, 512], BF16)
    Wv = A.alloc("Wv", [128, 8, 512], BF16)
    load_w_bf16(k, Wq.t[:], Wq, k.w_in[:, 0:512], 512, stg)
    load_w_bf16(k, Wqp.t[:], Wqp, k.w_in[:, 0:512], 512, stg, perm=True)
    load_w_bf16(k, Wk.t[:], Wk, k.w_in[:, 512:1024], 512, stg)
    load_w_bf16(k, Wkp.t[:], Wkp, k.w_in[:, 512:1024], 512, stg, perm=True)
    load_w_bf16(k, Wv.t[:], Wv, k.w_in[:, 1024:1536], 512, stg)
    alloc_hT_tmps(k)
    posi = A.alloc("posi", [128, 512], I32)
    ang = A.alloc("ang", [128, 512], F32)
    a2 = A.alloc("a2", [128, 512], F32)
    kq = A.alloc("kq", [128, 512], F32)
    cosb = A.alloc("cosb", [128, 512], F32)
    sinb = A.alloc("sinb", [128, 512], F32)
    t1 = [A.alloc(f"t1_{i}", [128, 512], F32) for i in range(2)]
    t2 = [A.alloc(f"t2_{i}", [128, 512], F32) for i in range(2)]
    qo = [A.alloc(f"qo{i}", [128, 512], BF16) for i in range(3)]
    invf = k.cst_sb.t[:, 0:1]
    sgn = k.cst_sb.t[:, 1:2]
    oi = 0
    emit_hT_block(k, 0, 0)
    for tb in range(NQB):
        blk = slice(tb * 512, (tb + 1) * 512)
        hT = k.hTb[tb % 2]
        nxt = 0
        S.dma('sp', posi.t[:], k.pos_in[0:1, blk].partition_broadcast(128), (), [posi])
        S.cp('dve', ang.t[:], posi.t[:], [posi], [ang])
        S.ts('dve', ang.t[:], ang.t[:], invf, None, ALU.mult, None, [ang, k.cst_sb], [ang])
        for shift, tab, scl in ((0.0, sinb, sgn), (np.pi / 2, cosb, None)):
            S.ts('dve', a2.t[:], ang.t[:], float(shift), None, ALU.add, None, [ang], [a2])
            S.ts('dve', kq.t[:], a2.t[:], 1.0 / TWO_PI, MAGIC, ALU.mult, ALU.add, [a2], [kq])
            S.ts('dve', kq.t[:], kq.t[:], -MAGIC, None, ALU.add, None, [kq], [kq])
            S.stt('dve', a2.t[:], kq.t[:], -C1, a2.t[:], ALU.mult, ALU.add, [kq, a2], [a2])
            S.stt('dve', a2.t[:], kq.t[:], -C2, a2.t[:], ALU.mult, ALU.add, [kq, a2], [a2])
            S.ts('dve', a2.t[:], a2.t[:], float(np.pi), float(-np.pi), ALU.min, ALU.max, [a2], [a2])
            if scl is None:
                S.act(tab.t[:], a2.t[:], AF.Sin, [a2], [tab])
            else:
                S.act(tab.t[:], a2.t[:], AF.Sin, [a2, k.cst_sb], [tab], scale=scl)
        for (W, Wp, dst) in ((Wq, Wqp, k.qT_d), (Wk, Wkp, k.kT_d)):
            for c in range(4):
                P0 = k.pb[1 + 2 * (oi % 2)]
                P1 = k.pb[2 + 2 * (oi % 2)]
                for kk in range(8):
                    S.mm(P0.t[:], W.t[:, kk, c * 128:(c + 1) * 128], hT.t[:, kk, :], kk == 0, kk == 7, [W, hT], [P0])
                for kk in range(8):
                    S.mm(P1.t[:], Wp.t[:, kk, c * 128:(c + 1) * 128], hT.t[:, kk, :], kk == 0, kk == 7, [Wp, hT], [P1])
                ta, tb_ = t1[oi % 2], t2[oi % 2]
                qb_ = qo[oi % 3]
                S.tt('dve', ta.t[:], P0.t[:], cosb.t[:], ALU.mult, [P0, cosb], [ta])
                S.tt('dve', tb_.t[:], P1.t[:], sinb.t[:], ALU.mult, [P1, sinb], [tb_])
                S.tt('pool', qb_.t[:], ta.t[:], tb_.t[:], ALU.add, [ta, tb_], [qb_])
                S.dma('sp', dst[c * 128:(c + 1) * 128, blk], qb_.t[:], [qb_], ())
                oi += 1
                if tb + 1 < NQB and c % 2 == 1:
                    emit_hT_tile(k, tb + 1, nxt, 0)
                    nxt += 1
        if tb == 0:
            k.dbg("hT", hT, hT.t[:].rearrange("p a b -> p (a b)"), [128, 8 * 512], BF16)
            k.dbg("sinb", sinb, sinb.t[:], [128, 512], F32)
            k.dbg("cosb", cosb, cosb.t[:], [128, 512], F32)
            k.dbg("ang", ang, ang.t[:], [128, 512], F32)
        for j in range(4):
            t = tb * 4 + j
            P = k.pb[5 + (j % 2)]
            for kk in range(8):
                S.mm(P.t[:], hT.t[:, kk, j * 128:(j + 1) * 128], Wv.t[:, kk, :], kk == 0, kk == 7, [hT, Wv], [P])
            S.cp('act', k.V1.t[:, t, :, 0:64], P.t[:].rearrange("p (h d) -> p h d", h=8), [P], [k.V1])
    S.barrier()
    A.release(m0)


def make_consts():
    cst = np.zeros((128, 8), np.float32)
    p = np.arange(128)
    j = (p % 32).astype(np.float32)
    cst[:, 0] = (np.float32(10000.0) ** (-j / np.float32(32.0))).astype(np.float32)
    cst[:, 1] = np.where((p % 64) < 32, -1.0, 1.0)
    return cst


def make_in_map(inputs, b):
    m = {
        "x": np.ascontiguousarray(inputs["x"][b]),
        "c": np.ascontiguousarray(inputs["c"][b].reshape(8, 128)),
        "positions": np.ascontiguousarray(inputs["positions"][b].reshape(1, -1)).astype(np.int32),
        "w_ada": np.ascontiguousarray(inputs["w_ada"][0]),
        "b_ada": np.ascontiguousarray(inputs["b_ada"][0].reshape(1, -1)),
        "norm1_g": np.ascontiguousarray(inputs["norm1_g"][0].reshape(1, -1)),
        "w_in": np.ascontiguousarray(inputs["w_in"][0]),
        "b_gate": np.ascontiguousarray(inputs["b_gate"][0].reshape(8, 1)),
        "conv_w": np.ascontiguousarray(inputs["conv_w"][0]),
        "conv_b": np.ascontiguousarray(inputs["conv_b"][0].reshape(1, -1)),
        "attn_out_g": np.ascontiguousarray(inputs["attn_out_g"][0].reshape(1, -1)),
        "mlstm_out_g": np.ascontiguousarray(inputs["mlstm_out_g"][0].reshape(1, -1)),
        "w_out": np.ascontiguousarray(inputs["w_out"][0]),
        "norm2_g": np.ascontiguousarray(inputs["norm2_g"][0].reshape(1, -1)),
        "w_rg": np.ascontiguousarray(inputs["w_rg"][0]),
        "b_rg": np.ascontiguousarray(inputs["b_rg"][0].reshape(1, -1)),
        "w_re": np.ascontiguousarray(inputs["w_re"][0]),
        "b_re": np.ascontiguousarray(inputs["b_re"][0].reshape(1, -1)),
        "w1": np.ascontiguousarray(inputs["w1"][0]),
        "w3": np.ascontiguousarray(inputs["w3"][0]),
        "w2": np.ascontiguousarray(inputs["w2"][0]),
        "norm_f_g": np.ascontiguousarray(inputs["norm_f_g"].reshape(1, -1)),
        "cst": make_consts(),
    }
    return m


def phase_attn(k):
    S, A = k.S, k.A
    NT, NQB, NBK, S_TOK = k.NT, k.NQB, k.NBK, k.S_TOK
    qT = A.alloc("qT", [128, 4, S_TOK], BF16)
    kT = A.alloc("kT", [128, 4, S_TOK], BF16)
    for c in range(4):
        S.dma('sp', qT.t[:, c, :], k.qT_d[c * 128:(c + 1) * 128, :], (), [qT])
        S.dma('sp', kT.t[:, c, :], k.kT_d[c * 128:(c + 1) * 128, :], (), [kT])
    ksum = A.alloc("ksum", [128, 4, 16], F32)
    kmT = A.alloc("kmT", [128, 4, 16], BF16)
    S.ms('dve', ksum.t[:], 0.0, [ksum])
    for c in range(4):
        S.op('dve', lambda e, c=c: e.tensor_reduce(out=ksum.t[:, c, 0:NBK], in_=kT.t[:, c, :].rearrange("p (n s) -> p n s", s=256),
                                                   axis=AX.X, op=ALU.add), [kT], [ksum])
    S.ts('dve', kmT.t[:], ksum.t[:], 1.0 / 256.0, None, ALU.mult, None, [ksum], [kmT])
    LV = int(os.environ.get("ATT_LEVEL", "9"))
    if LV <= 1:
        S.barrier(); A.release(k.m_v1); return
    zer = A.alloc("zer", [128, 16, 16], F32)
    pastb = A.alloc("pastb", [128, 16, 16], F32)
    pastm = A.alloc("pastm", [128, 16, 16], F32)
    ownm = A.alloc("ownm", [128, 16, 16], F32)
    onesm = A.alloc("onesm", [128, 16, 16], F32)
    S.ms('pool', zer.t[:], 0.0, [zer])
    S.ms('pool', onesm.t[:], 1.0, [onesm])
    pat = [[1, 16], [-1, 16]]
    S.op('pool', lambda e: e.affine_select(out=pastb.t[:], in_=zer.t[:], pattern=pat, compare_op=ALU.is_gt,
                                           fill=NEG, base=0, channel_multiplier=0), [zer], [pastb])
    S.op('pool', lambda e: e.affine_select(out=pastm.t[:], in_=onesm.t[:], pattern=pat, compare_op=ALU.is_gt,
                                           fill=0.0, base=0, channel_multiplier=0), [onesm], [pastm])
    S.op('pool', lambda e: e.affine_select(out=ownm.t[:], in_=onesm.t[:], pattern=pat, compare_op=ALU.is_equal,
                                           fill=0.0, base=0, channel_multiplier=0), [onesm], [ownm])
    if LV <= 2:
        S.barrier(); A.release(k.m_v1); return
    selfull = A.alloc("selfull", [128, NT, 8, 16], F32)
    gm = A.alloc("gm", [128, 8, 16], F32)
    top8 = A.alloc("top8", [128, 8, 8], F32)
    selt = A.alloc("selt", [128, 8, 16], F32)
    PG = [k.pb[7], k.pb[6]]
    pg = [PG[hp].t[:, 0:64].rearrange("p (c n) -> p c n", c=4) for hp in range(2)]
    gmv = gm.t[:].rearrange("p (c two) n -> p c two n", two=2)
    m8 = A.alloc("m8", [128, 8], F32)
    g2 = A.alloc("g2", [128, 8, 16], F32)
    for t in range(NT):
        jb = t // 2
        for h in range(8):
            c, hp = h // 2, h % 2
            rows = slice(hp * 64, hp * 64 + 64)
            S.mm(pg[hp][:, c, :], qT.t[rows, c, t * 128:(t + 1) * 128], kmT.t[rows, c, :], True, True, [qT, kmT], [PG[hp]])
        for hp in range(2):
            S.tt('dve', gmv[:, :, hp, :], pg[hp], pastb.t[:, jb:jb + 1, :].broadcast_to([128, 4, 16]), ALU.add,
                 [PG[hp], pastb], [gm])
        src = gm
        for rnd in range(2):
            S.op('dve', lambda e, src=src: e.tensor_reduce(out=m8.t[:], in_=src.t[:], axis=AX.X, op=ALU.max), [src], [m8])
            S.tt('dve', selt.t[:], src.t[:], m8.t[:].unsqueeze(2).broadcast_to([128, 8, 16]), ALU.is_ge, [src, m8], [selt])
            S.stt('dve', g2.t[:], selt.t[:], NEG, src.t[:], ALU.mult, ALU.add, [selt, src], [g2])
            src = g2
        S.op('dve', lambda e: e.tensor_reduce(out=m8.t[:], in_=g2.t[:], axis=AX.X, op=ALU.max), [g2], [m8])
        S.tt('dve', selt.t[:], gm.t[:], m8.t[:].unsqueeze(2).broadcast_to([128, 8, 16]), ALU.is_ge, [gm, m8], [selt])
        S.tt('dve', selt.t[:], selt.t[:], pastm.t[:, jb:jb + 1, :].broadcast_to([128, 8, 16]), ALU.mult, [selt, pastm], [selt])
        S.tt('dve', selfull.t[:, t, :, :], selt.t[:], ownm.t[:, jb:jb + 1, :].broadcast_to([128, 8, 16]), ALU.add,
             [selt, ownm], [selfull])
    if LV <= 3:
        S.barrier(); A.release(k.m_v1); return
    PT = [A.alloc(f"PT{i}", [128, 512], BF16) for i in range(4)]
    acc = [A.alloc(f"acc{i}", [128, 4, 65], F32) for i in range(2)]
    rden = A.alloc("rden", [128, 4], F32)
    o_n = A.alloc("o_n", [128, 4, 64], F32)
    osq = A.alloc("osq", [128, 4, 64], F32)
    ss4 = A.alloc("ss4", [128, 4], F32)
    ss4t = A.alloc("ss4t", [128, 4], F32)
    rs4 = A.alloc("rs4", [128, 4], F32)
    gattn = A.alloc("gattn", [128, 512], F32)
    bcast_load(k, gattn, k.attn_g)
    attn_sb = A.alloc("attn_sb", [128, NT, 512], BF16)
    it = 0
    pti = 0
    atmp = [A.alloc(f"atmp{i}", [128, 4, 65], F32) for i in range(2)]
    pon = 0
    for h in range(8):
        c, hp = h // 2, h % 2
        rows = slice(hp * 64, hp * 64 + 64)
        for qb in range(NQB):
            ac = acc[it % 2]
            it += 1
            S.ms('pool', ac.t[:], 0.0, [ac])
            for n in range(2 * qb + 2):
                PO = k.pb[6 + (pon % 2)]
                pon += 1
                po = PO.t[:, 0:260].rearrange("p (j d) -> p j d", j=4)
                pts = {}
                for kc in (2 * n, 2 * n + 1):
                    jmin = max(0, kc - 4 * qb)
                    if jmin > 3:
                        continue
                    PS = k.pb[3 * hp + (pti % 3)]
                    pt = PT[pti % 3]
                    pti += 1
                    cols = slice(jmin * 128, 512)
                    S.mm(PS.t[:, cols], kT.t[rows, c, kc * 128:(kc + 1) * 128],
                         qT.t[rows, c, qb * 512 + jmin * 128:(qb + 1) * 512], True, True, [kT, qT], [PS])
                    S.act(pt.t[:, cols], PS.t[:, cols], AF.Exp, [PS], [pt], scale=0.125)
                    jd = kc - 4 * qb
                    if 0 <= jd <= 3:
                        S.tt('pool', pt.t[:, jd * 128:(jd + 1) * 128], pt.t[:, jd * 128:(jd + 1) * 128], k.tri_bf.t[:],
                             ALU.mult, [pt, k.tri_bf], [pt])
                    pts[kc] = pt
                full = (n <= 2 * qb)
                for j in range(4):
                    qt = 4 * qb + j
                    if n > qt // 2:
                        continue
                    chunks = [kc for kc in (2 * n, 2 * n + 1) if kc <= qt]
                    for idx, kc in enumerate(chunks):
                        S.mm(po[:, j, :], pts[kc].t[:, j * 128:(j + 1) * 128], k.V1.t[:, kc, h, :],
                             idx == 0, idx == len(chunks) - 1, [pts[kc], k.V1], [PO])
                    if not full:
                        S.stt('dve', ac.t[:, j, :], po[:, j, :], selfull.t[:, qt, h, n:n + 1], ac.t[:, j, :],
                              ALU.mult, ALU.add, [PO, selfull, ac], [ac])
                if full:
                    tm = atmp[pon % 2]
                    S.tt('dve', tm.t[:], po, selfull.t[:, 4 * qb:4 * qb + 4, h, n:n + 1].broadcast_to([128, 4, 65]),
                         ALU.mult, [PO, selfull], [tm])
                    S.tt('pool', ac.t[:], ac.t[:], tm.t[:], ALU.add, [ac, tm], [ac])
            S.op('dve', lambda e, ac=ac: e.reciprocal(rden.t[:], ac.t[:, :, 64]), [ac], [rden])
            S.tt('dve', o_n.t[:], ac.t[:, :, 0:64], rden.t[:].unsqueeze(2).broadcast_to([128, 4, 64]), ALU.mult, [ac, rden], [o_n])
            S.tt('pool', osq.t[:], o_n.t[:], o_n.t[:], ALU.mult, [o_n], [osq])
            S.op('dve', lambda e: e.tensor_reduce(out=ss4.t[:], in_=osq.t[:], axis=AX.X, op=ALU.add), [osq], [ss4])
            rstd_from_ss(k, ss4, ss4t, rs4, 64)
            S.tt('dve', o_n.t[:], o_n.t[:], rs4.t[:].unsqueeze(2).broadcast_to([128, 4, 64]), ALU.mult, [o_n, rs4], [o_n])
            S.tt('dve', attn_sb.t[:, qb * 4:(qb + 1) * 4, h * 64:(h + 1) * 64], o_n.t[:],
                 gattn.t[:, h * 64:(h + 1) * 64].unsqueeze(1).broadcast_to([128, 4, 64]), ALU.mult, [o_n, gattn], [attn_sb])
    S.dma('sp', k.cat_d[:, 0:512].rearrange("(t p) d -> p t d", p=128), attn_sb.t[:], [attn_sb], ())
    S.barrier()
    A.release(k.m_v1)


def phase_p2(k):
    S, A = k.S, k.A
    NT, NQB = k.NT, k.NQB
    m0 = A.mark()
    k.stg_i = 0
    stg = [A.alloc(f"stg{i}", [128, 8, 512], F32) for i in range(2)]
    Wqkm = A.alloc("Wqkm", [128, 8, 1024], BF16)
    Wvm = A.alloc("Wvm", [128, 8, 512], BF16)
    Wom = A.alloc("Wom", [128, 8, 512], BF16)
    Wg = A.alloc("Wg", [128, 8, 8], BF16)
    load_w_bf16(k, Wqkm.t[:, :, 0:512], Wqkm, k.w_in[:, 1536:2048], 512, stg)
    load_w_bf16(k, Wqkm.t[:, :, 512:1024], Wqkm, k.w_in[:, 2048:2560], 512, stg)
    load_w_bf16(k, Wvm.t[:], Wvm, k.w_in[:, 2560:3072], 512, stg)
    load_w_bf16(k, Wom.t[:], Wom, k.w_in[:, 3072:3584], 512, stg)
    load_w_bf16(k, Wg.t[:], Wg, k.w_in[:, 3584:3592], 8, stg)
    alloc_hT_tmps(k)
    qo = [A.alloc(f"qo{i}", [128, 512], BF16) for i in range(3)]
    gsb = [A.alloc(f"gsb{i}", [4, 512], F32) for i in range(2)]
    oi = 0
    emit_hT_block(k, 0, 0)
    for tb in range(NQB):
        blk = slice(tb * 512, (tb + 1) * 512)
        hT = k.hTb[tb % 2]
        nxt = 0
        for c in range(8):
            P = k.pb[1 + (oi % 2)]
            for kk in range(8):
                S.mm(P.t[:], Wqkm.t[:, kk, c * 128:(c + 1) * 128], hT.t[:, kk, :], kk == 0, kk == 7, [Wqkm, hT], [P])
            q_ = qo[oi % 3]
            S.cp('act' if oi % 2 else 'dve', q_.t[:], P.t[:], [P], [q_])
            S.dma('sp', k.qkm_d[c * 128:(c + 1) * 128, blk], q_.t[:], [q_], ())
            oi += 1
            if tb + 1 < NQB and c % 2 == 1:
                emit_hT_tile(k, tb + 1, nxt, 0)
                nxt += 1
        for gi, dst in ((0, k.ig_d), (1, k.fg_d)):
            P = k.pb[3 + gi]
            for kk in range(8):
                S.mm(P.t[0:4, :], Wg.t[:, kk, gi * 4:(gi + 1) * 4], hT.t[:, kk, :], kk == 0, kk == 7, [Wg, hT], [P])
            S.cp('dve', gsb[gi].t[:], P.t[0:4, :], [P], [gsb[gi]])
            S.dma('sp', dst[:, blk], gsb[gi].t[:], [gsb[gi]], ())
        for j in range(4):
            t = tb * 4 + j
            for wi, (W, dst) in enumerate(((Wvm, k.vm_d), (Wom, k.om_d))):
                P = k.pb[5 + wi]
                for kk in range(8):
                    S.mm(P.t[:], hT.t[:, kk, j * 128:(j + 1) * 128], W.t[:, kk, :], kk == 0, kk == 7, [hT, W], [P])
                q_ = qo[oi % 3]
                S.cp('act' if wi else 'dve', q_.t[:], P.t[:], [P], [q_])
                S.dma('sp', dst[t * 128:(t + 1) * 128, :], q_.t[:], [q_], ())
                oi += 1
    S.barrier()
    A.release(m0)


def phase_mlstm(k):
    S, A = k.S, k.A
    NT, S_TOK, NP = k.NT, k.S_TOK, k.NP
    m0 = A.mark()
    KSC = float(128.0 ** -0.5)
    cw5 = A.alloc("cw5", [5, D], F32)
    cwT = A.alloc("cwT", [128, 8, 5], F32)
    S.dma('sp', cw5.t[0:4, :], k.conv_w, (), [cw5])
    S.dma('sp', cw5.t[4:5, :], k.conv_b, (), [cw5])
    for fc in range(8):
        P = k.pb[fc % 2]
        S.tr(P.t[:, 0:5], cw5.t[:, fc * 128:(fc + 1) * 128], k.ident_f.t[0:5, 0:5], [cw5, k.ident_f], [P])
        S.cp('dve', cwT.t[:, fc, :], P.t[:, 0:5], [P], [cwT])
    qmT = A.alloc("qmT", [128, 4, S_TOK], BF16)
    kmT = A.alloc("kmT2", [128, 4, S_TOK], BF16)
    mc = A.mark()
    raw = [A.alloc(f"raw{i}", [128, 3 + S_TOK], BF16) for i in range(2)]
    cacc = A.alloc("cacc", [128, S_TOK], F32)
    for fc in range(8):
        rw = raw[fc % 2]
        S.ms('pool', rw.t[:, 0:3], 0.0, [rw])
        S.dma('sp', rw.t[:, 3:3 + S_TOK], k.qkm_d[fc * 128:(fc + 1) * 128, :], (), [rw])
        S.ts('dve', cacc.t[:], rw.t[:, 0:S_TOK], cwT.t[:, fc, 0:1], cwT.t[:, fc, 4:5], ALU.mult, ALU.add, [rw, cwT], [cacc])
        for j in range(1, 4):
            S.stt('dve', cacc.t[:], rw.t[:, j:j + S_TOK], cwT.t[:, fc, j:j + 1], cacc.t[:], ALU.mult, ALU.add,
                  [rw, cwT, cacc], [cacc])
        dst = qmT if fc < 4 else kmT
        S.act(dst.t[:, fc % 4, :], cacc.t[:], AF.Silu, [cacc], [dst])
    S.barrier()
    A.release(mc)
    def g_(name, shape=None):
        return A.alloc(name, shape or [NP, 128], F32)
    ig, fg, cs, u, cmu, r, tq, wint, enm, wz, eu, er = [g_(n) for n in
        ("ig", "fg", "cs", "u", "cmu", "r", "tq", "wint", "enm", "wz", "eu", "er")]
    bcol = g_("bcol", [NP, 2])
    nbf, acol, mloc, mcol, mpcol, amm = [g_(n, [NP, 1]) for n in ("nbf", "acol", "mloc", "mcol", "mpcol", "amm")]
    arow, mlrow, mrow, mprow, sprow = [g_(n, [1, NP]) for n in ("arow", "mlrow", "mrow", "mprow", "sprow")]
    sprev_b = A.alloc("sprev_b", [128, NP], F32)
    tmq = A.alloc("tmq", [128, 5, NP], F32)
    onesn = k.ones_f.t[0:NP, :]
    idn = k.ident_f.t[0:NP, 0:NP]
    S.dma('sp', ig.t[:], k.ig_d.rearrange("h (c l) -> (h c) l", l=128), (), [ig])
    S.dma('sp', fg.t[:], k.fg_d.rearrange("h (c l) -> (h c) l", l=128), (), [fg])
    for h in range(4):
        S.dma('sp', bcol.t[h * NT:(h + 1) * NT, 0:1], k.b_gate[h:h + 1, :].partition_broadcast(NT), (), [bcol])
        S.dma('sp', bcol.t[h * NT:(h + 1) * NT, 1:2], k.b_gate[4 + h:5 + h, :].partition_broadcast(NT), (), [bcol])
    S.ts('dve', nbf.t[:], bcol.t[:, 1:2], -1.0, None, ALU.mult, None, [bcol], [nbf])
    S.act(fg.t[:], fg.t[:], AF.Exp, [fg, nbf], [fg], bias=nbf.t[:, 0:1], scale=-1.0)
    S.act(fg.t[:], fg.t[:], AF.Ln, [fg], [fg], bias=1.0)
    S.op('dve', lambda e: e.tensor_tensor_scan(cs.t[:], onesn, fg.t[:], 0.0, ALU.mult, ALU.add), [fg, k.ones_f], [cs])
    S.ts('dve', ig.t[:], ig.t[:], bcol.t[:, 0:1], None, ALU.add, None, [ig, bcol], [ig])
    S.tt('dve', u.t[:], ig.t[:], cs.t[:], ALU.add, [ig, cs], [u])
    S.op('dve', lambda e: e.tensor_tensor_scan(cmu.t[:], onesn, u.t[:], NEG, ALU.mult, ALU.max), [u, k.ones_f], [cmu])
    S.ts('dve', acol.t[:], cs.t[:, 127:128], -1.0, None, ALU.mult, None, [cs], [acol])
    S.tt('dve', mloc.t[:], acol.t[:], cmu.t[:, 127:128], ALU.add, [acol, cmu], [mloc])
    P = k.pb[0]
    S.tr(P.t[0:1, 0:NP], acol.t[:], idn, [acol, k.ident_f], [P])
    S.cp('dve', arow.t[:], P.t[0:1, 0:NP], [P], [arow])
    P = k.pb[1]
    S.tr(P.t[0:1, 0:NP], mloc.t[:], idn, [mloc, k.ident_f], [P])
    S.cp('dve', mlrow.t[:], P.t[0:1, 0:NP], [P], [mlrow])
    S.ms('dve', mprow.t[:], 0.0, [mprow])
    for h in range(4):
        sl = slice(h * NT, (h + 1) * NT)
        S.op('dve', lambda e, sl=sl: e.tensor_tensor_scan(mrow.t[:, sl], arow.t[:, sl], mlrow.t[:, sl], 0.0, ALU.add, ALU.max),
             [arow, mlrow], [mrow])
        if NT > 1:
            S.cp('dve', mprow.t[:, h * NT + 1:(h + 1) * NT], mrow.t[:, h * NT:(h + 1) * NT - 1], [mrow], [mprow])
    S.tt('dve', sprow.t[:], arow.t[:], mprow.t[:], ALU.add, [arow, mprow], [sprow])
    S.tt('dve', sprow.t[:], sprow.t[:], mrow.t[:], ALU.subtract, [sprow, mrow], [sprow])
    S.act(sprow.t[:], sprow.t[:], AF.Exp, [sprow], [sprow])
    P = k.pb[2]
    S.mm(P.t[:, 0:NP], k.ones_f.t[0:1, :], sprow.t[:], True, True, [k.ones_f, sprow], [P])
    S.cp('dve', sprev_b.t[:], P.t[:, 0:NP], [P], [sprev_b])
    P = k.pb[3]
    S.mm(P.t[0:NP, 0:1], mrow.t[:], k.ones_f.t[0:1, 0:1], True, True, [mrow, k.ones_f], [P])
    S.cp('dve', mcol.t[:], P.t[0:NP, 0:1], [P], [mcol])
    P = k.pb[4]
    S.mm(P.t[0:NP, 0:1], mprow.t[:], k.ones_f.t[0:1, 0:1], True, True, [mprow, k.ones_f], [P])
    S.cp('dve', mpcol.t[:], P.t[0:NP, 0:1], [P], [mpcol])
    S.ts('dve', r.t[:], cmu.t[:], mpcol.t[:, 0:1], -1.0, ALU.max, ALU.mult, [cmu, mpcol], [r])
    S.act(wint.t[:], r.t[:], AF.Exp, [r, mpcol], [wint], bias=mpcol.t[:, 0:1])
    S.tt('dve', tq.t[:], r.t[:], cs.t[:], ALU.add, [r, cs], [tq])
    S.act(enm.t[:], tq.t[:], AF.Exp, [tq], [enm])
    S.tt('dve', amm.t[:], acol.t[:], mcol.t[:], ALU.subtract, [acol, mcol], [amm])
    S.ts('dve', amm.t[:], amm.t[:], float(np.log(KSC)), None, ALU.add, None, [amm], [amm])
    S.act(wz.t[:], u.t[:], AF.Exp, [u, amm], [wz], bias=amm.t[:, 0:1])
    S.act(eu.t[:], u.t[:], AF.Exp, [u], [eu])
    S.act(er.t[:], r.t[:], AF.Exp, [r], [er])
    for qi, Q in enumerate((eu, er, wint, enm, wz)):
        P = k.pb[5 + (qi % 2)]
        S.tr(P.t[:, 0:NP], Q.t[:], idn, [Q, k.ident_f], [P])
        S.cp('dve', tmq.t[:, qi, :], P.t[:, 0:NP], [P], [tmq])
    tri_s = A.alloc("tri_s", [128, 128], F32)
    S.ts('dve', tri_s.t[:], k.tri_bf.t[:], KSC, None, ALU.mult, None, [k.tri_bf], [tri_s])
    vaug = A.alloc("vaug", [128, NT, 4, 129], BF16)
    S.ms('pool', vaug.t[:], 1.0, [vaug])
    for h in range(4):
        S.dma('sp', vaug.t[:, :, h, 0:128], k.vm_d[:, h * 128:(h + 1) * 128].rearrange("(c p) d -> p c d", p=128), (), [vaug])
    CT = [A.alloc(f"CT{h}", [128, 129], F32) for h in range(4)]
    CTb = [A.alloc(f"CTb{h}", [128, 129], BF16) for h in range(4)]
    for h in range(4):
        S.ms('pool', CT[h].t[:], 0.0, [CT[h]])
        S.ms('pool', CTb[h].t[:], 0.0, [CTb[h]])
    gml = A.alloc("gml", [128, 512], F32)
    bcast_load(k, gml, k.mlstm_g)
    Am = [A.alloc(f"Am{i}", [128, 128], BF16) for i in range(2)]
    vu = [A.alloc(f"vu{i}", [128, 129], BF16) for i in range(2)]
    wv = [A.alloc(f"wv{i}", [128, 129], BF16) for i in range(2)]
    ktm = [A.alloc(f"ktm{i}", [128, 128], BF16) for i in range(2)]
    inter = [A.alloc(f"inter{i}", [128, 129], F32) for i in range(2)]
    tot = [A.alloc(f"tot{i}", [128, 129], F32) for i in range(2)]
    den = A.alloc("den", [128, 1], F32)
    rdn = A.alloc("rdn", [128, 1], F32)
    hmraw = [A.alloc(f"hmraw{i}", [128, 4, 128], F32) for i in range(2)]
    hsq = A.alloc("hsq", [128, 4, 128], F32)
    ss4 = A.alloc("mss4", [128, 4], F32)
    ss4t = A.alloc("mss4t", [128, 4], F32)
    rs4 = A.alloc("mrs4", [128, 4], F32)
    omt = [A.alloc(f"omt{i}", [128, 512], BF16) for i in range(2)]
    sgm = A.alloc("sgm", [128, 512], F32)
    hout = [A.alloc(f"hout{i}", [128, 512], BF16) for i in range(2)]
    it = 0
    for c in range(NT):
        ck = slice(c * 128, (c + 1) * 128)
        hr = hmraw[c % 2]
        S.dma('sp', omt[c % 2].t[:], k.om_d[ck, :], (), [omt[c % 2]])
        for h in range(4):
            hc = h * NT + c
            i2 = it % 2
            it += 1
            PA = k.pb[0 + i2]
            S.mm(PA.t[:, 0:128], kmT.t[:, h, ck], qmT.t[:, h, ck], True, True, [kmT, qmT], [PA])
            S.tt('dve', Am[i2].t[:], PA.t[:, 0:128], tri_s.t[:], ALU.mult, [PA, tri_s], [Am[i2]])
            S.ts('pool', vu[i2].t[:], vaug.t[:, c, h, :], tmq.t[:, 0, hc:hc + 1], None, ALU.mult, None, [vaug, tmq], [vu[i2]])
            PI = k.pb[2 + i2]
            S.mm(PI.t[:, 0:129], Am[i2].t[:], vu[i2].t[:], True, True, [Am[i2], vu[i2]], [PI])
            PN = k.pb[4 + i2]
            S.mm(PN.t[:, 0:129], qmT.t[:, h, ck], CTb[h].t[:], True, True, [qmT, CTb[h]], [PN])
            S.act(inter[i2].t[:], PN.t[:, 0:129], AF.Copy, [PN, tmq], [inter[i2]], scale=tmq.t[:, 2, hc:hc + 1])
            S.stt('dve', tot[i2].t[:], PI.t[:, 0:129], tmq.t[:, 1, hc:hc + 1], inter[i2].t[:], ALU.mult, ALU.add,
                  [PI, tmq, inter[i2]], [tot[i2]])
            S.ts('dve', rdn.t[:], tot[i2].t[:, 128:129], -1.0, None, ALU.mult, None, [tot[i2]], [rdn])
            S.stt('dve', den.t[:], tot[i2].t[:, 128:129], rdn.t[:, 0:1], tmq.t[:, 3, hc:hc + 1], ALU.max, ALU.max,
                  [tot[i2], rdn, tmq], [den])
            S.op('dve', lambda e: e.reciprocal(rdn.t[:], den.t[:]), [den], [rdn])
            S.act(hr.t[:, h, :], tot[i2].t[:, 0:128], AF.Copy, [tot[i2], rdn], [hr], scale=rdn.t[:, 0:1])
            if c < NT - 1:
                S.ts('pool', wv[i2].t[:], vaug.t[:, c, h, :], tmq.t[:, 4, hc:hc + 1], None, ALU.mult, None, [vaug, tmq], [wv[i2]])
                PK = k.pb[7]
                pk = k.pbf(7)[:, 0:128]
                S.tr(pk, kmT.t[:, h, ck], k.ident_bf.t[:], [kmT, k.ident_bf], [PK])
                S.cp('act', ktm[i2].t[:], pk, [PK], [ktm[i2]])
                PC = k.pb[6]
                S.mm(PC.t[:, 0:129], ktm[i2].t[:], wv[i2].t[:], True, True, [ktm[i2], wv[i2]], [PC])
                S.stt('dve', CT[h].t[:], CT[h].t[:], sprev_b.t[:, hc:hc + 1], PC.t[:, 0:129], ALU.mult, ALU.add,
                      [CT[h], sprev_b, PC], [CT[h]])
                S.cp('act', CTb[h].t[:], CT[h].t[:], [CT[h]], [CTb[h]])
        om = omt[c % 2]
        ho = hout[c % 2]
        S.tt('pool', hsq.t[:], hr.t[:], hr.t[:], ALU.mult, [hr], [hsq])
        S.op('dve', lambda e: e.tensor_reduce(out=ss4.t[:], in_=hsq.t[:], axis=AX.X, op=ALU.add), [hsq], [ss4])
        rstd_from_ss(k, ss4, ss4t, rs4, 128)
        S.tt('dve', hr.t[:], hr.t[:], rs4.t[:].unsqueeze(2).broadcast_to([128, 4, 128]), ALU.mult, [hr, rs4], [hr])
        hr2 = hr.t[:].rearrange("p h d -> p (h d)")
        S.tt('pool', hr2, hr2, gml.t[:], ALU.mult, [hr, gml], [hr])
        S.act(sgm.t[:], om.t[:], AF.Sigmoid, [om], [sgm])
        S.tt('dve', ho.t[:], hr2, sgm.t[:], ALU.mult, [hr, sgm], [ho])
        S.dma('sp', k.cat_d[ck, 512:1024], ho.t[:], [ho], ())
    S.barrier()
    A.release(m0)


def phase_out(k):
    S, A = k.S, k.A
    NT, CAP, NSLOT = k.NT, k.CAP, k.NSLOT
    m0 = A.mark()
    k.stg_i = 0
    stg = [A.alloc(f"stg{i}", [128, 8, 512], F32) for i in range(2)]
    Wout = A.alloc("Wout", [128, 8, 1024], BF16)
    load_w_bf16(k, Wout.t[:, :, 0:512], Wout, k.w_out[:, 0:512], 512, stg)
    load_w_bf16(k, Wout.t[:, :, 512:1024], Wout, k.w_out[:, 512:1024], 512, stg)
    Wr = A.alloc("Wr", [128, 8, 36], F32)
    S.dma('sp', Wr.t[:, :, 0:4], k.w_rg.rearrange("(c p) n -> p c n", p=128), (), [Wr])
    S.dma('sp', Wr.t[:, :, 4:36], k.w_re.rearrange("(c p) n -> p c n", p=128), (), [Wr])
    brow = A.alloc("brow_r", [1, 36], F32)
    S.dma('sp', brow.t[:, 0:4], k.b_rg, (), [brow])
    S.dma('sp', brow.t[:, 4:36], k.b_re, (), [brow])
    ecap = A.alloc("ecap", [128, 32], F32)
    S.op('pool', lambda e: e.iota(ecap.t[:], pattern=[[CAP, 32]], base=0, channel_multiplier=0,
                                  allow_small_or_imprecise_dtypes=True), (), [ecap])
    cntb = A.alloc("cntb", [128, 32], F32)
    S.ms('pool', cntb.t[:], 0.0, [cntb])
    catt = [A.alloc(f"catt{i}", [128, D], BF16) for i in range(2)]
    catT = [A.alloc(f"catT{i}", [128, 8, 128], BF16) for i in range(2)]
    xt = [A.alloc(f"oxt{i}", [128, D], F32) for i in range(2)]
    ytmp = A.alloc("ytmp", [128, D], F32)
    x1 = [A.alloc(f"x1_{i}", [128, D], F32) for i in range(2)]
    junk = A.alloc("ojunk", [128, D], BF16)
    ss = A.alloc("oss", [128, 1], F32)
    sst = A.alloc("osst", [128, 1], F32)
    rstd = A.alloc("orstd", [128, 1], F32)
    h2f = A.alloc("h2f", [128, D], F32)
    h2b = [A.alloc(f"h2b{i}", [128, D], BF16) for i in range(2)]
    h2T = A.alloc("h2T", [128, 8, 128], F32)
    lg = A.alloc("lg", [128, 36], F32)
    sm = {n: A.alloc("r_" + n, sh, F32) for n, sh in (
        ("gmax", [128, 1]), ("ngmax", [128, 1]), ("eg", [128, 4]), ("sumg", [128, 1]), ("gw", [128, 1]),
        ("ohg", [128, 4]), ("elm", [128, 4, 8]), ("els", [128, 8]), ("top8", [128, 8]), ("dd", [128, 1]),
        ("rd", [128, 1]), ("wk", [128, 2]), ("oh", [128, 2, 8]), ("E", [128, 2, 32]), ("mask", [128, 32]),
        ("pos", [128, 32]), ("val", [128, 32]), ("tmp32", [128, 32]), ("sk", [128, 2]), ("pk", [128, 2]),
        ("ok", [128, 2]))}
    maskb = A.alloc("maskb", [128, 32], BF16)
    BIGV = float(NSLOT + 4096)
    def load_t(t):
        rows = slice(t * 128, (t + 1) * 128)
        S.dma('sp', catt[t % 2].t[:], k.cat_d[rows, :], (), [catt[t % 2]])
        S.dma('sp', xt[t % 2].t[:], k.x[rows, :], (), [xt[t % 2]])
    load_t(0)
    for t in range(NT):
        rows = slice(t * 128, (t + 1) * 128)
        i2 = t % 2
        if t + 1 < NT:
            load_t(t + 1)
        PT_ = k.pb[0]
        pv = k.pbf(0).rearrange("p (a b) -> p a b", a=8)
        for kk in range(8):
            S.tr(pv[:, kk, :], catt[i2].t[:, kk * 128:(kk + 1) * 128], k.ident_bf.t[:], [catt[i2], k.ident_bf], [PT_])
        S.cp('act', catT[i2].t[:], pv, [PT_], [catT[i2]])
        for half in range(2):
            P = k.pb[1 + half]
            hs = slice(half * 512, (half + 1) * 512)
            for kk in range(8):
                S.mm(P.t[:], catT[i2].t[:, kk, :], Wout.t[:, kk, hs], kk == 0, kk == 7, [catT[i2], Wout], [P])
            S.tt('dve', ytmp.t[:, hs], P.t[:], k.G1b.t[:, hs], ALU.mult, [P, k.G1b], [ytmp])
        S.tt('pool', x1[i2].t[:], ytmp.t[:], xt[i2].t[:], ALU.add, [ytmp, xt[i2]], [x1[i2]])
        S.dma('sp', k.x1_d[rows, :], x1[i2].t[:], [x1[i2]], ())
        S.act(junk.t[:], x1[i2].t[:], AF.Square, [x1[i2]], [junk, ss], accum_out=ss.t[:])
        rstd_from_ss(k, ss, sst, rstd, D)
        S.stt('dve', h2f.t[:], x1[i2].t[:], rstd.t[:, 0:1], k.A2.t[:], ALU.mult, ALU.mult, [x1[i2], rstd, k.A2], [h2f])
        S.tt('pool', h2f.t[:], h2f.t[:], k.B2.t[:], ALU.add, [h2f, k.B2], [h2f])
        S.cp('act', h2b[i2].t[:], h2f.t[:], [h2f], [h2b[i2]])
        for g4 in range(2):
            P = k.pb[3 + g4]
            pv4 = P.t[:].rearrange("p (a b) -> p a b", a=4)
            for q in range(4):
                kk = g4 * 4 + q
                S.tr(pv4[:, q, :], h2f.t[:, kk * 128:(kk + 1) * 128], k.ident_f.t[:], [h2f, k.ident_f], [P])
            S.cp('act' if g4 else 'dve', h2T.t[:, g4 * 4:(g4 + 1) * 4, :], pv4, [P], [h2T])
        P = k.pb[5]
        for kk in range(8):
            S.mm(P.t[:, 0:36], h2T.t[:, kk, :], Wr.t[:, kk, :], kk == 0, False, [h2T, Wr], [P])
        S.mm(P.t[:, 0:36], k.ones_f.t[0:1, :], brow.t[:], False, True, [k.ones_f, brow], [P])
        S.cp('dve', lg.t[:], P.t[:, 0:36], [P], [lg])
        g = sm
        S.op('dve', lambda e: e.tensor_reduce(out=g["gmax"].t[:], in_=lg.t[:, 0:4], axis=AX.X, op=ALU.max), [lg], [g["gmax"]])
        S.ts('dve', g["ngmax"].t[:], g["gmax"].t[:], -1.0, None, ALU.mult, None, [g["gmax"]], [g["ngmax"]])
        S.act(g["eg"].t[:], lg.t[:, 0:4], AF.Exp, [lg, g["ngmax"]], [g["eg"], g["sumg"]], bias=g["ngmax"].t[:, 0:1],
              accum_out=g["sumg"].t[:])
        S.op('dve', lambda e: e.reciprocal(g["gw"].t[:], g["sumg"].t[:]), [g["sumg"]], [g["gw"]])
        S.ts('dve', g["ohg"].t[:], lg.t[:, 0:4], g["gmax"].t[:, 0:1], None, ALU.is_equal, None, [lg, g["gmax"]], [g["ohg"]])
        S.tt('dve', g["elm"].t[:], lg.t[:, 4:36].rearrange("p (g e) -> p g e", g=4),
             g["ohg"].t[:].unsqueeze(2).broadcast_to([128, 4, 8]), ALU.mult, [lg, g["ohg"]], [g["elm"]])
        S.op('dve', lambda e: e.tensor_reduce(out=g["els"].t[:], in_=g["elm"].t[:].rearrange("p g e -> p e g"),
                                              axis=AX.X, op=ALU.add), [g["elm"]], [g["els"]])
        S.op('dve', lambda e: e.tensor_reduce(out=g["top8"].t[:, 0:1], in_=g["els"].t[:], axis=AX.X, op=ALU.max), [g["els"]], [g["top8"]])
        S.ts('dve', g["oh"].t[:, 0, :], g["els"].t[:], g["top8"].t[:, 0:1], None, ALU.is_equal, None, [g["els"], g["top8"]], [g["oh"]])
        S.stt('dve', g["oh"].t[:, 1, :], g["oh"].t[:, 0, :], NEG, g["els"].t[:], ALU.mult, ALU.add, [g["oh"], g["els"]], [g["oh"]])
        S.op('dve', lambda e: e.tensor_reduce(out=g["top8"].t[:, 1:2], in_=g["oh"].t[:, 1, :], axis=AX.X, op=ALU.max), [g["oh"]], [g["top8"]])
        S.tt('dve', g["dd"].t[:], g["top8"].t[:, 1:2], g["top8"].t[:, 0:1], ALU.subtract, [g["top8"]], [g["dd"]])
        S.act(g["dd"].t[:], g["dd"].t[:], AF.Exp, [g["dd"]], [g["dd"]])
        S.ts('dve', g["dd"].t[:], g["dd"].t[:], 1.0, None, ALU.add, None, [g["dd"]], [g["dd"]])
        S.op('dve', lambda e: e.reciprocal(g["rd"].t[:], g["dd"].t[:]), [g["dd"]], [g["rd"]])
        S.tt('dve', g["wk"].t[:, 0:1], g["gw"].t[:], g["rd"].t[:], ALU.mult, [g["gw"], g["rd"]], [g["wk"]])
        S.tt('dve', g["wk"].t[:, 1:2], g["gw"].t[:], g["wk"].t[:, 0:1], ALU.subtract, [g["gw"], g["wk"]], [g["wk"]])
        for kk in range(2):
            S.ts('dve', g["oh"].t[:, kk, :], g["els"].t[:], g["top8"].t[:, kk:kk + 1], None, ALU.is_equal, None,
                 [g["els"], g["top8"]], [g["oh"]])
            S.tt('dve', g["E"].t[:, kk, :].rearrange("p (g e) -> p g e", g=4),
                 g["ohg"].t[:].unsqueeze(2).broadcast_to([128, 4, 8]),
                 g["oh"].t[:, kk, :].unsqueeze(1).broadcast_to([128, 4, 8]), ALU.mult, [g["ohg"], g["oh"]], [g["E"]])
        S.tt('dve', g["mask"].t[:], g["E"].t[:, 0, :], g["E"].t[:, 1, :], ALU.add, [g["E"]], [g["mask"]])
        S.cp('dve', maskb.t[:], g["mask"].t[:], [g["mask"]], [maskb])
        P = k.pb[6]
        S.mm(P.t[:, 0:32], k.stri_bf.t[:], maskb.t[:], True, True, [k.stri_bf, maskb], [P])
        S.mm(P.t[:, 32:64], k.ones_bf.t[:], maskb.t[:], True, True, [k.ones_bf, maskb], [P])
        S.tt('dve', g["pos"].t[:], P.t[:, 0:32], cntb.t[:], ALU.add, [P, cntb], [g["pos"]])
        S.tt('dve', cntb.t[:], P.t[:, 32:64], cntb.t[:], ALU.add, [P, cntb], [cntb])
        S.tt('dve', g["val"].t[:], g["pos"].t[:], ecap.t[:], ALU.add, [g["pos"], ecap], [g["val"]])
        for kk in range(2):
            S.tt('dve', g["tmp32"].t[:], g["E"].t[:, kk, :], g["val"].t[:], ALU.mult, [g["E"], g["val"]], [g["tmp32"]])
            S.op('dve', lambda e, kk=kk: e.tensor_reduce(out=g["sk"].t[:, kk:kk + 1], in_=g["tmp32"].t[:], axis=AX.X, op=ALU.add),
                 [g["tmp32"]], [g["sk"]])
            S.tt('dve', g["tmp32"].t[:], g["E"].t[:, kk, :], g["pos"].t[:], ALU.mult, [g["E"], g["pos"]], [g["tmp32"]])
            S.op('dve', lambda e, kk=kk: e.tensor_reduce(out=g["pk"].t[:, kk:kk + 1], in_=g["tmp32"].t[:], axis=AX.X, op=ALU.add),
                 [g["tmp32"]], [g["pk"]])
        S.ts('dve', g["ok"].t[:], g["pk"].t[:], float(CAP) - 0.5, None, ALU.is_lt, None, [g["pk"]], [g["ok"]])
        S.ts('dve', g["sk"].t[:], g["sk"].t[:], -BIGV, None, ALU.add, None, [g["sk"]], [g["sk"]])
        S.tt('dve', g["sk"].t[:], g["sk"].t[:], g["ok"].t[:], ALU.mult, [g["sk"], g["ok"]], [g["sk"]])
        S.ts('dve', g["sk"].t[:], g["sk"].t[:], BIGV, None, ALU.add, None, [g["sk"]], [g["sk"]])
        S.cp('dve', k.slot_i.t[:, t, :], g["sk"].t[:], [g["sk"]], [k.slot_i])
        S.tt('dve', k.wts.t[:, t, :], g["wk"].t[:], g["ok"].t[:], ALU.mult, [g["wk"], g["ok"]], [k.wts])
        for kk in range(2):
            def fn(e, t=t, kk=kk, src=h2b[i2]):
                return e.indirect_dma_start(out=k.XS_d[:, :], out_offset=bass.IndirectOffsetOnAxis(ap=k.slot_i.t[:, t, kk:kk + 1], axis=0),
                                            in_=src.t[:], in_offset=None, bounds_check=k.bcreg(e), oob_is_err=False)
            S.swdma(fn, [h2b[i2], k.slot_i, k.xs_tok], [])
    S.barrier()
    A.release(m0)


def phase_experts(k):
    S, A = k.S, k.A
    CAP, NG = k.CAP, k.NG
    m0 = A.mark()
    stg = [A.alloc(f"estg{i}", [128, 8, 512], F32) for i in range(3)]
    W1b = [A.alloc(f"W1b{i}", [128, 8, 512], BF16) for i in range(2)]
    W3b = [A.alloc(f"W3b{i}", [128, 8, 512], BF16) for i in range(2)]
    W2b = [A.alloc(f"W2b{i}", [128, 4, 1024], BF16) for i in range(2)]
    xs = [A.alloc(f"xs{i}", [128, NG // 128, D], BF16) for i in range(2)]
    xT = [A.alloc(f"xT{i}", [128, 8, NG], BF16) for i in range(2)]
    sl = [A.alloc(f"sl{i}", [128, NG], F32) for i in range(2)]
    G = [A.alloc(f"G{i}", [128, 4, NG], BF16) for i in range(2)]
    yb = [A.alloc(f"yb{i}", [128, D], BF16) for i in range(2)]
    st = {"si": 0, "yi": 0}

    def load_w(e_):
        i2 = e_ % 2
        for (src, dst, kc) in ((k.w1[e_], W1b[i2], 8), (k.w3[e_], W3b[i2], 8)):
            sg = stg[st["si"] % len(stg)]
            st["si"] += 1
            S.dma('sp', sg.t[:], src.rearrange("(c p) n -> p c n", p=128), (), [sg])
            S.cp('pool', dst.t[:], sg.t[:], [sg], [dst])
        for hh in range(2):
            sg = stg[st["si"] % len(stg)]
            st["si"] += 1
            sv = sg.t[:, 0:4, :]
            S.dma('sp', sv, k.w2[e_][:, hh * 512:(hh + 1) * 512].rearrange("(c p) n -> p c n", p=128), (), [sg])
            S.cp('pool', W2b[i2].t[:, :, hh * 512:(hh + 1) * 512], sv, [sg], [W2b[i2]])

    groups = [(e_, gq) for e_ in range(N_EXP) for gq in range(CAP // NG)]

    def load_xs(idx):
        e_, gq = groups[idx]
        r0 = e_ * CAP + gq * NG
        S.dma('sp', xs[idx % 2].t[:], k.XS_d[r0:r0 + NG, :].rearrange("(j p) d -> p j d", p=128), (), [xs[idx % 2]])

    load_w(0)
    load_xs(0)
    for idx, (e_, gq) in enumerate(groups):
        i2 = e_ % 2
        g2 = idx % 2
        r0 = e_ * CAP + gq * NG
        if gq == 0 and e_ + 1 < N_EXP:
            load_w(e_ + 1)
        if idx + 1 < len(groups):
            load_xs(idx + 1)
        for j in range(NG // 128):
            PT_ = k.pb[j % 2]
            pv = k.pbf(j % 2).rearrange("p (a b) -> p a b", a=8)
            for kk in range(8):
                S.tr(pv[:, kk, :], xs[g2].t[:, j, kk * 128:(kk + 1) * 128], k.ident_bf.t[:], [xs[g2], k.ident_bf], [PT_])
            S.cp('act' if j % 2 else 'dve', xT[g2].t[:, :, j * 128:(j + 1) * 128], pv, [PT_], [xT[g2]])
        for f in range(4):
            P1_ = k.pb[2 + (f % 2)]
            P3_ = k.pb[4 + (f % 2)]
            fs = slice(f * 128, (f + 1) * 128)
            for kk in range(8):
                S.mm(P1_.t[:, 0:NG], W1b[i2].t[:, kk, fs], xT[g2].t[:, kk, :], kk == 0, kk == 7, [W1b[i2], xT[g2]], [P1_])
            for kk in range(8):
                S.mm(P3_.t[:, 0:NG], W3b[i2].t[:, kk, fs], xT[g2].t[:, kk, :], kk == 0, kk == 7, [W3b[i2], xT[g2]], [P3_])
            S.act(sl[f % 2].t[:], P1_.t[:, 0:NG], AF.Silu, [P1_], [sl[f % 2]])
            S.tt('dve', G[g2].t[:, f, :], sl[f % 2].t[:], P3_.t[:, 0:NG], ALU.mult, [sl[f % 2], P3_], [G[g2]])
        for j in range(NG // 128):
            y_ = yb[st["yi"] % len(yb)]
            st["yi"] += 1
            for half in range(2):
                P = k.pb[6 + half]
                for f in range(4):
                    S.mm(P.t[:], G[g2].t[:, f, j * 128:(j + 1) * 128], W2b[i2].t[:, f, half * 512:(half + 1) * 512],
                         f == 0, f == 3, [G[g2], W2b[i2]], [P])
                S.cp('act' if half else 'dve', y_.t[:, half * 512:(half + 1) * 512], P.t[:], [P], [y_])
            S.dma('sp', k.YS_d[r0 + j * 128:r0 + (j + 1) * 128, :], y_.t[:], [y_], ())
    S.barrier()
    A.release(m0)


def phase_final(k):
    S, A = k.S, k.A
    NT, NSLOT = k.NT, k.NSLOT
    m0 = A.mark()
    gnf = A.alloc("gnf", [128, D], F32)
    bcast_load(k, gnf, k.normf_g)
    Y = [[A.alloc(f"Y{i}{kk}", [128, D], BF16) for kk in range(2)] for i in range(2)]
    for i in range(2):
        for kk in range(2):
            S.ms('pool', Y[i][kk].t[:], 0.0, [Y[i][kk]])
    x1 = [A.alloc(f"fx1_{i}", [128, D], F32) for i in range(2)]
    moe = A.alloc("moe", [128, D], F32)
    x2 = A.alloc("x2", [128, D], F32)
    junk = A.alloc("fjunk", [128, D], BF16)
    ss = A.alloc("fss", [128, 1], F32)
    sst = A.alloc("fsst", [128, 1], F32)
    rstd = A.alloc("frstd", [128, 1], F32)
    ot = [A.alloc(f"ot{i}", [128, D], F32) for i in range(2)]
    def load_f(t):
        for kk in range(2):
            def fn(e, t=t, kk=kk, dst=Y[t % 2][kk]):
                return e.indirect_dma_start(out=dst.t[:], out_offset=None, in_=k.YS_d[:, :],
                                            in_offset=bass.IndirectOffsetOnAxis(ap=k.slot_i.t[:, t, kk:kk + 1], axis=0),
                                            bounds_check=k.bcreg(e), oob_is_err=False)
            S.swdma(fn, [k.slot_i], [Y[t % 2][kk]])
        S.dma('sp', x1[t % 2].t[:], k.x1_d[t * 128:(t + 1) * 128, :], (), [x1[t % 2]])
    load_f(0)
    for t in range(NT):
        rows = slice(t * 128, (t + 1) * 128)
        i2 = t % 2
        if t + 1 < NT:
            load_f(t + 1)
        S.ts('dve', moe.t[:], Y[i2][0].t[:], k.wts.t[:, t, 0:1], None, ALU.mult, None, [Y[i2][0], k.wts], [moe])
        S.stt('dve', moe.t[:], Y[i2][1].t[:], k.wts.t[:, t, 1:2], moe.t[:], ALU.mult, ALU.add, [Y[i2][1], k.wts, moe], [moe])
        S.tt('dve', moe.t[:], moe.t[:], k.G2b.t[:], ALU.mult, [moe, k.G2b], [moe])
        S.tt('dve', x2.t[:], moe.t[:], x1[i2].t[:], ALU.add, [moe, x1[i2]], [x2])
        S.act(junk.t[:], x2.t[:], AF.Square, [x2], [junk, ss], accum_out=ss.t[:])
        rstd_from_ss(k, ss, sst, rstd, D)
        S.stt('dve', ot[i2].t[:], x2.t[:], rstd.t[:, 0:1], gnf.t[:], ALU.mult, ALU.mult, [x2, rstd, gnf], [ot[i2]])
        S.dma('sp', k.out[rows, :], ot[i2].t[:], [ot[i2]], ())
    S.barrier()
    A.release(m0)


CAP_DEFAULT = 1024


def kernel(**inputs):
    inputs = {kk: np.asarray(v) for kk, v in inputs.items()}
    n = inputs["x"].shape[0]
    s_tok = inputs["x"].shape[1]
    nc = build_nc(s_tok, CAP_DEFAULT)
    in_maps = [make_in_map(inputs, b) for b in range(n)]
    res = run_bass_kernel_spmd(nc, in_maps, core_ids=list(range(n)))
    out = np.stack([np.asarray(r["out"]) for r in res.results], axis=0)
    return out.astype(np.float32)
```

```python
import contextlib
import os
import numpy as np
import concourse.bass as bass
import concourse.mybir as mybir
from concourse.bass_utils import run_bass_kernel_spmd

F32 = mybir.dt.float32
BF16 = mybir.dt.bfloat16
I32 = mybir.dt.int32
U32 = mybir.dt.uint32
AF = mybir.ActivationFunctionType
ALU = mybir.AluOpType
AX = mybir.AxisListType

ENGS = ['pe', 'act', 'dve', 'pool', 'sp']


class Buf:
    __slots__ = ('name', 't', 'w', 'r')

    def __init__(self, name, t=None):
        self.name = name
        self.t = t
        self.w = None
        self.r = {}


class Sched:
    def __init__(self, nc, n_dma_sems=32, same_engine_sync=True):
        self.nc = nc
        self.stack = contextlib.ExitStack()
        self.lists = {e: [] for e in ENGS}
        self.cnt = {e: 0 for e in ENGS}
        self.esem = {e: self.stack.enter_context(nc.semaphore(f"s_{e}")) for e in ['pe', 'act', 'dve', 'pool']}
        self.dsem = [self.stack.enter_context(nc.semaphore(f"d_{i}")) for i in range(n_dma_sems)]
        self.dcnt = [0] * n_dma_sems
        self.dnext = 0
        self.swsem = [self.stack.enter_context(nc.semaphore(f"w_{i}")) for i in range(40)]
        self.swcnt = [0] * 40
        self.swnext = 0
        self.mark_t = self.stack.enter_context(nc.sbuf_tensor("mark_t", [1, 8], F32))
        self.waited = {e: {} for e in ENGS}
        self.same_engine_sync = same_engine_sync
        self.nops = 0

    def sb(self, name, shape, dtype):
        return Buf(name, self.stack.enter_context(self.nc.sbuf_tensor(name, shape, dtype)))

    def ps(self, name, shape, dtype):
        return Buf(name, self.stack.enter_context(self.nc.psum_tensor(name, shape, dtype)))

    def view(self, name, t):
        return Buf(name, t)

    def sem_of(self, k):
        if k[0] == 'e':
            return self.esem[k[1]]
        if k[0] == 'w':
            return self.swsem[k[1]]
        return self.dsem[k[1]]

    def swdma(self, fn, reads=(), writes=()):
        deps = self._deps(reads, writes)
        i = self.swnext
        self.swnext = (i + 1) % len(self.swsem)
        if self.swcnt[i] > 0:
            kk = ('w', i)
            deps[kk] = max(deps.get(kk, 0), 16 * self.swcnt[i])
        ws = self._waits('pool', deps)
        self.swcnt[i] += 1
        tok = (('w', i), 16 * self.swcnt[i])
        self.lists['pool'].append((ws, fn, (self.swsem[i], 16)))
        self._commit(tok, reads, writes)
        self.nops += 1
        return tok

    def _deps(self, reads, writes):
        deps = {}

        def add(k, v):
            if deps.get(k, 0) < v:
                deps[k] = v
        for b in reads:
            if b.w is not None:
                add(*b.w)
        for b in writes:
            if b.w is not None:
                add(*b.w)
            for k, v in b.r.items():
                add(k, v)
        return deps

    def _waits(self, eng, deps):
        ws = []
        for k, v in deps.items():
            if k == ('e', eng) and (eng == 'pe' or not self.same_engine_sync):
                continue
            if self.waited[eng].get(k, 0) >= v:
                continue
            self.waited[eng][k] = v
            ws.append((k, v))
        return ws

    def _commit(self, tok, reads, writes):
        k, v = tok
        for b in reads:
            if b.r.get(k, 0) < v:
                b.r[k] = v
        for b in writes:
            b.w = tok
            b.r = {}

    def op(self, eng, fn, reads=(), writes=()):
        deps = self._deps(reads, writes)
        ws = self._waits(eng, deps)
        self.cnt[eng] += 1
        tok = (('e', eng), self.cnt[eng])
        self.lists[eng].append((ws, fn, (self.esem[eng], 1)))
        self._commit(tok, reads, writes)
        self.nops += 1
        return tok

    def dma(self, eng, out, in_, reads=(), writes=(), fn=None, **kw):
        deps = self._deps(reads, writes)
        i = self.dnext
        self.dnext = (self.dnext + 1) % len(self.dsem)
        if self.dcnt[i] > 0:
            k = ('d', i)
            deps[k] = max(deps.get(k, 0), 16 * self.dcnt[i])
        ws = self._waits(eng, deps)
        self.dcnt[i] += 1
        tok = (('d', i), 16 * self.dcnt[i])
        if fn is None:
            def fn(e, out=out, in_=in_, kw=kw):
                return e.dma_start(out=out, in_=in_, **kw)
        self.lists[eng].append((ws, fn, (self.dsem[i], 16)))
        self._commit(tok, reads, writes)
        self.nops += 1
        return tok

    def finish(self):
        nc = self.nc
        fin = []
        for i in range(len(self.dsem)):
            if self.dcnt[i] > 0 and self.waited['sp'].get(('d', i), 0) < 16 * self.dcnt[i]:
                fin.append((('d', i), 16 * self.dcnt[i]))
        for i in range(len(self.swsem)):
            if self.swcnt[i] > 0:
                fin.append((('w', i), 16 * self.swcnt[i]))
        for e in ['pe', 'act', 'dve', 'pool']:
            if self.cnt[e] > 0:
                fin.append((('e', e), self.cnt[e]))
        self.lists['sp'].append((fin, None, None))

        def replay(name, eng):
            for ent in self.lists[name]:
                ws, fn, inc = ent[0], ent[1], ent[2]
                for k, v in ws:
                    eng.wait_ge(self.sem_of(k), v)
                if len(ent) > 3:
                    for sm_ in ent[3]:
                        eng.sem_clear(sm_)
                if fn is not None:
                    ins = fn(eng)
                    ins.then_inc(inc[0], inc[1])

        with nc.Block() as block:
            @block.tensor
            def _(e):
                replay('pe', e)

            @block.scalar
            def _(e):
                replay('act', e)

            @block.vector
            def _(e):
                replay('dve', e)

            @block.gpsimd
            def _(e):
                replay('pool', e)

            @block.sync
            def _(e):
                replay('sp', e)
        self.stack.close()

    def make_identity(self, ident_bf, ident_f32):
        for b in (ident_bf, ident_f32):
            if b is None:
                continue
            n = b.t.shape[0]
            m = b.t.shape[1]
            self.op('pool', lambda e, b=b: e.memset(b.t[:], 1.0), writes=[b])
            self.op('pool', lambda e, b=b, m=m: e.affine_select(
                out=b.t[:], in_=b.t[:], pattern=[[-1, m]], compare_op=ALU.is_equal,
                fill=0.0, base=0, channel_multiplier=1), reads=[b], writes=[b])

    def barrier(self):
        cur = {}
        for e in ['pe', 'act', 'dve', 'pool']:
            if self.cnt[e] > 0:
                cur[('e', e)] = self.cnt[e]
        for i in range(len(self.dsem)):
            if self.dcnt[i] > 0:
                cur[('d', i)] = 16 * self.dcnt[i]
        for i in range(len(self.swsem)):
            if self.swcnt[i] > 0:
                cur[('w', i)] = 16 * self.swcnt[i]
        for e in ENGS:
            ws = []
            for k, v in cur.items():
                if self.waited[e].get(k, 0) < v:
                    self.waited[e][k] = v
                    ws.append((k, v))
            if ws:
                self.lists[e].append((ws, None, None))

    def tt(self, eng, out, in0, in1, op, R, W):
        return self.op(eng, lambda e: e.tensor_tensor(out, in0, in1, op=op), R, W)

    def ts(self, eng, out, in0, s1, s2, op0, op1, R, W, accum_out=None):
        if accum_out is not None:
            return self.op(eng, lambda e: e.tensor_scalar(out, in0, s1, s2, op0, op1, accum_out=accum_out), R, W)
        if op1 is None:
            return self.op(eng, lambda e: e.tensor_scalar(out, in0, s1, None, op0), R, W)
        return self.op(eng, lambda e: e.tensor_scalar(out, in0, s1, s2, op0, op1), R, W)

    def stt(self, eng, out, in0, sc, in1, op0, op1, R, W):
        return self.op(eng, lambda e: e.scalar_tensor_tensor(out, in0, sc, in1, op0, op1), R, W)

    def act(self, out, in_, func, R, W, bias=None, scale=None, accum_out=None):
        kw = {}
        if bias is not None:
            kw['bias'] = bias
        if scale is not None:
            kw['scale'] = scale
        if accum_out is not None:
            kw['accum_out'] = accum_out
        return self.op('act', lambda e: e.activation(out, in_, func, **kw), R, W)

    def cp(self, eng, out, in_, R, W):
        if eng == 'act':
            return self.op('act', lambda e: e.copy(out, in_), R, W)
        return self.op(eng, lambda e: e.tensor_copy(out, in_), R, W)

    def mm(self, out, lhsT, rhs, start, stop, R, W):
        return self.op('pe', lambda e: e.matmul(out, lhsT, rhs, start=start, stop=stop), R, W)

    def tr(self, out, in_, ident, R, W):
        return self.op('pe', lambda e: e.transpose(out, in_, ident), R, W)

    def ms(self, eng, ap, val, W):
        return self.op(eng, lambda e: e.memset(ap, val), (), W)


class Arena:
    def __init__(self, S, words):
        self.S = S
        self.base = S.stack.enter_context(S.nc.sbuf_tensor("arena", [128, words], F32))
        self.words = words
        self.top = 0

    def mark(self):
        return self.top

    def release(self, m):
        self.top = m

    def alloc(self, name, shape, dtype, parts=128):
        n = 1
        for s in shape[1:]:
            n *= s
        esz = 4 if dtype in (F32, I32, U32) else 2
        w = (n * esz + 3) // 4
        w = (w + 7) // 8 * 8
        assert self.top + w <= self.words, f"arena overflow at {name}: {self.top + w} > {self.words}"
        v = self.base[0:shape[0], self.top:self.top + w]
        if esz == 2:
            v = v.bitcast(BF16)
        elif dtype != F32:
            v = v.bitcast(dtype)
        v = v[:, 0:n]
        if len(shape) == 3:
            v = v.rearrange("p (a b) -> p a b", a=shape[1])
        elif len(shape) == 4:
            v = v.rearrange("p (a b c) -> p a b c", a=shape[1], b=shape[2])
        elif len(shape) == 5:
            v = v.rearrange("p (a b c d) -> p a b c d", a=shape[1], b=shape[2], c=shape[3])
        self.top += w
        return Buf(name, v)


D = 1024
EPS = 1e-6
TWO_PI = 6.283185307179586
C1 = 6.28125
C2 = TWO_PI - C1
MAGIC = 12582912.0
NEG = -1.0e30
N_EXP = 32


class K:
    pass


def build_nc(S_TOK=4096, CAP=1024, debug=False, phases="0123456"):
    nc = bass.Bass("TRN2", target_bir_lowering=False)
    NT = S_TOK // 128
    NBK = S_TOK // 256
    NQB = S_TOK // 512
    NP = 4 * NT
    NSLOT = N_EXP * CAP
    NG = min(CAP, 512)

    def din(name, shape, dt=F32):
        return nc.dram_tensor(name, shape, dt, kind="ExternalInput").ap()

    def dscr(name, shape, dt):
        return nc.dram_tensor(name, shape, dt, kind=("ExternalOutput" if debug else "Internal")).ap()

    x = din("x", [S_TOK, D])
    c_in = din("c", [8, 128])
    pos_in = din("positions", [1, S_TOK], I32)
    w_ada = din("w_ada", [D, 6 * D])
    b_ada = din("b_ada", [1, 6 * D])
    norm1_g = din("norm1_g", [1, D])
    w_in = din("w_in", [D, 3592])
    b_gate = din("b_gate", [8, 1])
    conv_w = din("conv_w", [4, D])
    conv_b = din("conv_b", [1, D])
    attn_g = din("attn_out_g", [1, 512])
    mlstm_g = din("mlstm_out_g", [1, 512])
    w_out = din("w_out", [D, D])
    norm2_g = din("norm2_g", [1, D])
    w_rg = din("w_rg", [D, 4])
    b_rg = din("b_rg", [1, 4])
    w_re = din("w_re", [D, 32])
    b_re = din("b_re", [1, 32])
    w1 = din("w1", [N_EXP, D, 512])
    w3 = din("w3", [N_EXP, D, 512])
    w2 = din("w2", [N_EXP, 512, D])
    normf_g = din("norm_f_g", [1, D])
    cst = din("cst", [128, 8])
    out = nc.dram_tensor("out", [S_TOK, D], F32, kind="ExternalOutput").ap()

    qT_d = dscr("qT_d", [512, S_TOK], BF16)
    kT_d = dscr("kT_d", [512, S_TOK], BF16)
    qkm_d = dscr("qkm_d", [1024, S_TOK], BF16)
    ig_d = dscr("ig_d", [4, S_TOK], F32)
    fg_d = dscr("fg_d", [4, S_TOK], F32)
    vm_d = dscr("vm_d", [S_TOK, 512], BF16)
    om_d = dscr("om_d", [S_TOK, 512], BF16)
    cat_d = dscr("cat_d", [S_TOK, D], BF16)
    x1_d = dscr("x1_d", [S_TOK, D], F32)
    XS_d = dscr("XS_d", [NSLOT, D], BF16)
    YS_d = dscr("YS_d", [NSLOT, D], BF16)

    S = Sched(nc, same_engine_sync=(os.environ.get("SES", "1") == "1"))
    xs_tok = Buf("xs_tok")
    A = Arena(S, 51200)
    pb = [S.ps(f"pb{i}", [128, 512], F32) for i in range(8)]

    def pbf(i):
        return pb[i].t[:].bitcast(BF16)

    ident_bf = A.alloc("ident_bf", [128, 128], BF16)
    ident_f = A.alloc("ident_f", [128, 128], F32)
    ones_f = A.alloc("ones_f", [128, 128], F32)
    ones_bf = A.alloc("ones_bf", [128, 128], BF16)
    tri_bf = A.alloc("tri_bf", [128, 128], BF16)
    stri_bf = A.alloc("stri_bf", [128, 128], BF16)
    cst_sb = A.alloc("cst_sb", [128, 8], F32)
    A1 = A.alloc("A1", [128, D], F32)
    B1 = A.alloc("B1", [128, D], F32)
    G1b = A.alloc("G1b", [128, D], F32)
    A2 = A.alloc("A2", [128, D], F32)
    B2 = A.alloc("B2", [128, D], F32)
    G2b = A.alloc("G2b", [128, D], F32)
    slot_i = A.alloc("slot_i", [128, NT, 2], I32)
    wts = A.alloc("wts", [128, NT, 2], F32)

    S.make_identity(ident_bf, ident_f)
    S.ms('pool', ones_f.t[:], 1.0, [ones_f])
    S.ms('pool', ones_bf.t[:], 1.0, [ones_bf])
    for b_, cmp_ in ((tri_bf, ALU.is_ge), (stri_bf, ALU.is_gt)):
        S.ms('pool', b_.t[:], 1.0, [b_])
        S.op('pool', lambda e, b_=b_, cmp_=cmp_: e.affine_select(
            out=b_.t[:], in_=b_.t[:], pattern=[[1, 128]], compare_op=cmp_,
            fill=0.0, base=0, channel_multiplier=-1), [b_], [b_])
    S.dma('sp', cst_sb.t[:], cst, (), [cst_sb])

    zt = A.alloc("zt", [128, 4, D], BF16)
    S.ms('pool', zt.t[:], 0.0, [zt])
    xs_v = XS_d.rearrange("(n p) d -> p n d", p=128)
    nrow = NSLOT // 128
    for i0 in range(0, nrow, 64):
        nn = min(64, nrow - i0)
        S.dma('sp', xs_v[:, i0:i0 + nn, :], zt.t[:, 0:1, :].broadcast_to([128, nn, D]), [zt], [xs_tok])

    k = K()
    k.__dict__.update(locals())
    k._bcreg = None
    k.dbg_names = []

    def dbg(name, buf, ap, shape, dt):
        if not debug:
            return
        d_ = nc.dram_tensor("dbg_" + name, shape, dt, kind="ExternalOutput").ap()
        S.dma('sp', d_, ap, [buf], ())
        k.dbg_names.append("dbg_" + name)
    k.dbg = dbg

    def bcreg(e):
        if k._bcreg is None:
            k._bcreg = e.to_reg(NSLOT - 1)
        return k._bcreg
    k.bcreg = bcreg
    if '0' in phases:
        phase0_mod(k)
    if '1' in phases:
        phase_p1(k)
    if '2' in phases:
        phase_attn(k)
    if '3' in phases:
        phase_p2(k)
    if '4' in phases:
        phase_mlstm(k)
    if '5' in phases:
        phase_out(k)
    if '6' in phases:
        ph6 = os.environ.get("PH6", "ef")
        if 'e' in ph6:
            phase_experts(k)
        if 'f' in ph6:
            phase_final(k)
    S.finish()
    return nc


def rstd_from_ss(k, ss, tmp, rstd, n, R=()):
    S = k.S
    S.ts('dve', tmp.t[:], ss.t[:], 1.0 / n, EPS, ALU.mult, ALU.add, [ss], [tmp])
    S.act(tmp.t[:], tmp.t[:], AF.Sqrt, [tmp], [tmp])
    S.op('dve', lambda e: e.reciprocal(rstd.t[:], tmp.t[:]), [tmp], [rstd])


def load_w_bf16(k, dst_ap, dstbuf, src_ap, ncols, stg, perm=False, kc=8):
    S = k.S
    st = stg[k.stg_i % len(stg)]
    k.stg_i += 1
    sv = st.t[:, 0:kc, 0:ncols]
    S.dma('sp', sv, src_ap.rearrange("(c p) n -> p c n", p=128), (), [st])
    if not perm:
        S.cp('pool', dst_ap, sv, [st], [dstbuf])
    else:
        nh = ncols // 64
        d5 = dst_ap.rearrange("p c (h two j) -> p c h two j", two=2, j=32)
        s5 = sv.rearrange("p c (h two j) -> p c h two j", two=2, j=32)
        for cc in range(kc):
            S.cp('pool', d5[:, cc, :, 0, :], s5[:, cc, :, 1, :], [st], [dstbuf])
            S.cp('pool', d5[:, cc, :, 1, :], s5[:, cc, :, 0, :], [st], [dstbuf])


def bcast_load(k, dst, src_row):
    k.S.dma('sp', dst.t[:], src_row.partition_broadcast(128), (), [dst])


def phase0_mod(k):
    S, A = k.S, k.A
    m0 = A.mark()
    c8 = A.alloc("c8", [8, 128], F32)
    sc = A.alloc("sc", [128, 8], F32)
    rep = A.alloc("rep", [128, 8, 128], F32)
    brow = A.alloc("brow", [1, 6 * D], F32)
    wst = [A.alloc(f"wst{i}", [128, 8, 512], F32) for i in range(2)]
    modb = A.alloc("modb", [128, 6, D], F32)
    gn1 = A.alloc("gn1", [128, D], F32)
    gn2 = A.alloc("gn2", [128, D], F32)
    S.dma('sp', c8.t[:], k.c_in, (), [c8])
    S.dma('sp', brow.t[:], k.b_ada, (), [brow])
    bcast_load(k, gn1, k.norm1_g)
    bcast_load(k, gn2, k.norm2_g)
    S.act(c8.t[:], c8.t[:], AF.Silu, [c8], [c8])
    S.tr(k.pb[0].t[:, 0:8], c8.t[:], k.ident_f.t[0:8, 0:8], [c8, k.ident_f], [k.pb[0]])
    S.cp('dve', sc.t[:], k.pb[0].t[:, 0:8], [k.pb[0]], [sc])
    for kk in range(8):
        S.ts('dve', rep.t[:, kk, :], k.ones_f.t[:], sc.t[:, kk:kk + 1], None, ALU.mult, None, [k.ones_f, sc], [rep])
    for j in range(12):
        st = wst[j % 2]
        S.dma('sp', st.t[:], k.w_ada[:, j * 512:(j + 1) * 512].rearrange("(c p) n -> p c n", p=128), (), [st])
        P = k.pb[1 + (j % 2)]
        for kk in range(8):
            S.mm(P.t[:], rep.t[:, kk, :], st.t[:, kk, :], kk == 0, False, [rep, st], [P])
        S.mm(P.t[:], k.ones_f.t[0:1, :], brow.t[0:1, j * 512:(j + 1) * 512], False, True, [k.ones_f, brow], [P])
        S.cp('act', modb.t[:, j // 2, (j % 2) * 512:(j % 2 + 1) * 512], P.t[:], [P], [modb])
    S.stt('dve', k.A1.t[:], modb.t[:, 1, :], 1.0, gn1.t[:], ALU.add, ALU.mult, [modb, gn1], [k.A1])
    S.cp('pool', k.B1.t[:], modb.t[:, 0, :], [modb], [k.B1])
    S.cp('pool', k.G1b.t[:], modb.t[:, 2, :], [modb], [k.G1b])
    S.stt('dve', k.A2.t[:], modb.t[:, 4, :], 1.0, gn2.t[:], ALU.add, ALU.mult, [modb, gn2], [k.A2])
    S.cp('pool', k.B2.t[:], modb.t[:, 3, :], [modb], [k.B2])
    S.cp('pool', k.G2b.t[:], modb.t[:, 5, :], [modb], [k.G2b])
    k.dbg("A1", k.A1, k.A1.t[:], [128, D], F32)
    k.dbg("B1", k.B1, k.B1.t[:], [128, D], F32)
    k.dbg("sc", sc, sc.t[:], [128, 8], F32)
    k.dbg("modb", modb, modb.t[:].rearrange("p a b -> p (a b)"), [128, 6 * D], F32)
    S.barrier()
    A.release(m0)


def alloc_hT_tmps(k):
    A = k.A
    k.xt = [A.alloc(f"xt{i}", [128, D], F32) for i in range(2)]
    k.junk = A.alloc("junk", [128, D], BF16)
    k.ss = A.alloc("ss", [128, 1], F32)
    k.sst = A.alloc("sst", [128, 1], F32)
    k.rstd = A.alloc("rstd", [128, 1], F32)
    k.htmp = A.alloc("htmp", [128, D], F32)
    k.hb = [A.alloc(f"hb{i}", [128, D], BF16) for i in range(2)]
    k.hTb = [A.alloc(f"hTb{i}", [128, 8, 512], BF16) for i in range(2)]
    k.xi = 0


def emit_hT_block(k, tb, PTB):
    S = k.S
    hT = k.hTb[tb % 2]
    for j in range(4):
        t = tb * 4 + j
        xt = k.xt[k.xi % 2]
        hb = k.hb[k.xi % 2]
        k.xi += 1
        S.dma('sp', xt.t[:], k.x[t * 128:(t + 1) * 128, :], (), [xt])
        S.act(k.junk.t[:], xt.t[:], AF.Square, [xt], [k.junk, k.ss], accum_out=k.ss.t[:])
        rstd_from_ss(k, k.ss, k.sst, k.rstd, D)
        S.stt('dve', k.htmp.t[:], xt.t[:], k.rstd.t[:, 0:1], k.A1.t[:], ALU.mult, ALU.mult, [xt, k.rstd, k.A1], [k.htmp])
        S.tt('pool', hb.t[:], k.htmp.t[:], k.B1.t[:], ALU.add, [k.htmp, k.B1], [hb])
        P = k.pb[PTB]
        pv = k.pbf(PTB).rearrange("p (a b) -> p a b", a=8)
        for kk in range(8):
            S.tr(pv[:, kk, :], hb.t[:, kk * 128:(kk + 1) * 128], k.ident_bf.t[:], [hb, k.ident_bf], [P])
        S.cp('act', hT.t[:, :, j * 128:(j + 1) * 128], pv, [P], [hT])
    return hT


def phase_p1(k):
    S, A = k.S, k.A
    NT, NQB, S_TOK = k.NT, k.NQB, k.S_TOK
    k.m_v1 = A.mark()
    k.V1 = A.alloc("V1", [128, NT, 8, 65], BF16)
    S.ms('pool', k.V1.t[:], 1.0, [k.V1])
    m0 = A.mark()
    k.stg_i = 0
    stg = [A.alloc(f"stg{i}", [128, 8, 512], F32) for i in range(1)]
    Wq = A.alloc("Wq", [128, 8, 512], BF16)
    Wqp = A.alloc("Wqp", [128, 8, 512], BF16)
    Wk = A.alloc("Wk", [128, 8, 512], BF16)
    Wkp = A.alloc("Wkp", [128, 8, 512], BF16)
    Wv = A.alloc("Wv", [128, 8, 512], BF16)
    load_w_bf16(k, Wq.t[:], Wq, k.w_in[:, 0:512], 512, stg)
    load_w_bf16(k, Wqp.t[:], Wqp, k.w_in[:, 0:512], 512, stg, perm=True)
    load_w_bf16(k, Wk.t[:], Wk, k.w_in[:, 512:1024], 512, stg)
    load_w_bf16(k, Wkp.t[:], Wkp, k.w_in[:, 512:1024], 512, stg, perm=True)
    load_w_bf16(k, Wv.t[:], Wv, k.w_in[:, 1024:1536], 512, stg)
    alloc_hT_tmps(k)
    posi = A.alloc("posi", [128, 512], I32)
    ang = A.alloc("ang", [128, 512], F32)
    a2 = A.alloc("a2", [128, 512], F32)
    kq = A.alloc("kq", [128, 512], F32)
    cosb = A.alloc("cosb", [128, 512], F32)
    sinb = A.alloc("sinb", [128, 512], F32)
    t1 = [A.alloc(f"t1_{i}", [128, 512], F32) for i in range(2)]
    t2 = [A.alloc(f"t2_{i}", [128, 512], F32) for i in range(2)]
    qo = [A.alloc(f"qo{i}", [128, 512], BF16) for i in range(3)]
    invf = k.cst_sb.t[:, 0:1]
    sgn = k.cst_sb.t[:, 1:2]
    oi = 0
    for tb in range(NQB):
        blk = slice(tb * 512, (tb + 1) * 512)
        hT = emit_hT_block(k, tb, 0)
        S.dma('sp', posi.t[:], k.pos_in[0:1, blk].partition_broadcast(128), (), [posi])
        S.cp('dve', ang.t[:], posi.t[:], [posi], [ang])
        S.ts('dve', ang.t[:], ang.t[:], invf, None, ALU.mult, None, [ang, k.cst_sb], [ang])
        for shift, tab, scl in ((0.0, sinb, sgn), (np.pi / 2, cosb, None)):
            S.ts('dve', a2.t[:], ang.t[:], float(shift), None, ALU.add, None, [ang], [a2])
            S.ts('dve', kq.t[:], a2.t[:], 1.0 / TWO_PI, MAGIC, ALU.mult, ALU.add, [a2], [kq])
            S.ts('dve', kq.t[:], kq.t[:], -MAGIC, None, ALU.add, None, [kq], [kq])
            S.stt('dve', a2.t[:], kq.t[:], -C1, a2.t[:], ALU.mult, ALU.add, [kq, a2], [a2])
            S.stt('dve', a2.t[:], kq.t[:], -C2, a2.t[:], ALU.mult, ALU.add, [kq, a2], [a2])
            S.ts('dve', a2.t[:], a2.t[:], float(np.pi), float(-np.pi), ALU.min, ALU.max, [a2], [a2])
            if scl is None:
                S.act(tab.t[:], a2.t[:], AF.Sin, [a2], [tab])
            else:
                S.act(tab.t[:], a2.t[:], AF.Sin, [a2, k.cst_sb], [tab], scale=scl)
        for (W, Wp, dst) in ((Wq, Wqp, k.qT_d), (Wk, Wkp, k.kT_d)):
            for c in range(4):
                P0 = k.pb[1 + 2 * (oi % 2)]
                P1 = k.pb[2 + 2 * (oi % 2)]
                for kk in range(8):
                    S.mm(P0.t[:], W.t[:, kk, c * 128:(c + 1) * 128], hT.t[:, kk, :], kk == 0, kk == 7, [W, hT], [P0])
                for kk in range(8):
                    S.mm(P1.t[:], Wp.t[:, kk, c * 128:(c + 1) * 128], hT.t[:, kk, :], kk == 0, kk == 7, [Wp, hT], [P1])
                ta, tb_ = t1[oi % 2], t2[oi % 2]
                qb_ = qo[oi % 3]
                S.tt('dve', ta.t[:], P0.t[:], cosb.t[:], ALU.mult, [P0, cosb], [ta])
                S.tt('dve', tb_.t[:], P1.t[:], sinb.t[:], ALU.mult, [P1, sinb], [tb_])
                S.tt('pool', qb_.t[:], ta.t[:], tb_.t[:], ALU.add, [ta, tb_], [qb_])
                S.dma('sp', dst[c * 128:(c + 1) * 128, blk], qb_.t[:], [qb_], ())
                oi += 1
        if tb == 0:
            k.dbg("hT", hT, hT.t[:].rearrange("p a b -> p (a b)"), [128, 8 * 512], BF16)
            k.dbg("sinb", sinb, sinb.t[:], [128, 512], F32)
            k.dbg("cosb", cosb, cosb.t[:], [128, 512], F32)
            k.dbg("ang", ang, ang.t[:], [128, 512], F32)
        for j in range(4):
            t = tb * 4 + j
            P = k.pb[5 + (j % 2)]
            for kk in range(8):
                S.mm(P.t[:], hT.t[:, kk, j * 128:(j + 1) * 128], Wv.t[:, kk, :], kk == 0, kk == 7, [hT, Wv], [P])
            S.cp('act', k.V1.t[:, t, :, 0:64], P.t[:].rearrange("p (h d) -> p h d", h=8), [P], [k.V1])
    S.barrier()
    A.release(m0)


def make_consts():
    cst = np.zeros((128, 8), np.float32)
    p = np.arange(128)
    j = (p % 32).astype(np.float32)
    cst[:, 0] = (np.float32(10000.0) ** (-j / np.float32(32.0))).astype(np.float32)
    cst[:, 1] = np.where((p % 64) < 32, -1.0, 1.0)
    return cst


def make_in_map(inputs, b):
    m = {
        "x": np.ascontiguousarray(inputs["x"][b]),
        "c": np.ascontiguousarray(inputs["c"][b].reshape(8, 128)),
        "positions": np.ascontiguousarray(inputs["positions"][b].reshape(1, -1)).astype(np.int32),
        "w_ada": np.ascontiguousarray(inputs["w_ada"][0]),
        "b_ada": np.ascontiguousarray(inputs["b_ada"][0].reshape(1, -1)),
        "norm1_g": np.ascontiguousarray(inputs["norm1_g"][0].reshape(1, -1)),
        "w_in": np.ascontiguousarray(inputs["w_in"][0]),
        "b_gate": np.ascontiguousarray(inputs["b_gate"][0].reshape(8, 1)),
        "conv_w": np.ascontiguousarray(inputs["conv_w"][0]),
        "conv_b": np.ascontiguousarray(inputs["conv_b"][0].reshape(1, -1)),
        "attn_out_g": np.ascontiguousarray(inputs["attn_out_g"][0].reshape(1, -1)),
        "mlstm_out_g": np.ascontiguousarray(inputs["mlstm_out_g"][0].reshape(1, -1)),
        "w_out": np.ascontiguousarray(inputs["w_out"][0]),
        "norm2_g": np.ascontiguousarray(inputs["norm2_g"][0].reshape(1, -1)),
        "w_rg": np.ascontiguousarray(inputs["w_rg"][0]),
        "b_rg": np.ascontiguousarray(inputs["b_rg"][0].reshape(1, -1)),
        "w_re": np.ascontiguousarray(inputs["w_re"][0]),
        "b_re": np.ascontiguousarray(inputs["b_re"][0].reshape(1, -1)),
        "w1": np.ascontiguousarray(inputs["w1"][0]),
        "w3": np.ascontiguousarray(inputs["w3"][0]),
        "w2": np.ascontiguousarray(inputs["w2"][0]),
        "norm_f_g": np.ascontiguousarray(inputs["norm_f_g"].reshape(1, -1)),
        "cst": make_consts(),
    }
    return m


def phase_attn(k):
    S, A = k.S, k.A
    NT, NQB, NBK, S_TOK = k.NT, k.NQB, k.NBK, k.S_TOK
    qT = A.alloc("qT", [128, 4, S_TOK], BF16)
    kT = A.alloc("kT", [128, 4, S_TOK], BF16)
    for c in range(4):
        S.dma('sp', qT.t[:, c, :], k.qT_d[c * 128:(c + 1) * 128, :], (), [qT])
        S.dma('sp', kT.t[:, c, :], k.kT_d[c * 128:(c + 1) * 128, :], (), [kT])
    ksum = A.alloc("ksum", [128, 4, 16], F32)
    kmT = A.alloc("kmT", [128, 4, 16], BF16)
    S.ms('dve', ksum.t[:], 0.0, [ksum])
    for c in range(4):
        S.op('dve', lambda e, c=c: e.tensor_reduce(out=ksum.t[:, c, 0:NBK], in_=kT.t[:, c, :].rearrange("p (n s) -> p n s", s=256),
                                                   axis=AX.X, op=ALU.add), [kT], [ksum])
    S.ts('dve', kmT.t[:], ksum.t[:], 1.0 / 256.0, None, ALU.mult, None, [ksum], [kmT])
    LV = int(os.environ.get("ATT_LEVEL", "9"))
    if LV <= 1:
        S.barrier(); A.release(k.m_v1); return
    zer = A.alloc("zer", [128, 16, 16], F32)
    pastb = A.alloc("pastb", [128, 16, 16], F32)
    pastm = A.alloc("pastm", [128, 16, 16], F32)
    ownm = A.alloc("ownm", [128, 16, 16], F32)
    onesm = A.alloc("onesm", [128, 16, 16], F32)
    S.ms('pool', zer.t[:], 0.0, [zer])
    S.ms('pool', onesm.t[:], 1.0, [onesm])
    pat = [[1, 16], [-1, 16]]
    S.op('pool', lambda e: e.affine_select(out=pastb.t[:], in_=zer.t[:], pattern=pat, compare_op=ALU.is_gt,
                                           fill=NEG, base=0, channel_multiplier=0), [zer], [pastb])
    S.op('pool', lambda e: e.affine_select(out=pastm.t[:], in_=onesm.t[:], pattern=pat, compare_op=ALU.is_gt,
                                           fill=0.0, base=0, channel_multiplier=0), [onesm], [pastm])
    S.op('pool', lambda e: e.affine_select(out=ownm.t[:], in_=onesm.t[:], pattern=pat, compare_op=ALU.is_equal,
                                           fill=0.0, base=0, channel_multiplier=0), [onesm], [ownm])
    if LV <= 2:
        S.barrier(); A.release(k.m_v1); return
    selfull = A.alloc("selfull", [128, NT, 8, 16], F32)
    gm = A.alloc("gm", [128, 8, 16], F32)
    top8 = A.alloc("top8", [128, 8, 8], F32)
    selt = A.alloc("selt", [128, 8, 16], F32)
    PG = [k.pb[7], k.pb[6]]
    pg = [PG[hp].t[:, 0:64].rearrange("p (c n) -> p c n", c=4) for hp in range(2)]
    gmv = gm.t[:].rearrange("p (c two) n -> p c two n", two=2)
    m8 = A.alloc("m8", [128, 8], F32)
    g2 = A.alloc("g2", [128, 8, 16], F32)
    for t in range(NT):
        jb = t // 2
        for h in range(8):
            c, hp = h // 2, h % 2
            rows = slice(hp * 64, hp * 64 + 64)
            S.mm(pg[hp][:, c, :], qT.t[rows, c, t * 128:(t + 1) * 128], kmT.t[rows, c, :], True, True, [qT, kmT], [PG[hp]])
        for hp in range(2):
            S.tt('dve', gmv[:, :, hp, :], pg[hp], pastb.t[:, jb:jb + 1, :].broadcast_to([128, 4, 16]), ALU.add,
                 [PG[hp], pastb], [gm])
        src = gm
        for rnd in range(2):
            S.op('dve', lambda e, src=src: e.tensor_reduce(out=m8.t[:], in_=src.t[:], axis=AX.X, op=ALU.max), [src], [m8])
            S.tt('dve', selt.t[:], src.t[:], m8.t[:].unsqueeze(2).broadcast_to([128, 8, 16]), ALU.is_ge, [src, m8], [selt])
            S.stt('dve', g2.t[:], selt.t[:], NEG, src.t[:], ALU.mult, ALU.add, [selt, src], [g2])
            src = g2
        S.op('dve', lambda e: e.tensor_reduce(out=m8.t[:], in_=g2.t[:], axis=AX.X, op=ALU.max), [g2], [m8])
        S.tt('dve', selt.t[:], gm.t[:], m8.t[:].unsqueeze(2).broadcast_to([128, 8, 16]), ALU.is_ge, [gm, m8], [selt])
        S.tt('dve', selt.t[:], selt.t[:], pastm.t[:, jb:jb + 1, :].broadcast_to([128, 8, 16]), ALU.mult, [selt, pastm], [selt])
        S.tt('dve', selfull.t[:, t, :, :], selt.t[:], ownm.t[:, jb:jb + 1, :].broadcast_to([128, 8, 16]), ALU.add,
             [selt, ownm], [selfull])
    if LV <= 3:
        S.barrier(); A.release(k.m_v1); return
    PT = [A.alloc(f"PT{i}", [128, 512], BF16) for i in range(4)]
    acc = [A.alloc(f"acc{i}", [128, 4, 65], F32) for i in range(2)]
    rden = A.alloc("rden", [128, 4], F32)
    o_n = A.alloc("o_n", [128, 4, 64], F32)
    osq = A.alloc("osq", [128, 4, 64], F32)
    ss4 = A.alloc("ss4", [128, 4], F32)
    ss4t = A.alloc("ss4t", [128, 4], F32)
    rs4 = A.alloc("rs4", [128, 4], F32)
    gattn = A.alloc("gattn", [128, 512], F32)
    bcast_load(k, gattn, k.attn_g)
    attn_sb = A.alloc("attn_sb", [128, NT, 512], BF16)
    it = 0
    pti = 0
    atmp = [A.alloc(f"atmp{i}", [128, 4, 65], F32) for i in range(2)]
    pon = 0
    for h in range(8):
        c, hp = h // 2, h % 2
        rows = slice(hp * 64, hp * 64 + 64)
        for qb in range(NQB):
            ac = acc[it % 2]
            it += 1
            S.ms('pool', ac.t[:], 0.0, [ac])
            for n in range(2 * qb + 2):
                PO = k.pb[6 + (pon % 2)]
                pon += 1
                po = PO.t[:, 0:260].rearrange("p (j d) -> p j d", j=4)
                pts = {}
                for kc in (2 * n, 2 * n + 1):
                    jmin = max(0, kc - 4 * qb)
                    if jmin > 3:
                        continue
                    PS = k.pb[3 * hp + (pti % 3)]
                    pt = PT[pti % 3]
                    pti += 1
                    cols = slice(jmin * 128, 512)
                    S.mm(PS.t[:, cols], kT.t[rows, c, kc * 128:(kc + 1) * 128],
                         qT.t[rows, c, qb * 512 + jmin * 128:(qb + 1) * 512], True, True, [kT, qT], [PS])
                    S.act(pt.t[:, cols], PS.t[:, cols], AF.Exp, [PS], [pt], scale=0.125)
                    jd = kc - 4 * qb
                    if 0 <= jd <= 3:
                        S.tt('pool', pt.t[:, jd * 128:(jd + 1) * 128], pt.t[:, jd * 128:(jd + 1) * 128], k.tri_bf.t[:],
                             ALU.mult, [pt, k.tri_bf], [pt])
                    pts[kc] = pt
                full = (n <= 2 * qb)
                for j in range(4):
                    qt = 4 * qb + j
                    if n > qt // 2:
                        continue
                    chunks = [kc for kc in (2 * n, 2 * n + 1) if kc <= qt]
                    for idx, kc in enumerate(chunks):
                        S.mm(po[:, j, :], pts[kc].t[:, j * 128:(j + 1) * 128], k.V1.t[:, kc, h, :],
                             idx == 0, idx == len(chunks) - 1, [pts[kc], k.V1], [PO])
                    if not full:
                        S.stt('dve', ac.t[:, j, :], po[:, j, :], selfull.t[:, qt, h, n:n + 1], ac.t[:, j, :],
                              ALU.mult, ALU.add, [PO, selfull, ac], [ac])
                if full:
                    tm = atmp[pon % 2]
                    S.tt('dve', tm.t[:], po, selfull.t[:, 4 * qb:4 * qb + 4, h, n:n + 1].broadcast_to([128, 4, 65]),
                         ALU.mult, [PO, selfull], [tm])
                    S.tt('pool', ac.t[:], ac.t[:], tm.t[:], ALU.add, [ac, tm], [ac])
            S.op('dve', lambda e, ac=ac: e.reciprocal(rden.t[:], ac.t[:, :, 64]), [ac], [rden])
            S.tt('dve', o_n.t[:], ac.t[:, :, 0:64], rden.t[:].unsqueeze(2).broadcast_to([128, 4, 64]), ALU.mult, [ac, rden], [o_n])
            S.tt('pool', osq.t[:], o_n.t[:], o_n.t[:], ALU.mult, [o_n], [osq])
            S.op('dve', lambda e: e.tensor_reduce(out=ss4.t[:], in_=osq.t[:], axis=AX.X, op=ALU.add), [osq], [ss4])
            rstd_from_ss(k, ss4, ss4t, rs4, 64)
            S.tt('dve', o_n.t[:], o_n.t[:], rs4.t[:].unsqueeze(2).broadcast_to([128, 4, 64]), ALU.mult, [o_n, rs4], [o_n])
            S.tt('dve', attn_sb.t[:, qb * 4:(qb + 1) * 4, h * 64:(h + 1) * 64], o_n.t[:],
                 gattn.t[:, h * 64:(h + 1) * 64].unsqueeze(1).broadcast_to([128, 4, 64]), ALU.mult, [o_n, gattn], [attn_sb])
    S.dma('sp', k.cat_d[:, 0:512].rearrange("(t p) d -> p t d", p=128), attn_sb.t[:], [attn_sb], ())
    S.barrier()
    A.release(k.m_v1)


def phase_p2(k):
    S, A = k.S, k.A
    NT, NQB = k.NT, k.NQB
    m0 = A.mark()
    k.stg_i = 0
    stg = [A.alloc(f"stg{i}", [128, 8, 512], F32) for i in range(2)]
    Wqkm = A.alloc("Wqkm", [128, 8, 1024], BF16)
    Wvm = A.alloc("Wvm", [128, 8, 512], BF16)
    Wom = A.alloc("Wom", [128, 8, 512], BF16)
    Wg = A.alloc("Wg", [128, 8, 8], BF16)
    load_w_bf16(k, Wqkm.t[:, :, 0:512], Wqkm, k.w_in[:, 1536:2048], 512, stg)
    load_w_bf16(k, Wqkm.t[:, :, 512:1024], Wqkm, k.w_in[:, 2048:2560], 512, stg)
    load_w_bf16(k, Wvm.t[:], Wvm, k.w_in[:, 2560:3072], 512, stg)
    load_w_bf16(k, Wom.t[:], Wom, k.w_in[:, 3072:3584], 512, stg)
    load_w_bf16(k, Wg.t[:], Wg, k.w_in[:, 3584:3592], 8, stg)
    alloc_hT_tmps(k)
    qo = [A.alloc(f"qo{i}", [128, 512], BF16) for i in range(3)]
    gsb = [A.alloc(f"gsb{i}", [4, 512], F32) for i in range(2)]
    oi = 0
    for tb in range(NQB):
        blk = slice(tb * 512, (tb + 1) * 512)
        hT = emit_hT_block(k, tb, 0)
        for c in range(8):
            P = k.pb[1 + (oi % 2)]
            for kk in range(8):
                S.mm(P.t[:], Wqkm.t[:, kk, c * 128:(c + 1) * 128], hT.t[:, kk, :], kk == 0, kk == 7, [Wqkm, hT], [P])
            q_ = qo[oi % 3]
            S.cp('act' if oi % 2 else 'dve', q_.t[:], P.t[:], [P], [q_])
            S.dma('sp', k.qkm_d[c * 128:(c + 1) * 128, blk], q_.t[:], [q_], ())
            oi += 1
        for gi, dst in ((0, k.ig_d), (1, k.fg_d)):
            P = k.pb[3 + gi]
            for kk in range(8):
                S.mm(P.t[0:4, :], Wg.t[:, kk, gi * 4:(gi + 1) * 4], hT.t[:, kk, :], kk == 0, kk == 7, [Wg, hT], [P])
            S.cp('dve', gsb[gi].t[:], P.t[0:4, :], [P], [gsb[gi]])
            S.dma('sp', dst[:, blk], gsb[gi].t[:], [gsb[gi]], ())
        for j in range(4):
            t = tb * 4 + j
            for wi, (W, dst) in enumerate(((Wvm, k.vm_d), (Wom, k.om_d))):
                P = k.pb[5 + wi]
                for kk in range(8):
                    S.mm(P.t[:], hT.t[:, kk, j * 128:(j + 1) * 128], W.t[:, kk, :], kk == 0, kk == 7, [hT, W], [P])
                q_ = qo[oi % 3]
                S.cp('act' if wi else 'dve', q_.t[:], P.t[:], [P], [q_])
                S.dma('sp', dst[t * 128:(t + 1) * 128, :], q_.t[:], [q_], ())
                oi += 1
    S.barrier()
    A.release(m0)


def phase_mlstm(k):
    S, A = k.S, k.A
    NT, S_TOK, NP = k.NT, k.S_TOK, k.NP
    m0 = A.mark()
    KSC = float(128.0 ** -0.5)
    cw5 = A.alloc("cw5", [5, D], F32)
    cwT = A.alloc("cwT", [128, 8, 5], F32)
    S.dma('sp', cw5.t[0:4, :], k.conv_w, (), [cw5])
    S.dma('sp', cw5.t[4:5, :], k.conv_b, (), [cw5])
    for fc in range(8):
        P = k.pb[fc % 2]
        S.tr(P.t[:, 0:5], cw5.t[:, fc * 128:(fc + 1) * 128], k.ident_f.t[0:5, 0:5], [cw5, k.ident_f], [P])
        S.cp('dve', cwT.t[:, fc, :], P.t[:, 0:5], [P], [cwT])
    qmT = A.alloc("qmT", [128, 4, S_TOK], BF16)
    kmT = A.alloc("kmT2", [128, 4, S_TOK], BF16)
    mc = A.mark()
    raw = [A.alloc(f"raw{i}", [128, 3 + S_TOK], BF16) for i in range(2)]
    cacc = A.alloc("cacc", [128, S_TOK], F32)
    for fc in range(8):
        rw = raw[fc % 2]
        S.ms('pool', rw.t[:, 0:3], 0.0, [rw])
        S.dma('sp', rw.t[:, 3:3 + S_TOK], k.qkm_d[fc * 128:(fc + 1) * 128, :], (), [rw])
        S.ts('dve', cacc.t[:], rw.t[:, 0:S_TOK], cwT.t[:, fc, 0:1], cwT.t[:, fc, 4:5], ALU.mult, ALU.add, [rw, cwT], [cacc])
        for j in range(1, 4):
            S.stt('dve', cacc.t[:], rw.t[:, j:j + S_TOK], cwT.t[:, fc, j:j + 1], cacc.t[:], ALU.mult, ALU.add,
                  [rw, cwT, cacc], [cacc])
        dst = qmT if fc < 4 else kmT
        S.act(dst.t[:, fc % 4, :], cacc.t[:], AF.Silu, [cacc], [dst])
    S.barrier()
    A.release(mc)
    def g_(name, shape=None):
        return A.alloc(name, shape or [NP, 128], F32)
    ig, fg, cs, u, cmu, r, tq, wint, enm, wz, eu, er = [g_(n) for n in
        ("ig", "fg", "cs", "u", "cmu", "r", "tq", "wint", "enm", "wz", "eu", "er")]
    bcol = g_("bcol", [NP, 2])
    nbf, acol, mloc, mcol, mpcol, amm = [g_(n, [NP, 1]) for n in ("nbf", "acol", "mloc", "mcol", "mpcol", "amm")]
    arow, mlrow, mrow, mprow, sprow = [g_(n, [1, NP]) for n in ("arow", "mlrow", "mrow", "mprow", "sprow")]
    sprev_b = A.alloc("sprev_b", [128, NP], F32)
    tmq = A.alloc("tmq", [128, 5, NP], F32)
    onesn = k.ones_f.t[0:NP, :]
    idn = k.ident_f.t[0:NP, 0:NP]
    S.dma('sp', ig.t[:], k.ig_d.rearrange("h (c l) -> (h c) l", l=128), (), [ig])
    S.dma('sp', fg.t[:], k.fg_d.rearrange("h (c l) -> (h c) l", l=128), (), [fg])
    for h in range(4):
        S.dma('sp', bcol.t[h * NT:(h + 1) * NT, 0:1], k.b_gate[h:h + 1, :].partition_broadcast(NT), (), [bcol])
        S.dma('sp', bcol.t[h * NT:(h + 1) * NT, 1:2], k.b_gate[4 + h:5 + h, :].partition_broadcast(NT), (), [bcol])
    S.ts('dve', nbf.t[:], bcol.t[:, 1:2], -1.0, None, ALU.mult, None, [bcol], [nbf])
    S.act(fg.t[:], fg.t[:], AF.Exp, [fg, nbf], [fg], bias=nbf.t[:, 0:1], scale=-1.0)
    S.act(fg.t[:], fg.t[:], AF.Ln, [fg], [fg], bias=1.0)
    S.op('dve', lambda e: e.tensor_tensor_scan(cs.t[:], onesn, fg.t[:], 0.0, ALU.mult, ALU.add), [fg, k.ones_f], [cs])
    S.ts('dve', ig.t[:], ig.t[:], bcol.t[:, 0:1], None, ALU.add, None, [ig, bcol], [ig])
    S.tt('dve', u.t[:], ig.t[:], cs.t[:], ALU.add, [ig, cs], [u])
    S.op('dve', lambda e: e.tensor_tensor_scan(cmu.t[:], onesn, u.t[:], NEG, ALU.mult, ALU.max), [u, k.ones_f], [cmu])
    S.ts('dve', acol.t[:], cs.t[:, 127:128], -1.0, None, ALU.mult, None, [cs], [acol])
    S.tt('dve', mloc.t[:], acol.t[:], cmu.t[:, 127:128], ALU.add, [acol, cmu], [mloc])
    P = k.pb[0]
    S.tr(P.t[0:1, 0:NP], acol.t[:], idn, [acol, k.ident_f], [P])
    S.cp('dve', arow.t[:], P.t[0:1, 0:NP], [P], [arow])
    P = k.pb[1]
    S.tr(P.t[0:1, 0:NP], mloc.t[:], idn, [mloc, k.ident_f], [P])
    S.cp('dve', mlrow.t[:], P.t[0:1, 0:NP], [P], [mlrow])
    S.ms('dve', mprow.t[:], 0.0, [mprow])
    for h in range(4):
        sl = slice(h * NT, (h + 1) * NT)
        S.op('dve', lambda e, sl=sl: e.tensor_tensor_scan(mrow.t[:, sl], arow.t[:, sl], mlrow.t[:, sl], 0.0, ALU.add, ALU.max),
             [arow, mlrow], [mrow])
        if NT > 1:
            S.cp('dve', mprow.t[:, h * NT + 1:(h + 1) * NT], mrow.t[:, h * NT:(h + 1) * NT - 1], [mrow], [mprow])
    S.tt('dve', sprow.t[:], arow.t[:], mprow.t[:], ALU.add, [arow, mprow], [sprow])
    S.tt('dve', sprow.t[:], sprow.t[:], mrow.t[:], ALU.subtract, [sprow, mrow], [sprow])
    S.act(sprow.t[:], sprow.t[:], AF.Exp, [sprow], [sprow])
    P = k.pb[2]
    S.mm(P.t[:, 0:NP], k.ones_f.t[0:1, :], sprow.t[:], True, True, [k.ones_f, sprow], [P])
    S.cp('dve', sprev_b.t[:], P.t[:, 0:NP], [P], [sprev_b])
    P = k.pb[3]
    S.mm(P.t[0:NP, 0:1], mrow.t[:], k.ones_f.t[0:1, 0:1], True, True, [mrow, k.ones_f], [P])
    S.cp('dve', mcol.t[:], P.t[0:NP, 0:1], [P], [mcol])
    P = k.pb[4]
    S.mm(P.t[0:NP, 0:1], mprow.t[:], k.ones_f.t[0:1, 0:1], True, True, [mprow, k.ones_f], [P])
    S.cp('dve', mpcol.t[:], P.t[0:NP, 0:1], [P], [mpcol])
    S.ts('dve', r.t[:], cmu.t[:], mpcol.t[:, 0:1], -1.0, ALU.max, ALU.mult, [cmu, mpcol], [r])
    S.act(wint.t[:], r.t[:], AF.Exp, [r, mpcol], [wint], bias=mpcol.t[:, 0:1])
    S.tt('dve', tq.t[:], r.t[:], cs.t[:], ALU.add, [r, cs], [tq])
    S.act(enm.t[:], tq.t[:], AF.Exp, [tq], [enm])
    S.tt('dve', amm.t[:], acol.t[:], mcol.t[:], ALU.subtract, [acol, mcol], [amm])
    S.ts('dve', amm.t[:], amm.t[:], float(np.log(KSC)), None, ALU.add, None, [amm], [amm])
    S.act(wz.t[:], u.t[:], AF.Exp, [u, amm], [wz], bias=amm.t[:, 0:1])
    S.act(eu.t[:], u.t[:], AF.Exp, [u], [eu])
    S.act(er.t[:], r.t[:], AF.Exp, [r], [er])
    for qi, Q in enumerate((eu, er, wint, enm, wz)):
        P = k.pb[5 + (qi % 2)]
        S.tr(P.t[:, 0:NP], Q.t[:], idn, [Q, k.ident_f], [P])
        S.cp('dve', tmq.t[:, qi, :], P.t[:, 0:NP], [P], [tmq])
    tri_s = A.alloc("tri_s", [128, 128], F32)
    S.ts('dve', tri_s.t[:], k.tri_bf.t[:], KSC, None, ALU.mult, None, [k.tri_bf], [tri_s])
    vaug = A.alloc("vaug", [128, NT, 4, 129], BF16)
    S.ms('pool', vaug.t[:], 1.0, [vaug])
    for h in range(4):
        S.dma('sp', vaug.t[:, :, h, 0:128], k.vm_d[:, h * 128:(h + 1) * 128].rearrange("(c p) d -> p c d", p=128), (), [vaug])
    CT = [A.alloc(f"CT{h}", [128, 129], F32) for h in range(4)]
    CTb = [A.alloc(f"CTb{h}", [128, 129], BF16) for h in range(4)]
    for h in range(4):
        S.ms('pool', CT[h].t[:], 0.0, [CT[h]])
        S.ms('pool', CTb[h].t[:], 0.0, [CTb[h]])
    gml = A.alloc("gml", [128, 512], F32)
    bcast_load(k, gml, k.mlstm_g)
    Am = [A.alloc(f"Am{i}", [128, 128], BF16) for i in range(2)]
    vu = [A.alloc(f"vu{i}", [128, 129], BF16) for i in range(2)]
    wv = [A.alloc(f"wv{i}", [128, 129], BF16) for i in range(2)]
    ktm = [A.alloc(f"ktm{i}", [128, 128], BF16) for i in range(2)]
    inter = [A.alloc(f"inter{i}", [128, 129], F32) for i in range(2)]
    tot = [A.alloc(f"tot{i}", [128, 129], F32) for i in range(2)]
    den = A.alloc("den", [128, 1], F32)
    rdn = A.alloc("rdn", [128, 1], F32)
    hmraw = [A.alloc(f"hmraw{i}", [128, 4, 128], F32) for i in range(2)]
    hsq = A.alloc("hsq", [128, 4, 128], F32)
    ss4 = A.alloc("mss4", [128, 4], F32)
    ss4t = A.alloc("mss4t", [128, 4], F32)
    rs4 = A.alloc("mrs4", [128, 4], F32)
    omt = [A.alloc(f"omt{i}", [128, 512], BF16) for i in range(2)]
    sgm = A.alloc("sgm", [128, 512], F32)
    hout = [A.alloc(f"hout{i}", [128, 512], BF16) for i in range(2)]
    it = 0
    for c in range(NT):
        ck = slice(c * 128, (c + 1) * 128)
        hr = hmraw[c % 2]
        S.dma('sp', omt[c % 2].t[:], k.om_d[ck, :], (), [omt[c % 2]])
        for h in range(4):
            hc = h * NT + c
            i2 = it % 2
            it += 1
            PA = k.pb[0 + i2]
            S.mm(PA.t[:, 0:128], kmT.t[:, h, ck], qmT.t[:, h, ck], True, True, [kmT, qmT], [PA])
            S.tt('dve', Am[i2].t[:], PA.t[:, 0:128], tri_s.t[:], ALU.mult, [PA, tri_s], [Am[i2]])
            S.ts('pool', vu[i2].t[:], vaug.t[:, c, h, :], tmq.t[:, 0, hc:hc + 1], None, ALU.mult, None, [vaug, tmq], [vu[i2]])
            PI = k.pb[2 + i2]
            S.mm(PI.t[:, 0:129], Am[i2].t[:], vu[i2].t[:], True, True, [Am[i2], vu[i2]], [PI])
            PN = k.pb[4 + i2]
            S.mm(PN.t[:, 0:129], qmT.t[:, h, ck], CTb[h].t[:], True, True, [qmT, CTb[h]], [PN])
            S.act(inter[i2].t[:], PN.t[:, 0:129], AF.Copy, [PN, tmq], [inter[i2]], scale=tmq.t[:, 2, hc:hc + 1])
            S.stt('dve', tot[i2].t[:], PI.t[:, 0:129], tmq.t[:, 1, hc:hc + 1], inter[i2].t[:], ALU.mult, ALU.add,
                  [PI, tmq, inter[i2]], [tot[i2]])
            S.ts('dve', rdn.t[:], tot[i2].t[:, 128:129], -1.0, None, ALU.mult, None, [tot[i2]], [rdn])
            S.stt('dve', den.t[:], tot[i2].t[:, 128:129], rdn.t[:, 0:1], tmq.t[:, 3, hc:hc + 1], ALU.max, ALU.max,
                  [tot[i2], rdn, tmq], [den])
            S.op('dve', lambda e: e.reciprocal(rdn.t[:], den.t[:]), [den], [rdn])
            S.act(hr.t[:, h, :], tot[i2].t[:, 0:128], AF.Copy, [tot[i2], rdn], [hr], scale=rdn.t[:, 0:1])
            if c < NT - 1:
                S.ts('pool', wv[i2].t[:], vaug.t[:, c, h, :], tmq.t[:, 4, hc:hc + 1], None, ALU.mult, None, [vaug, tmq], [wv[i2]])
                PK = k.pb[7]
                pk = k.pbf(7)[:, 0:128]
                S.tr(pk, kmT.t[:, h, ck], k.ident_bf.t[:], [kmT, k.ident_bf], [PK])
                S.cp('act', ktm[i2].t[:], pk, [PK], [ktm[i2]])
                PC = k.pb[6]
                S.mm(PC.t[:, 0:129], ktm[i2].t[:], wv[i2].t[:], True, True, [ktm[i2], wv[i2]], [PC])
                S.stt('dve', CT[h].t[:], CT[h].t[:], sprev_b.t[:, hc:hc + 1], PC.t[:, 0:129], ALU.mult, ALU.add,
                      [CT[h], sprev_b, PC], [CT[h]])
                S.cp('act', CTb[h].t[:], CT[h].t[:], [CT[h]], [CTb[h]])
        om = omt[c % 2]
        ho = hout[c % 2]
        S.tt('pool', hsq.t[:], hr.t[:], hr.t[:], ALU.mult, [hr], [hsq])
        S.op('dve', lambda e: e.tensor_reduce(out=ss4.t[:], in_=hsq.t[:], axis=AX.X, op=ALU.add), [hsq], [ss4])
        rstd_from_ss(k, ss4, ss4t, rs4, 128)
        S.tt('dve', hr.t[:], hr.t[:], rs4.t[:].unsqueeze(2).broadcast_to([128, 4, 128]), ALU.mult, [hr, rs4], [hr])
        hr2 = hr.t[:].rearrange("p h d -> p (h d)")
        S.tt('pool', hr2, hr2, gml.t[:], ALU.mult, [hr, gml], [hr])
        S.act(sgm.t[:], om.t[:], AF.Sigmoid, [om], [sgm])
        S.tt('dve', ho.t[:], hr2, sgm.t[:], ALU.mult, [hr, sgm], [ho])
        S.dma('sp', k.cat_d[ck, 512:1024], ho.t[:], [ho], ())
    S.barrier()
    A.release(m0)


def phase_out(k):
    S, A = k.S, k.A
    NT, CAP, NSLOT = k.NT, k.CAP, k.NSLOT
    m0 = A.mark()
    k.stg_i = 0
    stg = [A.alloc(f"stg{i}", [128, 8, 512], F32) for i in range(2)]
    Wout = A.alloc("Wout", [128, 8, 1024], BF16)
    load_w_bf16(k, Wout.t[:, :, 0:512], Wout, k.w_out[:, 0:512], 512, stg)
    load_w_bf16(k, Wout.t[:, :, 512:1024], Wout, k.w_out[:, 512:1024], 512, stg)
    Wr = A.alloc("Wr", [128, 8, 36], F32)
    S.dma('sp', Wr.t[:, :, 0:4], k.w_rg.rearrange("(c p) n -> p c n", p=128), (), [Wr])
    S.dma('sp', Wr.t[:, :, 4:36], k.w_re.rearrange("(c p) n -> p c n", p=128), (), [Wr])
    brow = A.alloc("brow_r", [1, 36], F32)
    S.dma('sp', brow.t[:, 0:4], k.b_rg, (), [brow])
    S.dma('sp', brow.t[:, 4:36], k.b_re, (), [brow])
    ecap = A.alloc("ecap", [128, 32], F32)
    S.op('pool', lambda e: e.iota(ecap.t[:], pattern=[[CAP, 32]], base=0, channel_multiplier=0,
                                  allow_small_or_imprecise_dtypes=True), (), [ecap])
    cntb = A.alloc("cntb", [128, 32], F32)
    S.ms('pool', cntb.t[:], 0.0, [cntb])
    catt = [A.alloc(f"catt{i}", [128, D], BF16) for i in range(2)]
    catT = [A.alloc(f"catT{i}", [128, 8, 128], BF16) for i in range(2)]
    xt = [A.alloc(f"oxt{i}", [128, D], F32) for i in range(2)]
    ytmp = A.alloc("ytmp", [128, D], F32)
    x1 = [A.alloc(f"x1_{i}", [128, D], F32) for i in range(2)]
    junk = A.alloc("ojunk", [128, D], BF16)
    ss = A.alloc("oss", [128, 1], F32)
    sst = A.alloc("osst", [128, 1], F32)
    rstd = A.alloc("orstd", [128, 1], F32)
    h2f = A.alloc("h2f", [128, D], F32)
    h2b = [A.alloc(f"h2b{i}", [128, D], BF16) for i in range(2)]
    h2T = A.alloc("h2T", [128, 8, 128], F32)
    lg = A.alloc("lg", [128, 36], F32)
    sm = {n: A.alloc("r_" + n, sh, F32) for n, sh in (
        ("gmax", [128, 1]), ("ngmax", [128, 1]), ("eg", [128, 4]), ("sumg", [128, 1]), ("gw", [128, 1]),
        ("ohg", [128, 4]), ("elm", [128, 4, 8]), ("els", [128, 8]), ("top8", [128, 8]), ("dd", [128, 1]),
        ("rd", [128, 1]), ("wk", [128, 2]), ("oh", [128, 2, 8]), ("E", [128, 2, 32]), ("mask", [128, 32]),
        ("pos", [128, 32]), ("val", [128, 32]), ("tmp32", [128, 32]), ("sk", [128, 2]), ("pk", [128, 2]),
        ("ok", [128, 2]))}
    maskb = A.alloc("maskb", [128, 32], BF16)
    BIGV = float(NSLOT + 4096)
    def load_t(t):
        rows = slice(t * 128, (t + 1) * 128)
        S.dma('sp', catt[t % 2].t[:], k.cat_d[rows, :], (), [catt[t % 2]])
        S.dma('sp', xt[t % 2].t[:], k.x[rows, :], (), [xt[t % 2]])
    load_t(0)
    for t in range(NT):
        rows = slice(t * 128, (t + 1) * 128)
        i2 = t % 2
        if t + 1 < NT:
            load_t(t + 1)
        PT_ = k.pb[0]
        pv = k.pbf(0).rearrange("p (a b) -> p a b", a=8)
        for kk in range(8):
            S.tr(pv[:, kk, :], catt[i2].t[:, kk * 128:(kk + 1) * 128], k.ident_bf.t[:], [catt[i2], k.ident_bf], [PT_])
        S.cp('act', catT[i2].t[:], pv, [PT_], [catT[i2]])
        for half in range(2):
            P = k.pb[1 + half]
            hs = slice(half * 512, (half + 1) * 512)
            for kk in range(8):
                S.mm(P.t[:], catT[i2].t[:, kk, :], Wout.t[:, kk, hs], kk == 0, kk == 7, [catT[i2], Wout], [P])
            S.tt('dve', ytmp.t[:, hs], P.t[:], k.G1b.t[:, hs], ALU.mult, [P, k.G1b], [ytmp])
        S.tt('pool', x1[i2].t[:], ytmp.t[:], xt[i2].t[:], ALU.add, [ytmp, xt[i2]], [x1[i2]])
        S.dma('sp', k.x1_d[rows, :], x1[i2].t[:], [x1[i2]], ())
        S.act(junk.t[:], x1[i2].t[:], AF.Square, [x1[i2]], [junk, ss], accum_out=ss.t[:])
        rstd_from_ss(k, ss, sst, rstd, D)
        S.stt('dve', h2f.t[:], x1[i2].t[:], rstd.t[:, 0:1], k.A2.t[:], ALU.mult, ALU.mult, [x1[i2], rstd, k.A2], [h2f])
        S.tt('pool', h2f.t[:], h2f.t[:], k.B2.t[:], ALU.add, [h2f, k.B2], [h2f])
        S.cp('act', h2b[i2].t[:], h2f.t[:], [h2f], [h2b[i2]])
        for g4 in range(2):
            P = k.pb[3 + g4]
            pv4 = P.t[:].rearrange("p (a b) -> p a b", a=4)
            for q in range(4):
                kk = g4 * 4 + q
                S.tr(pv4[:, q, :], h2f.t[:, kk * 128:(kk + 1) * 128], k.ident_f.t[:], [h2f, k.ident_f], [P])
            S.cp('act' if g4 else 'dve', h2T.t[:, g4 * 4:(g4 + 1) * 4, :], pv4, [P], [h2T])
        P = k.pb[5]
        for kk in range(8):
            S.mm(P.t[:, 0:36], h2T.t[:, kk, :], Wr.t[:, kk, :], kk == 0, False, [h2T, Wr], [P])
        S.mm(P.t[:, 0:36], k.ones_f.t[0:1, :], brow.t[:], False, True, [k.ones_f, brow], [P])
        S.cp('dve', lg.t[:], P.t[:, 0:36], [P], [lg])
        g = sm
        S.op('dve', lambda e: e.tensor_reduce(out=g["gmax"].t[:], in_=lg.t[:, 0:4], axis=AX.X, op=ALU.max), [lg], [g["gmax"]])
        S.ts('dve', g["ngmax"].t[:], g["gmax"].t[:], -1.0, None, ALU.mult, None, [g["gmax"]], [g["ngmax"]])
        S.act(g["eg"].t[:], lg.t[:, 0:4], AF.Exp, [lg, g["ngmax"]], [g["eg"], g["sumg"]], bias=g["ngmax"].t[:, 0:1],
              accum_out=g["sumg"].t[:])
        S.op('dve', lambda e: e.reciprocal(g["gw"].t[:], g["sumg"].t[:]), [g["sumg"]], [g["gw"]])
        S.ts('dve', g["ohg"].t[:], lg.t[:, 0:4], g["gmax"].t[:, 0:1], None, ALU.is_equal, None, [lg, g["gmax"]], [g["ohg"]])
        S.tt('dve', g["elm"].t[:], lg.t[:, 4:36].rearrange("p (g e) -> p g e", g=4),
             g["ohg"].t[:].unsqueeze(2).broadcast_to([128, 4, 8]), ALU.mult, [lg, g["ohg"]], [g["elm"]])
        S.op('dve', lambda e: e.tensor_reduce(out=g["els"].t[:], in_=g["elm"].t[:].rearrange("p g e -> p e g"),
                                              axis=AX.X, op=ALU.add), [g["elm"]], [g["els"]])
        S.op('dve', lambda e: e.tensor_reduce(out=g["top8"].t[:, 0:1], in_=g["els"].t[:], axis=AX.X, op=ALU.max), [g["els"]], [g["top8"]])
        S.ts('dve', g["oh"].t[:, 0, :], g["els"].t[:], g["top8"].t[:, 0:1], None, ALU.is_equal, None, [g["els"], g["top8"]], [g["oh"]])
        S.stt('dve', g["oh"].t[:, 1, :], g["oh"].t[:, 0, :], NEG, g["els"].t[:], ALU.mult, ALU.add, [g["oh"], g["els"]], [g["oh"]])
        S.op('dve', lambda e: e.tensor_reduce(out=g["top8"].t[:, 1:2], in_=g["oh"].t[:, 1, :], axis=AX.X, op=ALU.max), [g["oh"]], [g["top8"]])
        S.tt('dve', g["dd"].t[:], g["top8"].t[:, 1:2], g["top8"].t[:, 0:1], ALU.subtract, [g["top8"]], [g["dd"]])
        S.act(g["dd"].t[:], g["dd"].t[:], AF.Exp, [g["dd"]], [g["dd"]])
        S.ts('dve', g["dd"].t[:], g["dd"].t[:], 1.0, None, ALU.add, None, [g["dd"]], [g["dd"]])
        S.op('dve', lambda e: e.reciprocal(g["rd"].t[:], g["dd"].t[:]), [g["dd"]], [g["rd"]])
        S.tt('dve', g["wk"].t[:, 0:1], g["gw"].t[:], g["rd"].t[:], ALU.mult, [g["gw"], g["rd"]], [g["wk"]])
        S.tt('dve', g["wk"].t[:, 1:2], g["gw"].t[:], g["wk"].t[:, 0:1], ALU.subtract, [g["gw"], g["wk"]], [g["wk"]])
        for kk in range(2):
            S.ts('dve', g["oh"].t[:, kk, :], g["els"].t[:], g["top8"].t[:, kk:kk + 1], None, ALU.is_equal, None,
                 [g["els"], g["top8"]], [g["oh"]])
            S.tt('dve', g["E"].t[:, kk, :].rearrange("p (g e) -> p g e", g=4),
                 g["ohg"].t[:].unsqueeze(2).broadcast_to([128, 4, 8]),
                 g["oh"].t[:, kk, :].unsqueeze(1).broadcast_to([128, 4, 8]), ALU.mult, [g["ohg"], g["oh"]], [g["E"]])
        S.tt('dve', g["mask"].t[:], g["E"].t[:, 0, :], g["E"].t[:, 1, :], ALU.add, [g["E"]], [g["mask"]])
        S.cp('dve', maskb.t[:], g["mask"].t[:], [g["mask"]], [maskb])
        P = k.pb[6]
        S.mm(P.t[:, 0:32], k.stri_bf.t[:], maskb.t[:], True, True, [k.stri_bf, maskb], [P])
        S.mm(P.t[:, 32:64], k.ones_bf.t[:], maskb.t[:], True, True, [k.ones_bf, maskb], [P])
        S.tt('dve', g["pos"].t[:], P.t[:, 0:32], cntb.t[:], ALU.add, [P, cntb], [g["pos"]])
        S.tt('dve', cntb.t[:], P.t[:, 32:64], cntb.t[:], ALU.add, [P, cntb], [cntb])
        S.tt('dve', g["val"].t[:], g["pos"].t[:], ecap.t[:], ALU.add, [g["pos"], ecap], [g["val"]])
        for kk in range(2):
            S.tt('dve', g["tmp32"].t[:], g["E"].t[:, kk, :], g["val"].t[:], ALU.mult, [g["E"], g["val"]], [g["tmp32"]])
            S.op('dve', lambda e, kk=kk: e.tensor_reduce(out=g["sk"].t[:, kk:kk + 1], in_=g["tmp32"].t[:], axis=AX.X, op=ALU.add),
                 [g["tmp32"]], [g["sk"]])
            S.tt('dve', g["tmp32"].t[:], g["E"].t[:, kk, :], g["pos"].t[:], ALU.mult, [g["E"], g["pos"]], [g["tmp32"]])
            S.op('dve', lambda e, kk=kk: e.tensor_reduce(out=g["pk"].t[:, kk:kk + 1], in_=g["tmp32"].t[:], axis=AX.X, op=ALU.add),
                 [g["tmp32"]], [g["pk"]])
        S.ts('dve', g["ok"].t[:], g["pk"].t[:], float(CAP) - 0.5, None, ALU.is_lt, None, [g["pk"]], [g["ok"]])
        S.ts('dve', g["sk"].t[:], g["sk"].t[:], -BIGV, None, ALU.add, None, [g["sk"]], [g["sk"]])
        S.tt('dve', g["sk"].t[:], g["sk"].t[:], g["ok"].t[:], ALU.mult, [g["sk"], g["ok"]], [g["sk"]])
        S.ts('dve', g["sk"].t[:], g["sk"].t[:], BIGV, None, ALU.add, None, [g["sk"]], [g["sk"]])
        S.cp('dve', k.slot_i.t[:, t, :], g["sk"].t[:], [g["sk"]], [k.slot_i])
        S.tt('dve', k.wts.t[:, t, :], g["wk"].t[:], g["ok"].t[:], ALU.mult, [g["wk"], g["ok"]], [k.wts])
        for kk in range(2):
            def fn(e, t=t, kk=kk, src=h2b[i2]):
                return e.indirect_dma_start(out=k.XS_d[:, :], out_offset=bass.IndirectOffsetOnAxis(ap=k.slot_i.t[:, t, kk:kk + 1], axis=0),
                                            in_=src.t[:], in_offset=None, bounds_check=k.bcreg(e), oob_is_err=False)
            S.swdma(fn, [h2b[i2], k.slot_i, k.xs_tok], [])
    S.barrier()
    A.release(m0)


def phase_experts(k):
    S, A = k.S, k.A
    CAP, NG = k.CAP, k.NG
    m0 = A.mark()
    stg = [A.alloc(f"estg{i}", [128, 8, 512], F32) for i in range(3)]
    W1b = [A.alloc(f"W1b{i}", [128, 8, 512], BF16) for i in range(2)]
    W3b = [A.alloc(f"W3b{i}", [128, 8, 512], BF16) for i in range(2)]
    W2b = [A.alloc(f"W2b{i}", [128, 4, 1024], BF16) for i in range(2)]
    xs = [A.alloc(f"xs{i}", [128, NG // 128, D], BF16) for i in range(2)]
    xT = [A.alloc(f"xT{i}", [128, 8, NG], BF16) for i in range(2)]
    sl = [A.alloc(f"sl{i}", [128, NG], F32) for i in range(2)]
    G = [A.alloc(f"G{i}", [128, 4, NG], BF16) for i in range(2)]
    yb = [A.alloc(f"yb{i}", [128, D], BF16) for i in range(2)]
    st = {"si": 0, "yi": 0}

    def load_w(e_):
        i2 = e_ % 2
        for (src, dst, kc) in ((k.w1[e_], W1b[i2], 8), (k.w3[e_], W3b[i2], 8)):
            sg = stg[st["si"] % len(stg)]
            st["si"] += 1
            S.dma('sp', sg.t[:], src.rearrange("(c p) n -> p c n", p=128), (), [sg])
            S.cp('pool', dst.t[:], sg.t[:], [sg], [dst])
        for hh in range(2):
            sg = stg[st["si"] % len(stg)]
            st["si"] += 1
            sv = sg.t[:, 0:4, :]
            S.dma('sp', sv, k.w2[e_][:, hh * 512:(hh + 1) * 512].rearrange("(c p) n -> p c n", p=128), (), [sg])
            S.cp('pool', W2b[i2].t[:, :, hh * 512:(hh + 1) * 512], sv, [sg], [W2b[i2]])

    groups = [(e_, gq) for e_ in range(N_EXP) for gq in range(CAP // NG)]

    def load_xs(idx):
        e_, gq = groups[idx]
        r0 = e_ * CAP + gq * NG
        S.dma('sp', xs[idx % 2].t[:], k.XS_d[r0:r0 + NG, :].rearrange("(j p) d -> p j d", p=128), (), [xs[idx % 2]])

    load_w(0)
    load_xs(0)
    for idx, (e_, gq) in enumerate(groups):
        i2 = e_ % 2
        g2 = idx % 2
        r0 = e_ * CAP + gq * NG
        if gq == 0 and e_ + 1 < N_EXP:
            load_w(e_ + 1)
        if idx + 1 < len(groups):
            load_xs(idx + 1)
        for j in range(NG // 128):
            PT_ = k.pb[j % 2]
            pv = k.pbf(j % 2).rearrange("p (a b) -> p a b", a=8)
            for kk in range(8):
                S.tr(pv[:, kk, :], xs[g2].t[:, j, kk * 128:(kk + 1) * 128], k.ident_bf.t[:], [xs[g2], k.ident_bf], [PT_])
            S.cp('act' if j % 2 else 'dve', xT[g2].t[:, :, j * 128:(j + 1) * 128], pv, [PT_], [xT[g2]])
        for f in range(4):
            P1_ = k.pb[2 + (f % 2)]
            P3_ = k.pb[4 + (f % 2)]
            fs = slice(f * 128, (f + 1) * 128)
            for kk in range(8):
                S.mm(P1_.t[:, 0:NG], W1b[i2].t[:, kk, fs], xT[g2].t[:, kk, :], kk == 0, kk == 7, [W1b[i2], xT[g2]], [P1_])
            for kk in range(8):
                S.mm(P3_.t[:, 0:NG], W3b[i2].t[:, kk, fs], xT[g2].t[:, kk, :], kk == 0, kk == 7, [W3b[i2], xT[g2]], [P3_])
            S.act(sl[f % 2].t[:], P1_.t[:, 0:NG], AF.Silu, [P1_], [sl[f % 2]])
            S.tt('dve', G[g2].t[:, f, :], sl[f % 2].t[:], P3_.t[:, 0:NG], ALU.mult, [sl[f % 2], P3_], [G[g2]])
        for j in range(NG // 128):
            y_ = yb[st["yi"] % len(yb)]
            st["yi"] += 1
            for half in range(2):
                P = k.pb[6 + half]
                for f in range(4):
                    S.mm(P.t[:], G[g2].t[:, f, j * 128:(j + 1) * 128], W2b[i2].t[:, f, half * 512:(half + 1) * 512],
                         f == 0, f == 3, [G[g2], W2b[i2]], [P])
                S.cp('act' if half else 'dve', y_.t[:, half * 512:(half + 1) * 512], P.t[:], [P], [y_])
            S.dma('sp', k.YS_d[r0 + j * 128:r0 + (j + 1) * 128, :], y_.t[:], [y_], ())
    S.barrier()
    A.release(m0)


def phase_final(k):
    S, A = k.S, k.A
    NT, NSLOT = k.NT, k.NSLOT
    m0 = A.mark()
    gnf = A.alloc("gnf", [128, D], F32)
    bcast_load(k, gnf, k.normf_g)
    Y = [[A.alloc(f"Y{i}{kk}", [128, D], BF16) for kk in range(2)] for i in range(2)]
    for i in range(2):
        for kk in range(2):
            S.ms('pool', Y[i][kk].t[:], 0.0, [Y[i][kk]])
    x1 = [A.alloc(f"fx1_{i}", [128, D], F32) for i in range(2)]
    moe = A.alloc("moe", [128, D], F32)
    x2 = A.alloc("x2", [128, D], F32)
    junk = A.alloc("fjunk", [128, D], BF16)
    ss = A.alloc("fss", [128, 1], F32)
    sst = A.alloc("fsst", [128, 1], F32)
    rstd = A.alloc("frstd", [128, 1], F32)
    ot = [A.alloc(f"ot{i}", [128, D], F32) for i in range(2)]
    def load_f(t):
        for kk in range(2):
            def fn(e, t=t, kk=kk, dst=Y[t % 2][kk]):
                return e.indirect_dma_start(out=dst.t[:], out_offset=None, in_=k.YS_d[:, :],
                                            in_offset=bass.IndirectOffsetOnAxis(ap=k.slot_i.t[:, t, kk:kk + 1], axis=0),
                                            bounds_check=k.bcreg(e), oob_is_err=False)
            S.swdma(fn, [k.slot_i], [Y[t % 2][kk]])
        S.dma('sp', x1[t % 2].t[:], k.x1_d[t * 128:(t + 1) * 128, :], (), [x1[t % 2]])
    load_f(0)
    for t in range(NT):
        rows = slice(t * 128, (t + 1) * 128)
        i2 = t % 2
        if t + 1 < NT:
            load_f(t + 1)
        S.ts('dve', moe.t[:], Y[i2][0].t[:], k.wts.t[:, t, 0:1], None, ALU.mult, None, [Y[i2][0], k.wts], [moe])
        S.stt('dve', moe.t[:], Y[i2][1].t[:], k.wts.t[:, t, 1:2], moe.t[:], ALU.mult, ALU.add, [Y[i2][1], k.wts, moe], [moe])
        S.tt('dve', moe.t[:], moe.t[:], k.G2b.t[:], ALU.mult, [moe, k.G2b], [moe])
        S.tt('dve', x2.t[:], moe.t[:], x1[i2].t[:], ALU.add, [moe, x1[i2]], [x2])
        S.act(junk.t[:], x2.t[:], AF.Square, [x2], [junk, ss], accum_out=ss.t[:])
        rstd_from_ss(k, ss, sst, rstd, D)
        S.stt('dve', ot[i2].t[:], x2.t[:], rstd.t[:, 0:1], gnf.t[:], ALU.mult, ALU.mult, [x2, rstd, gnf], [ot[i2]])
        S.dma('sp', k.out[rows, :], ot[i2].t[:], [ot[i2]], ())
    S.barrier()
    A.release(m0)


CAP_DEFAULT = 1024


def kernel(**inputs):
    inputs = {kk: np.asarray(v) for kk, v in inputs.items()}
    n = inputs["x"].shape[0]
    s_tok = inputs["x"].shape[1]
    nc = build_nc(s_tok, CAP_DEFAULT)
    in_maps = [make_in_map(inputs, b) for b in range(n)]
    res = run_bass_kernel_spmd(nc, in_maps, core_ids=list(range(n)))
    out = np.stack([np.asarray(r["out"]) for r in res.results], axis=0)
    return out.astype(np.float32)
```
